# Optimizing a Trainium2 kernel written in Bass

```python
import math
import jax, jax.numpy as jnp
from jax import lax
import numpy as np

D_MODEL = 2048
BATCH = 4
SEQ = 4096
DEPTH = 2

GRID_W = 64
CTX_LEN = 256
EPS = 1e-6
ATT_HEADS = 8
ATT_DQK = 64
ATT_DV = 2 * ATT_DQK
ATT_W = ATT_HEADS * ATT_DV
QK_COLS = ATT_HEADS * 2 * ATT_DQK
Q_BLOCK = 128
ROPE_BASE = 10000.0
CONF_W = 512
CONF_K = 31
HY_W = 512
HY_ORDER = 2
HY_SHORT_K = 3
HY_EMB = 33
HY_BANDS = (HY_EMB - 1) // 2
HY_FFN = 64
HY_FILT = HY_ORDER * 2 * HY_W
N_BRANCH = 3
SPLIT_POINTS = (QK_COLS, 2 * QK_COLS, 2 * QK_COLS + ATT_W, 2 * QK_COLS + ATT_W + 2 * CONF_W,
                2 * QK_COLS + ATT_W + 2 * CONF_W + (HY_ORDER + 1) * HY_W)
IN_COLS = SPLIT_POINTS[-1] + N_BRANCH * D_MODEL
N_GROUPS = 8
E_PER_GROUP = 8
N_EXPERTS = N_GROUPS * E_PER_GROUP
TOP_K = 2
D_EXPERT = 512
MOE_BLOCK = 128

kernel_name = "hybrid_diffattn_conformer_hyena_hmoe_dit"

F32 = jnp.float32


def rms_norm(x, g, eps=EPS):
    xf = x.astype(F32)
    y = xf * lax.rsqrt(jnp.mean(xf * xf, axis=-1, keepdims=True) + eps)
    return (y * g.astype(F32)).astype(x.dtype)


def layer_norm(x, g, b, eps=1e-5):
    xf = x.astype(F32)
    mu = jnp.mean(xf, axis=-1, keepdims=True)
    xc = xf - mu
    y = xc * lax.rsqrt(jnp.mean(xc * xc, axis=-1, keepdims=True) + eps)
    return (y * g.astype(F32) + b.astype(F32)).astype(x.dtype)


def ada_norm(x, g, shift, scale):
    return rms_norm(x, g) * (1 + scale) + shift


def depthwise_conv(x, w, b):
    C = x.shape[-1]
    y = lax.conv_general_dilated(x, w[:, None, :].astype(x.dtype), window_strides=(1,), padding='SAME',
                                 dimension_numbers=('NWC', 'WIO', 'NWC'), feature_group_count=C)
    return y + b.astype(x.dtype)


def axial_rope_tables(rows, cols):
    half = ATT_DQK // 2
    inv = ROPE_BASE ** (-jnp.arange(0, half, 2, dtype=F32) / half)
    ar = rows.astype(F32)[:, None] * inv
    ac = cols.astype(F32)[:, None] * inv
    return jnp.cos(ar), jnp.sin(ar), jnp.cos(ac), jnp.sin(ac)


def _rot_half(x, cos, sin):
    x1, x2 = jnp.split(x, 2, axis=-1)
    cos = cos[:, None, None, :]
    sin = sin[:, None, None, :]
    return jnp.concatenate([x1 * cos - x2 * sin, x1 * sin + x2 * cos], axis=-1).astype(x.dtype)


def apply_axial_rope(x, tables):
    cr, sr, cc, sc = tables
    xr, xc = jnp.split(x, 2, axis=-1)
    return jnp.concatenate([_rot_half(xr, cr, sr), _rot_half(xc, cc, sc)], axis=-1)


def diff_attention(q, k, v, lam):
    s = jnp.einsum('bqhcd,bkhcd->bhcqk', q, k).astype(F32) * (q.shape[-1] ** -0.5)
    p = jax.nn.softmax(s, axis=-1)
    a = p[:, :, 0] - lam * p[:, :, 1]
    return jnp.einsum('bhqk,bkhe->bqhe', a.astype(v.dtype), v)


def blocked_diff_attention(q, k, v, lam):
    B, S, H, _, dqk = q.shape
    nb = S // Q_BLOCK
    qb = jnp.moveaxis(q.reshape(B, nb, Q_BLOCK, H, 2, dqk), 1, 0)
    ob = lax.map(lambda qi: diff_attention(qi, k, v, lam), qb)
    return jnp.moveaxis(ob, 0, 1).reshape(B, S, H, v.shape[-1])


def diff_attn_out(o, subln_g, lam_init, w_o):
    B, L = o.shape[:2]
    o = rms_norm(o, subln_g, 1e-5) * (1.0 - lam_init)
    return o.reshape(B, L, ATT_W) @ w_o


def conformer_branch(u, dw_w, dw_b, ln_g, ln_b, w_o):
    a, g = jnp.split(u, 2, axis=-1)
    y = a * jax.nn.sigmoid(g)
    y = depthwise_conv(y, dw_w, dw_b)
    y = jax.nn.silu(layer_norm(y, ln_g, ln_b))
    return y @ w_o


def hyena_filter_fft(L, w1, b1, freq, w2, b2, w3, decay):
    n = jnp.arange(L, dtype=F32)
    t = n / max(L - 1, 1)
    w = 2.0 * math.pi * n / L
    f = jnp.linspace(1e-4, HY_BANDS - 1, HY_BANDS, dtype=F32)
    fw = w[:, None] * f[None, :]
    emb = jnp.concatenate([t[:, None], jnp.cos(fw), -jnp.sin(fw)], axis=-1)
    h = jnp.sin(freq[0].astype(F32) * (emb @ w1.astype(F32) + b1.astype(F32)))
    h = jnp.sin(freq[1].astype(F32) * (h @ w2.astype(F32) + b2.astype(F32)))
    h = (h @ w3.astype(F32)) * jnp.exp(-t[:, None] * jnp.abs(decay.astype(F32)))
    h = h.reshape(L, HY_ORDER, 2, HY_W)
    hf, hb = h[:, :, 0], h[:, :, 1]
    k = jnp.concatenate([hf, jnp.zeros((1, HY_ORDER, HY_W), F32), hb[1:][::-1]], axis=0)
    k = k / jnp.sum(jnp.abs(k), axis=0, keepdims=True)
    return jnp.fft.rfft(k, axis=0)


def fft_conv(z, kf):
    L = z.shape[1]
    zf = jnp.fft.rfft(z.astype(F32), n=2 * L, axis=1)
    y = jnp.fft.irfft(zf * kf[None], n=2 * L, axis=1)[:, :L]
    return y.astype(z.dtype)


def hyena_branch(u, sc_w, sc_b, w1, b1, w2, b2, freq, w3, decay, bias, w_o):
    L = u.shape[1]
    u = depthwise_conv(u, sc_w, sc_b)
    x1, x2, z = jnp.split(u, 3, axis=-1)
    kf = hyena_filter_fft(L, w1, b1, freq, w2, b2, w3, decay)
    for n, gate in enumerate((x1, x2)):
        z = gate * (fft_conv(z, kf[:, n]) + bias[n].astype(z.dtype) * z)
    return z @ w_o


def gated_merge(gate_logits, a, b, c, w_out):
    B, L, _ = gate_logits.shape
    g = jax.nn.sigmoid(gate_logits.astype(F32)).reshape(B, L, N_BRANCH, D_MODEL).astype(a.dtype)
    return (g[:, :, 0] * a + g[:, :, 1] * b + g[:, :, 2] * c) @ w_out


def token_mixer(hl, hc, rope_tab, lam, lam_init, w_in, attn_subln_g, w_attn_o, conf_dw_w, conf_dw_b,
                conf_ln_g, conf_ln_b, w_conf_o, hy_sc_w, hy_sc_b, hy_w1, hy_b1, hy_w2, hy_b2, hy_freq,
                hy_w3, hy_decay, hy_bias, w_hy_o, w_out, need_ctx):
    B, S, _ = hl.shape
    Lc = hc.shape[1]
    q_l, k_l, v_l, glu_l, hy_l, gate_l = jnp.split(hl @ w_in, SPLIT_POINTS, axis=-1)
    if need_ctx:
        q_c, k_c, v_c, glu_c, hy_c, gate_c = jnp.split(hc @ w_in, SPLIT_POINTS, axis=-1)
    else:
        k_c, v_c = jnp.split(hc @ w_in[:, SPLIT_POINTS[0]:SPLIT_POINTS[2]], 2, axis=-1)
    k_c = k_c.reshape(B, Lc, ATT_HEADS, 2, ATT_DQK)
    v_c = v_c.reshape(B, Lc, ATT_HEADS, ATT_DV)
    q_l = apply_axial_rope(q_l.reshape(B, S, ATT_HEADS, 2, ATT_DQK), rope_tab)
    k_l = apply_axial_rope(k_l.reshape(B, S, ATT_HEADS, 2, ATT_DQK), rope_tab)
    k_all = jnp.concatenate([k_c, k_l], axis=1)
    v_all = jnp.concatenate([v_c, v_l.reshape(B, S, ATT_HEADS, ATT_DV)], axis=1)
    att_l = diff_attn_out(blocked_diff_attention(q_l, k_all, v_all, lam), attn_subln_g, lam_init, w_attn_o)
    conf_l = conformer_branch(glu_l, conf_dw_w, conf_dw_b, conf_ln_g, conf_ln_b, w_conf_o)
    hyena_l = hyena_branch(hy_l, hy_sc_w, hy_sc_b, hy_w1, hy_b1, hy_w2, hy_b2, hy_freq, hy_w3, hy_decay,
                           hy_bias, w_hy_o)
    mix_l = gated_merge(gate_l, att_l, conf_l, hyena_l, w_out)
    if not need_ctx:
        return mix_l, None
    q_c = q_c.reshape(B, Lc, ATT_HEADS, 2, ATT_DQK)
    att_c = diff_attn_out(diff_attention(q_c, k_c, v_c, lam), attn_subln_g, lam_init, w_attn_o)
    conf_c = conformer_branch(glu_c, conf_dw_w, conf_dw_b, conf_ln_g, conf_ln_b, w_conf_o)
    hyena_c = hyena_branch(hy_c, hy_sc_w, hy_sc_b, hy_w1, hy_b1, hy_w2, hy_b2, hy_freq, hy_w3, hy_decay,
                           hy_bias, w_hy_o)
    mix_c = gated_merge(gate_c, att_c, conf_c, hyena_c, w_out)
    return mix_l, mix_c


def hier_moe(h, w_rg, w_re, w_gate, w_up, w_down):
    T, D = h.shape
    lg = (h @ w_rg).astype(F32)
    pg = jax.nn.softmax(lg, axis=-1)
    _, g_idx = lax.top_k(lg, 1)
    g_idx = g_idx[:, 0]
    p_grp = jnp.take_along_axis(pg, g_idx[:, None], axis=1)[:, 0]
    le = jnp.einsum('td,gde->tge', h, w_re).astype(F32)
    le = jnp.take_along_axis(le, g_idx[:, None, None], axis=1)[:, 0]
    e_val, e_idx = lax.top_k(le, TOP_K)
    wts = (jax.nn.softmax(e_val, axis=-1) * p_grp[:, None]).reshape(-1)
    eid = (g_idx[:, None] * E_PER_GROUP + e_idx).reshape(-1)
    tok = jnp.repeat(jnp.arange(T, dtype=jnp.int32), TOP_K)
    order = jnp.argsort(eid)
    e_s, t_s, w_s = eid[order], tok[order], wts[order]
    counts = jnp.bincount(eid, length=N_EXPERTS).astype(jnp.int32)
    pcounts = (counts + MOE_BLOCK - 1) // MOE_BLOCK * MOE_BLOCK
    pend = jnp.cumsum(pcounts)
    pstart = pend - pcounts
    cstart = jnp.cumsum(counts) - counts
    A = T * TOP_K
    dest = pstart[e_s] + (jnp.arange(A, dtype=jnp.int32) - cstart[e_s])
    n_blocks = -(-A // MOE_BLOCK) + N_EXPERTS
    P = n_blocks * MOE_BLOCK
    buf_tok = jnp.full((P,), T, jnp.int32).at[dest].set(t_s)
    buf_w = jnp.zeros((P,), F32).at[dest].set(w_s)
    blk_e = jnp.minimum(jnp.searchsorted(pend, jnp.arange(n_blocks, dtype=jnp.int32) * MOE_BLOCK, side='right'),
                        N_EXPERTS - 1)
    h_pad = jnp.concatenate([h, jnp.zeros((1, D), h.dtype)], axis=0)

    def run_block(args):
        idx, e = args
        xb = h_pad[idx]
        return (jax.nn.silu(xb @ w_gate[e]) * (xb @ w_up[e])) @ w_down[e]

    y = lax.map(run_block, (buf_tok.reshape(n_blocks, MOE_BLOCK), blk_e)).reshape(P, D)
    y = y * buf_w[:, None].astype(y.dtype)
    return jax.ops.segment_sum(y, buf_tok, num_segments=T + 1)[:T]


def setup_inputs(seed: int = 0) -> dict:
    key = jax.random.key(seed)
    ks = iter(jax.random.split(key, 64))
    D = D_MODEL

    def nrm(shape, scale):
        return jax.random.normal(next(ks), shape, F32) * scale

    return {
        'x': nrm((BATCH, SEQ, D), 1.0),
        'c': nrm((BATCH, D), 1.0),
        'ctx': nrm((BATCH, CTX_LEN, D), 1.0),
        'c_ctx': nrm((D,), 1.0),
        'w_mod': nrm((DEPTH, D, 6 * D), 0.5 * D ** -0.5),
        'b_mod': nrm((DEPTH, 6 * D), 0.02),
        'norm1_g': 1.0 + nrm((DEPTH, D), 0.02),
        'norm2_g': 1.0 + nrm((DEPTH, D), 0.02),
        'w_in': nrm((DEPTH, D, IN_COLS), D ** -0.5),
        'lam_q1': nrm((DEPTH, ATT_DQK), 0.1),
        'lam_k1': nrm((DEPTH, ATT_DQK), 0.1),
        'lam_q2': nrm((DEPTH, ATT_DQK), 0.1),
        'lam_k2': nrm((DEPTH, ATT_DQK), 0.1),
        'attn_subln_g': 1.0 + nrm((DEPTH, ATT_DV), 0.02),
        'w_attn_o': nrm((DEPTH, ATT_W, D), ATT_W ** -0.5),
        'conf_dw_w': nrm((DEPTH, CONF_K, CONF_W), CONF_K ** -0.5),
        'conf_dw_b': nrm((DEPTH, CONF_W), 0.02),
        'conf_ln_g': 1.0 + nrm((DEPTH, CONF_W), 0.02),
        'conf_ln_b': nrm((DEPTH, CONF_W), 0.02),
        'w_conf_o': nrm((DEPTH, CONF_W, D), CONF_W ** -0.5),
        'hy_sc_w': nrm((DEPTH, HY_SHORT_K, (HY_ORDER + 1) * HY_W), HY_SHORT_K ** -0.5),
        'hy_sc_b': nrm((DEPTH, (HY_ORDER + 1) * HY_W), 0.02),
        'hy_w1': nrm((DEPTH, HY_EMB, HY_FFN), HY_EMB ** -0.5),
        'hy_b1': nrm((DEPTH, HY_FFN), 0.02),
        'hy_w2': nrm((DEPTH, HY_FFN, HY_FFN), HY_FFN ** -0.5),
        'hy_b2': nrm((DEPTH, HY_FFN), 0.02),
        'hy_freq': 1.0 + nrm((DEPTH, 2, HY_FFN), 0.1),
        'hy_w3': nrm((DEPTH, HY_FFN, HY_FILT), HY_FFN ** -0.5),
        'hy_decay': jax.random.uniform(next(ks), (DEPTH, HY_FILT), F32, minval=3.0, maxval=15.0),
        'hy_bias': nrm((DEPTH, HY_ORDER, HY_W), 0.1),
        'w_hy_o': nrm((DEPTH, HY_W, D), HY_W ** -0.5),
        'w_out': nrm((DEPTH, D, D), D ** -0.5),
        'w_router_group': nrm((DEPTH, D, N_GROUPS), D ** -0.5),
        'w_router_expert': nrm((DEPTH, N_GROUPS, D, E_PER_GROUP), D ** -0.5),
        'w_exp_gate': nrm((DEPTH, N_EXPERTS, D, D_EXPERT), D ** -0.5),
        'w_exp_up': nrm((DEPTH, N_EXPERTS, D, D_EXPERT), D ** -0.5),
        'w_exp_down': nrm((DEPTH, N_EXPERTS, D_EXPERT, D), D_EXPERT ** -0.5),
        'norm_f_g': 1.0 + nrm((D,), 0.02),
    }


def reference(x, c, ctx, c_ctx, w_mod, b_mod, norm1_g, norm2_g, w_in, lam_q1, lam_k1, lam_q2, lam_k2,
              attn_subln_g, w_attn_o, conf_dw_w, conf_dw_b, conf_ln_g, conf_ln_b, w_conf_o, hy_sc_w, hy_sc_b,
              hy_w1, hy_b1, hy_w2, hy_b2, hy_freq, hy_w3, hy_decay, hy_bias, w_hy_o, w_out, w_router_group,
              w_router_expert, w_exp_gate, w_exp_up, w_exp_down, norm_f_g):
    B, S, D = x.shape
    Lc = ctx.shape[1]
    ROWS = S // GRID_W
    rows = jnp.repeat(jnp.arange(ROWS, dtype=jnp.int32), GRID_W)
    cols = jnp.tile(jnp.arange(GRID_W, dtype=jnp.int32), ROWS)
    rope_tab = axial_rope_tables(rows, cols)
    s_c = jax.nn.silu(c)
    s_cc = jax.nn.silu(c_ctx)
    x_l, x_c = x, ctx
    for l in range(DEPTH):
        need_ctx = l < DEPTH - 1
        lam_init = 0.8 - 0.6 * math.exp(-0.3 * l)
        mod_l = (s_c @ w_mod[l] + b_mod[l])[:, None, :]
        mod_c = s_cc @ w_mod[l] + b_mod[l]
        sh1, sc1, g1, sh2, sc2, g2 = jnp.split(mod_l, 6, axis=-1)
        csh1, csc1, cg1, csh2, csc2, cg2 = jnp.split(mod_c, 6, axis=-1)
        lam = (jnp.exp(jnp.sum(lam_q1[l].astype(F32) * lam_k1[l].astype(F32)))
               - jnp.exp(jnp.sum(lam_q2[l].astype(F32) * lam_k2[l].astype(F32))) + lam_init)
        hl = ada_norm(x_l, norm1_g[l], sh1, sc1)
        hc = ada_norm(x_c, norm1_g[l], csh1, csc1)
        mix_l, mix_c = token_mixer(hl, hc, rope_tab, lam, lam_init, w_in[l], attn_subln_g[l], w_attn_o[l],
                                   conf_dw_w[l], conf_dw_b[l], conf_ln_g[l], conf_ln_b[l], w_conf_o[l],
                                   hy_sc_w[l], hy_sc_b[l], hy_w1[l], hy_b1[l], hy_w2[l], hy_b2[l], hy_freq[l],
                                   hy_w3[l], hy_decay[l], hy_bias[l], w_hy_o[l], w_out[l], need_ctx)
        x_l = x_l + g1 * mix_l
        hl2 = ada_norm(x_l, norm2_g[l], sh2, sc2)
        if need_ctx:
            x_c = x_c + cg1 * mix_c
            hc2 = ada_norm(x_c, norm2_g[l], csh2, csc2)
            tokens = jnp.concatenate([hc2.reshape(-1, D), hl2.reshape(-1, D)], axis=0)
            y = hier_moe(tokens, w_router_group[l], w_router_expert[l], w_exp_gate[l], w_exp_up[l], w_exp_down[l])
            x_c = x_c + cg2 * y[:B * Lc].reshape(B, Lc, D)
            y_l = y[B * Lc:]
        else:
            y_l = hier_moe(hl2.reshape(-1, D), w_router_group[l], w_router_expert[l], w_exp_gate[l],
                           w_exp_up[l], w_exp_down[l])
        x_l = x_l + g2 * y_l.reshape(B, S, D)
    return rms_norm(x_l, norm_f_g)
```

```python
import numpy as np
import concourse.bass as bass
import concourse.mybir as mybir
from concourse.bass_utils import run_bass_kernel_spmd

F32 = mybir.dt.float32
BF16 = mybir.dt.bfloat16
I32 = mybir.dt.int32
ALU = mybir.AluOpType
AF = mybir.ActivationFunctionType
AX = mybir.AxisListType

ENGS = ("tensor", "vector", "scalar", "gpsimd", "sync")


class TT:
    def __init__(self, prog, t, name, acc=False):
        self.prog = prog
        self.t = t
        self.name = name
        self.acc = acc
        self.wr = {}
        self.rd = {}
        self.sem_in = None
        self.sem_out = None

    def __getitem__(self, k):
        return self.t[k]

    def ap(self):
        return self.t[:] if not hasattr(self.t, "ap") else self.t.ap()


class _Rec:
    def __init__(self):
        self.call = None

    def __getattr__(self, name):
        def f(*a, **kw):
            self.call = (name, a, kw)
            return self
        return f


def _capture(fn):
    r = _Rec()
    fn(r)
    assert r.call is not None
    return r.call


class Prog:
    def __init__(self, nc, self_wait=True):
        self.nc = nc
        self.q = {e: [] for e in ENGS}
        self.esem = {}
        self.ecnt = {e: 0 for e in ENGS}
        self.waited = {e: {} for e in ENGS}
        self.sems = {}
        self.semcnt = {}
        self.self_wait = self_wait
        self.nsem = 0
        self._ctx = []
        self._semctx = []
        self.free_sems = []
        self._tiles = []
        self.uid = 0
        self.cur_phase = "init"
        self.use_scopes = False
        for e in ENGS:
            self.esem[e] = self.new_sem("e_" + e)

    def new_sem(self, name):
        if name.startswith("d") and self.free_sems:
            return self.free_sems.pop()
        cm = self.nc.semaphore(name + "_%d" % self.nsem)
        s = cm.__enter__()
        self._semctx.append(cm)
        self.nsem += 1
        self.semcnt[id(s)] = 0
        self.sems[id(s)] = s
        return s

    def sb(self, name, shape, dt, acc=False):
        self.uid += 1
        name = "%s_u%d" % (name, self.uid)
        cm = self.nc.sbuf_tensor(name, list(shape), dt)
        t = cm.__enter__()
        self._ctx.append(cm)
        tt = TT(self, t, name, acc)
        self._tiles.append(tt)
        return tt

    def ps(self, name, shape, dt=F32):
        self.uid += 1
        name = "%s_u%d" % (name, self.uid)
        cm = self.nc.psum_tensor(name, list(shape), dt)
        t = cm.__enter__()
        self._ctx.append(cm)
        tt = TT(self, t, name)
        self._tiles.append(tt)
        return tt

    def free_to(self, mark):
        while len(self._ctx) > mark:
            self._ctx.pop().__exit__(None, None, None)
            tt = self._tiles.pop()
            for s in (tt.sem_in, tt.sem_out):
                if s is not None:
                    self.free_sems.append(s)

    def barrier(self):
        cur = {}
        for e in ENGS:
            cur[id(self.esem[e])] = self.ecnt[e]
        for k, v in self.semcnt.items():
            if v > 0 and k not in cur:
                cur[k] = v
        for e in ENGS:
            waits = []
            wd = self.waited[e]
            for k, v in cur.items():
                if v == 0 or wd.get(k, 0) >= v:
                    continue
                wd[k] = v
                waits.append((self.sems[k], v))
            self.q[e].append((waits, None, None, 0, self.cur_phase))

    def dram(self, name, shape, dt, kind="Internal", acc=True, addr_space="Local"):
        t = self.nc.dram_tensor(name, list(shape), dt, kind=kind, addr_space=addr_space)
        return TT(self, t, name, acc)

    def _deps(self, eng, reads, writes):
        deps = {}

        def add(d):
            for k, v in d.items():
                if deps.get(k, 0) < v:
                    deps[k] = v

        for r in reads:
            add(r.wr)
        for w in writes:
            add(w.rd)
            if not w.acc:
                add(w.wr)
        out = []
        wd = self.waited[eng]
        own = id(self.esem[eng])
        for k, v in deps.items():
            if k == own and (not self.self_wait or eng == "tensor"):
                continue
            if wd.get(k, 0) >= v:
                continue
            wd[k] = v
            out.append((self.sems[k], v))
        return out

    def _post(self, ev, reads, writes):
        k, v = ev
        for r in reads:
            if r.rd.get(k, 0) < v:
                r.rd[k] = v
        for w in writes:
            if w.acc:
                if w.wr.get(k, 0) < v:
                    w.wr[k] = v
            else:
                w.wr = {k: v}
                w.rd = {}

    def op(self, eng, fn, reads=(), writes=()):
        waits = self._deps(eng, reads, writes)
        self.ecnt[eng] += 1
        n = self.ecnt[eng]
        s = self.esem[eng]
        self.q[eng].append((waits, _capture(fn), s, 1, self.cur_phase))
        self._post((id(s), n), reads, writes)

    def dma(self, eng, fn, reads=(), writes=(), sb=None, into=True):
        waits = self._deps(eng, reads, writes)
        if into:
            if sb.sem_in is None:
                sb.sem_in = self.new_sem("di_" + sb.name)
            s = sb.sem_in
        else:
            if sb.sem_out is None:
                sb.sem_out = self.new_sem("do_" + sb.name)
            s = sb.sem_out
        self.semcnt[id(s)] += 16
        v = self.semcnt[id(s)]
        self.q[eng].append((waits, _capture(fn), s, 16, self.cur_phase))
        self._post((id(s), v), reads, writes)

    def dma_fn(self, eng, emit_fn, reads=(), writes=(), sb=None, into=True):
        waits = self._deps(eng, reads, writes)
        if into:
            if sb.sem_in is None:
                sb.sem_in = self.new_sem("di_" + sb.name)
            s = sb.sem_in
        else:
            if sb.sem_out is None:
                sb.sem_out = self.new_sem("do_" + sb.name)
            s = sb.sem_out
        self.semcnt[id(s)] += 16
        v = self.semcnt[id(s)]
        self.q[eng].append((waits, ("__fn__", emit_fn), s, 16, self.cur_phase))
        self._post((id(s), v), reads, writes)

    def wait_all(self, eng, tiles):
        waits = self._deps(eng, tiles, ())
        self.q[eng].append((waits, None, None, 0, self.cur_phase))

    def emit(self):
        nc = self.nc
        with nc.Block() as block:
            def mk(e):
                def body(engine):
                    cur = None
                    cm = None
                    for waits, fn, s, inc, ph in self.q[e]:
                        if self.use_scopes and ph != cur:
                            if cm is not None:
                                cm.__exit__(None, None, None)
                            cm = nc.named_scope(ph)
                            cm.__enter__()
                            cur = ph
                        for (ws, wv) in waits:
                            engine.wait_ge(ws, wv)
                        if fn is not None:
                            if fn[0] == "__fn__":
                                fn[1](engine).then_inc(s, inc)
                                continue
                            name, a, kw = fn
                            try:
                                ins = getattr(engine, name)(*a, **kw)
                            except Exception:
                                def _d(x):
                                    try:
                                        return (x.tensor.name, x.shape, str(x.dtype)) if hasattr(x, "shape") else x
                                    except Exception:
                                        return repr(x)[:80]
                                print("EMIT FAIL", e, name, [_d(x) for x in a], {k: _d(v) for k, v in kw.items()}, flush=True)
                                raise
                            ins.then_inc(s, inc)
                    if cm is not None:
                        cm.__exit__(None, None, None)
                return body
            block.sync(mk("sync"))
            block.tensor(mk("tensor"))
            block.vector(mk("vector"))
            block.scalar(mk("scalar"))
            block.gpsimd(mk("gpsimd"))

    def close(self):
        for cm in reversed(self._ctx):
            cm.__exit__(None, None, None)
        for cm in reversed(self._semctx):
            cm.__exit__(None, None, None)
        self._ctx = []


import math
import numpy as np

D = 2048
S = 4096
LC = 256
NT = S + LC
INC = 11776
EPS = 1e-6


class Pool:
    def __init__(self, P, name, shape, dt, n, psum=False):
        self.tiles = [(P.ps if psum else P.sb)(f"{name}{i}", shape, dt) for i in range(n)]
        self.i = 0

    def get(self):
        t = self.tiles[self.i % len(self.tiles)]
        self.i += 1
        return t


def dap(tt, offset, pattern):
    return bass.AP(tensor=tt.t, offset=offset, ap=[list(p) for p in pattern])


class LayerBuilder:
    def __init__(self, layer, taps=(), own=2048, shared=None, sfx="", xa=None, xc=None, ext_out=True):
        self.l = layer
        self.ctx_full = (layer == 0)
        self.taps = set(taps)
        if shared is None:
            self.nc = bass.Bass("TRN2", target_bir_lowering=False)
            self.P = Prog(self.nc)
        else:
            self.nc, self.P = shared
        self.sfx = sfx
        self.xa_over, self.xc_over = xa, xc
        self.ext_out = ext_out
        self.OWN = own
        self.NQ = own + LC
        self.NGL = min(own + 512, S)
        self.NG = self.NGL + LC
        self.NTL = own // 128
        self.inputs = {}
        self.outputs = {}

    def inp(self, name, shape, dt=F32):
        t = self.P.dram(name + self.sfx, shape, dt, kind="ExternalInput")
        self.inputs[name] = t
        return t

    def scratch(self, name, shape, dt, out=False):
        kind = "ExternalOutput" if (out or name in self.taps) else "Internal"
        t = self.P.dram(name + self.sfx, shape, dt, kind=kind)
        if kind == "ExternalOutput":
            self.outputs[name] = t
        return t

    def load(self, dst, dst_ap, src, src_ap, eng="sync", extra_reads=()):
        self.P.dma(eng, lambda en: en.dma_start(out=dst_ap, in_=src_ap), reads=[src, *extra_reads], writes=[dst], sb=dst)

    def store(self, dst, dst_ap, src, src_ap, eng="gpsimd"):
        self.P.dma(eng, lambda en: en.dma_start(out=dst_ap, in_=src_ap), reads=[src], writes=[dst], sb=src, into=False)

    def phase_begin(self):
        import inspect
        self.P.cur_phase = inspect.stack()[1].function + self.sfx
        self._mark = len(self.P._ctx)

    def phase_end(self):
        P = self.P
        P.barrier()
        P.free_to(self._mark)

    def declare(self):
        inp = self.inp
        OWN, NQ, NGL, NG = self.OWN, self.NQ, self.NGL, self.NG
        self.xa = self.xa_over if self.xa_over is not None else inp("xa", [S, D])
        self.xc = self.xc_over if self.xc_over is not None else inp("xc", [LC, D])
        self.ct = inp("ct", [128, 32])
        self.w_mod = inp("w_mod", [D, 6 * D])
        self.b_mod = inp("b_mod", [1, 6 * D])
        self.norm1_g = inp("norm1_g", [1, D])
        self.norm2_g = inp("norm2_g", [1, D])
        self.w_in = inp("w_in", [D, INC])
        self.cos_t = inp("cos_t", [128, S])
        self.sin_t = inp("sin_t", [128, S])
        self.rm = inp("rm", [128, 128])
        self.ident_f = inp("ident_f", [128, 128])
        self.ones_f = inp("ones_f", [128, 128])
        sc = self.scratch
        self.MOD = sc("MOD", [2, 6 * D], F32)
        self.HT = sc("HT", [16, 128, NT], BF16)
        self.QT = sc("QT", [8, 128, NQ], BF16)
        self.KT = sc("KT", [8, 128, NT], BF16)
        self.V = sc("V", [NT, 1024], BF16)
        self.SG = sc("SG", [4, 128, NG], F32)
        self.YG = sc("YG", [4, 128, NG], F32)
        self.GT = sc("GT", [48, 128, NQ], BF16)
        self.U = sc("U", [NT, 1536], F32)
        for nm in ("lam_q1", "lam_k1", "lam_q2", "lam_k2"):
            inp(nm, [1, 64])
        inp("attn_subln_g", [1, 128])
        inp("conf_w", [128, 4 * 31])
        inp("conf_v", [128, 12])
        self.ONT = sc("ONT", [8, 128, NQ], BF16)
        inp("hy_scw", [1, 3 * 1536]); inp("hy_scb", [1, 1536])
        inp("hy_v", [64, 4]); inp("hy_w1", [33, 64]); inp("hy_w2", [64, 64]); inp("hy_w3", [64, 2048])
        inp("hy_dec", [128, 16]); inp("hy_bias", [1, 1024]); inp("jrev", [128, 128])
        inp("emb_lat", [33, 2 * S - 1]); inp("tv_lat", [1, 2 * S - 1])
        inp("emb_ctx", [33, 2 * LC - 1]); inp("tv_ctx", [1, 2 * LC - 1])
        self.HU = sc("HU", [NT, 1536], F32)
        self.H2L = sc("H2L", [64, 2 * S - 1], F32)
        self.H2C = sc("H2C", [64, 2 * LC - 1], F32)
        self.EKL = sc("EKL", [1024, 2 * S], BF16)
        self.EKC = sc("EKC", [1024, 2 * LC], BF16)
        self.ZT = sc("ZT", [4, 128, NQ], BF16)
        inp("w_attn_o", [1024, D]); inp("w_conf_o", [512, D]); inp("w_hy_o", [512, D]); inp("w_out", [D, D])
        self.MG = sc("MG", [16, 128, NQ], BF16)
        self.X1 = sc("X1", [NQ, D], F32)
        if "MOE" in self.taps:
            self.MOE = sc("MOE", [NQ, D], F32)
        self.declare_moe()
        self.CT = sc("CT", [4, 128, NQ], BF16)

    def phase_mod(self):
        P = self.P
        self.phase_begin()
        ct = P.sb("ct_s", [128, 32], F32)
        st = P.sb("st_s", [128, 32], F32)
        self.load(ct, ct[:], self.ct, self.ct.t.ap())
        P.op("scalar", lambda en: en.activation(out=st[:], in_=ct[:], func=AF.Silu), reads=[ct], writes=[st])
        wpool = Pool(P, "wm", [128, 16, 512], F32, 2)
        bpool = Pool(P, "bm", [2, 512], F32, 2)
        opool = Pool(P, "om", [2, 512], F32, 2)
        pp = Pool(P, "pm", [2, 512], F32, 2, psum=True)
        wv = self.w_mod.t.ap().rearrange("(kc p) n -> p kc n", p=128)
        for gidx in range(24):
            w = wpool.get()
            self.load(w, w[:], self.w_mod, wv[:, :, gidx * 512:(gidx + 1) * 512])
            bt = bpool.get()
            self.load(bt, bt[:], self.b_mod, dap(self.b_mod, gidx * 512, [[0, 2], [1, 512]]))
            ps = pp.get()
            for kc in range(16):
                P.op("tensor", lambda en, kc=kc, w=w, ps=ps: en.matmul(ps[:], lhsT=st[:, 2 * kc:2 * kc + 2], rhs=w[:, kc, :],
                                                                      start=(kc == 0), stop=(kc == 15)),
                     reads=[st, w], writes=[ps])
            o = opool.get()
            P.op("vector", lambda en, o=o, ps=ps, bt=bt: en.tensor_tensor(out=o[:], in0=ps[:], in1=bt[:], op=ALU.add),
                 reads=[ps, bt], writes=[o])
            self.store(self.MOD, self.MOD.t.ap()[:, gidx * 512:(gidx + 1) * 512], o, o[:])
        self.phase_end()

    def bcast_mod(self, dst, row, j):
        self.load(dst, dst[:], self.MOD, dap(self.MOD, row * 6 * D + j * D, [[0, 128], [1, D]]))

    def make_AB(self, gain, jsh, jsc, tag):
        P = self.P
        gt = P.sb("gain_" + tag, [128, D], F32)
        self.load(gt, gt[:], gain, dap(gain, 0, [[0, 128], [1, D]]))
        res = {}
        for row in ((0, 1) if True else (0,)):
            A = P.sb(f"A_{tag}{row}", [128, D], F32)
            Bt = P.sb(f"B_{tag}{row}", [128, D], F32)
            self.bcast_mod(A, row, jsc)
            self.bcast_mod(Bt, row, jsh)
            P.op("vector", lambda en, A=A: en.scalar_tensor_tensor(out=A[:], in0=A[:], scalar=1.0, in1=gt[:], op0=ALU.add, op1=ALU.mult),
                 reads=[A, gt], writes=[A])
            res[row] = (A, Bt)
        return res

    def norm_tile(self, xt, A, Bt, out, sq, ss, rstd, tmp):
        P = self.P
        P.op("scalar", lambda en: en.activation(out=sq[:], in_=xt[:], func=AF.Square, accum_out=ss[:]), reads=[xt], writes=[sq, ss])
        P.op("vector", lambda en: en.tensor_scalar(out=rstd[:], in0=ss[:], scalar1=1.0 / D, scalar2=EPS, op0=ALU.mult, op1=ALU.add),
             reads=[ss], writes=[rstd])
        P.op("scalar", lambda en: en.activation(out=rstd[:], in_=rstd[:], func=AF.Sqrt), reads=[rstd], writes=[rstd])
        P.op("vector", lambda en: en.reciprocal(out=rstd[:], in_=rstd[:]), reads=[rstd], writes=[rstd])
        P.op("vector", lambda en: en.scalar_tensor_tensor(out=tmp[:], in0=xt[:], scalar=rstd[:, 0:1], in1=A[:], op0=ALU.mult, op1=ALU.mult),
             reads=[xt, rstd, A], writes=[tmp])
        P.op("vector", lambda en: en.tensor_tensor(out=out[:], in0=tmp[:], in1=Bt[:], op=ALU.add), reads=[tmp, Bt], writes=[out])

    def phase_norm1(self):
        P = self.P
        self.phase_begin()
        AB = self.make_AB(self.norm1_g, 0, 1, "n1")
        ident = P.sb("ident_b", [128, 128], BF16)
        idf = P.sb("ident_fs", [128, 128], F32)
        self.load(idf, idf[:], self.ident_f, self.ident_f.t.ap())
        P.op("vector", lambda en: en.tensor_copy(out=ident[:], in_=idf[:]), reads=[idf], writes=[ident])
        xpool = Pool(P, "xt", [128, D], F32, 2)
        sqp = Pool(P, "sq", [128, D], F32, 1)
        tmpp = Pool(P, "tmp", [128, D], F32, 1)
        hbp = Pool(P, "hb", [128, D], BF16, 2)
        ssp = Pool(P, "ss", [128, 1], F32, 2)
        rsp = Pool(P, "rs", [128, 1], F32, 2)
        htp = Pool(P, "hts", [128, 16, 512], BF16, 2)
        ptp = Pool(P, "ptr", [128, 8, 128], BF16, 2, psum=True)
        htv = self.HT.t.ap().rearrange("kc p t -> p kc t")
        groups = [(g * 512, 512, self.xa, 0) for g in range(8)] + [(S, 256, self.xc, 1)]
        for (t0, n, src, row) in groups:
            hts = htp.get()
            A, Bt = AB[row]
            for i in range(n // 128):
                r0 = (t0 if row == 0 else 0) + i * 128
                xt = xpool.get()
                self.load(xt, xt[:], src, src.t.ap()[r0:r0 + 128, :])
                hb = hbp.get()
                self.norm_tile(xt, A, Bt, hb, sqp.get(), ssp.get(), rsp.get(), tmpp.get())
                for half in range(2):
                    pt = ptp.get()
                    for k8 in range(8):
                        kc = half * 8 + k8
                        P.op("tensor", lambda en, pt=pt, k8=k8, kc=kc, hb=hb: en.transpose(pt[:, k8, :], hb[:, kc * 128:(kc + 1) * 128], ident[:]),
                             reads=[hb, ident], writes=[pt])
                    eng = "scalar" if half == 0 else "vector"
                    if eng == "scalar":
                        P.op("scalar", lambda en, pt=pt, hts=hts, half=half, i=i: en.copy(out=hts[:, half * 8:half * 8 + 8, i * 128:(i + 1) * 128], in_=pt[:]),
                             reads=[pt], writes=[hts])
                    else:
                        P.op("vector", lambda en, pt=pt, hts=hts, half=half, i=i: en.tensor_copy(out=hts[:, half * 8:half * 8 + 8, i * 128:(i + 1) * 128], in_=pt[:]),
                             reads=[pt], writes=[hts])
            self.store(self.HT, htv[:, :, t0:t0 + n], hts, hts[:, :, 0:n])
        self.phase_end()

    def phase_inproj(self):
        OWN, NQ, NGL, NG, NTL = self.OWN, self.NQ, self.NGL, self.NG, self.NTL
        P = self.P
        full = self.ctx_full
        self.phase_begin()
        cos = P.sb("cos_s", [128, S], F32)
        sin = P.sb("sin_s", [128, S], F32)
        rm = P.sb("rm_s", [128, 128], F32)
        self.load(cos, cos[:], self.cos_t, self.cos_t.t.ap())
        self.load(sin, sin[:], self.sin_t, self.sin_t.t.ap())
        self.load(rm, rm[:], self.rm, self.rm.t.ap())
        wst = Pool(P, "wst", [128, 16, 512], F32, 2)
        wbp = Pool(P, "wb", [128, 16, 512], BF16, 2)
        htp = Pool(P, "hti", [128, 16, 512], BF16, 2)
        psp = Pool(P, "pin", [128, 512], F32, 4, psum=True)
        prp = Pool(P, "prr", [128, 512], F32, 2, psum=True)
        qfp = Pool(P, "qf", [128, 512], F32, 2)
        t1p = Pool(P, "t1", [128, 512], F32, 2)
        t2p = Pool(P, "t2", [128, 512], F32, 2)
        obp = Pool(P, "ob", [128, 512], BF16, 3)
        ofp = Pool(P, "of", [128, 512], F32, 3)
        sgp = Pool(P, "sgl", [128, 512], F32, 2)
        wv = self.w_in.t.ap().rearrange("(kc p) n -> p kc n", p=128)
        htv = self.HT.t.ap().rearrange("kc p t -> p kc t")
        lat_all = [(g * 512, 512) for g in range(8)]
        lat_own = [(g * 512, 512) for g in range(OWN // 512)]
        lat_glu = [(g * 512, 512) for g in range(NGL // 512)]
        ctxg = [(S, 256)]
        plan = [("q", 0), ("q", 1), ("k", 2), ("k", 3), ("v", 4), ("v", 5), ("glug", 7), ("glua", 6),
                ("hy", 8), ("hy", 9), ("hy", 10)] + [("gate", 11 + i) for i in range(12)]
        cnt = 0
        for kind, cg in plan:
            ws = wst.get()
            self.load(ws, ws[:], self.w_in, wv[:, :, cg * 512:(cg + 1) * 512])
            wb = wbp.get()
            P.op("gpsimd", lambda en, wb=wb, ws=ws: en.tensor_copy(out=wb[:, 0:8, :], in_=ws[:, 0:8, :]), reads=[ws], writes=[wb])
            P.op("gpsimd", lambda en, wb=wb, ws=ws: en.tensor_copy(out=wb[:, 8:16, :], in_=ws[:, 8:16, :]), reads=[ws, wb], writes=[wb])
            if kind == "q":
                groups = lat_own + (ctxg if full else [])
            elif kind in ("k", "v"):
                groups = lat_all + ctxg
            elif kind in ("glug", "glua"):
                groups = lat_glu + (ctxg if full else [])
            elif kind == "hy":
                groups = lat_all + (ctxg if full else [])
            else:
                groups = lat_own + (ctxg if full else [])
            for (t0, n) in groups:
                isctx = t0 >= S
                ht = htp.get()
                self.load(ht, ht[:, :, 0:n], self.HT, htv[:, :, t0:t0 + n])
                if kind in ("v", "hy"):
                    for i in range(n // 128):
                        ps = psp.get()
                        for kc in range(16):
                            P.op("tensor", lambda en, ps=ps, ht=ht, wb=wb, kc=kc, i=i: en.matmul(ps[:], lhsT=ht[:, kc, i * 128:(i + 1) * 128], rhs=wb[:, kc, :],
                                                                                                 start=(kc == 0), stop=(kc == 15)),
                                 reads=[ht, wb], writes=[ps])
                        r0 = t0 + i * 128
                        cnt += 1
                        eng = "scalar" if cnt % 2 else "vector"
                        if kind == "v":
                            o = obp.get()
                            dst, dst_ap = self.V, self.V.t.ap()[r0:r0 + 128, (cg - 4) * 512:(cg - 3) * 512]
                        else:
                            o = ofp.get()
                            dst, dst_ap = self.U, self.U.t.ap()[r0:r0 + 128, (cg - 8) * 512:(cg - 7) * 512]
                        if eng == "scalar":
                            P.op("scalar", lambda en, o=o, ps=ps: en.copy(out=o[:], in_=ps[:]), reads=[ps], writes=[o])
                        else:
                            P.op("vector", lambda en, o=o, ps=ps: en.tensor_copy(out=o[:], in_=ps[:]), reads=[ps], writes=[o])
                        self.store(dst, dst_ap, o, o[:])
                    continue
                for j in range(4):
                    ps = psp.get()
                    for kc in range(16):
                        P.op("tensor", lambda en, ps=ps, ht=ht, wb=wb, kc=kc, j=j, n=n: en.matmul(ps[:, 0:n], lhsT=wb[:, kc, j * 128:(j + 1) * 128], rhs=ht[:, kc, 0:n],
                                                                                                 start=(kc == 0), stop=(kc == 15)),
                             reads=[ht, wb], writes=[ps])
                    if kind in ("q", "k"):
                        head = (cg % 2) * 4 + j
                        o = obp.get()
                        if isctx:
                            P.op("scalar", lambda en, o=o, ps=ps, n=n: en.copy(out=o[:, 0:n], in_=ps[:, 0:n]), reads=[ps], writes=[o])
                        else:
                            qf = qfp.get()
                            P.op("scalar", lambda en, qf=qf, ps=ps: en.copy(out=qf[:], in_=ps[:]), reads=[ps], writes=[qf])
                            pr = prp.get()
                            P.op("tensor", lambda en, pr=pr, qf=qf: en.matmul(pr[:], lhsT=rm[:], rhs=qf[:], start=True, stop=True),
                                 reads=[rm, qf], writes=[pr])
                            t1 = t1p.get()
                            t2 = t2p.get()
                            P.op("vector", lambda en, t1=t1, qf=qf, t0=t0: en.tensor_tensor(out=t1[:], in0=qf[:], in1=cos[:, t0:t0 + 512], op=ALU.mult),
                                 reads=[qf, cos], writes=[t1])
                            P.op("vector", lambda en, t2=t2, pr=pr, t0=t0: en.tensor_tensor(out=t2[:], in0=pr[:], in1=sin[:, t0:t0 + 512], op=ALU.mult),
                                 reads=[pr, sin], writes=[t2])
                            P.op("vector", lambda en, o=o, t1=t1, t2=t2: en.tensor_tensor(out=o[:], in0=t1[:], in1=t2[:], op=ALU.add),
                                 reads=[t1, t2], writes=[o])
                        if kind == "q":
                            c0 = (OWN + t0 - S) if isctx else t0
                            self.store(self.QT, self.QT.t.ap()[head, :, c0:c0 + n], o, o[:, 0:n])
                        else:
                            self.store(self.KT, self.KT.t.ap()[head, :, t0:t0 + n], o, o[:, 0:n])
                    elif kind == "glug":
                        o = ofp.get()
                        P.op("scalar", lambda en, o=o, ps=ps, n=n: en.activation(out=o[:, 0:n], in_=ps[:, 0:n], func=AF.Sigmoid), reads=[ps], writes=[o])
                        c0 = (NGL + t0 - S) if isctx else t0
                        self.store(self.SG, self.SG.t.ap()[j, :, c0:c0 + n], o, o[:, 0:n])
                    elif kind == "glua":
                        c0 = (NGL + t0 - S) if isctx else t0
                        sg = sgp.get()
                        self.load(sg, sg[:, 0:n], self.SG, self.SG.t.ap()[j, :, c0:c0 + n])
                        o = ofp.get()
                        P.op("vector", lambda en, o=o, ps=ps, sg=sg, n=n: en.tensor_tensor(out=o[:, 0:n], in0=ps[:, 0:n], in1=sg[:, 0:n], op=ALU.mult),
                             reads=[ps, sg], writes=[o])
                        self.store(self.YG, self.YG.t.ap()[j, :, c0:c0 + n], o, o[:, 0:n])
                    else:
                        o = obp.get()
                        P.op("scalar", lambda en, o=o, ps=ps, n=n: en.activation(out=o[:, 0:n], in_=ps[:, 0:n], func=AF.Sigmoid), reads=[ps], writes=[o])
                        c0 = (OWN + t0 - S) if isctx else t0
                        ch = (cg - 11) * 4 + j
                        self.store(self.GT, self.GT.t.ap()[ch, :, c0:c0 + n], o, o[:, 0:n])
        self.phase_end()


def phase_attn(self):
    OWN, NQ, NGL, NG, NTL = self.OWN, self.NQ, self.NGL, self.NG, self.NTL
    P = self.P
    full = self.ctx_full
    lam_init = 0.8 - 0.6 * math.exp(-0.3 * self.l)
    self.phase_begin()
    lt = {}
    for nm in ("lam_q1", "lam_k1", "lam_q2", "lam_k2"):
        t = P.sb(nm + "_s", [128, 64], F32)
        src = self.inputs[nm]
        self.load(t, t[:], src, dap(src, 0, [[0, 128], [1, 64]]))
        lt[nm] = t
    pr1 = P.sb("lpr1", [128, 64], F32)
    pr2 = P.sb("lpr2", [128, 64], F32)
    e1 = P.sb("le1", [128, 1], F32)
    e2 = P.sb("le2", [128, 1], F32)
    nlam = P.sb("nlam", [128, 1], F32)
    P.op("vector", lambda en: en.tensor_tensor(out=pr1[:], in0=lt["lam_q1"][:], in1=lt["lam_k1"][:], op=ALU.mult), reads=[lt["lam_q1"], lt["lam_k1"]], writes=[pr1])
    P.op("vector", lambda en: en.tensor_tensor(out=pr2[:], in0=lt["lam_q2"][:], in1=lt["lam_k2"][:], op=ALU.mult), reads=[lt["lam_q2"], lt["lam_k2"]], writes=[pr2])
    P.op("vector", lambda en: en.reduce_sum(out=e1[:], in_=pr1[:], axis=AX.X), reads=[pr1], writes=[e1])
    P.op("vector", lambda en: en.reduce_sum(out=e2[:], in_=pr2[:], axis=AX.X), reads=[pr2], writes=[e2])
    P.op("scalar", lambda en: en.activation(out=e1[:], in_=e1[:], func=AF.Exp), reads=[e1], writes=[e1])
    P.op("scalar", lambda en: en.activation(out=e2[:], in_=e2[:], func=AF.Exp), reads=[e2], writes=[e2])
    P.op("vector", lambda en: en.scalar_tensor_tensor(out=nlam[:], in0=e2[:], scalar=-lam_init, in1=e1[:], op0=ALU.add, op1=ALU.subtract),
         reads=[e1, e2], writes=[nlam])
    gsub = P.sb("gsub", [128, 1], F32)
    sg_in = self.inputs["attn_subln_g"]
    self.load(gsub, gsub[:], sg_in, dap(sg_in, 0, [[1, 128], [1, 1]]))
    P.op("vector", lambda en: en.tensor_scalar(out=gsub[:], in0=gsub[:], scalar1=(1.0 - lam_init), scalar2=None, op0=ALU.mult), reads=[gsub], writes=[gsub])
    ones_f = P.sb("ones_fs", [128, 128], F32)
    ones_b = P.sb("ones_bs", [128, 128], BF16)
    self.load(ones_f, ones_f[:], self.ones_f, self.ones_f.t.ap())
    P.op("vector", lambda en: en.tensor_copy(out=ones_b[:], in_=ones_f[:]), reads=[ones_f], writes=[ones_b])

    qp = Pool(P, "q_sb", [128, NQ], BF16, 2)
    kp = Pool(P, "k_sb", [128, NT], BF16, 2)
    vp = Pool(P, "v_sb", [128, 34, 128], BF16, 2)
    sp = Pool(P, "s_ps", [128, 512], F32, 2, psum=True)
    O = [P.ps("o_ps%d" % c, [128, 512], F32) for c in range(2)]
    Z = [P.ps("z_ps%d" % c, [128, 512], F32) for c in range(2)]
    ssps = P.ps("ss_ps", [128, 512], F32)
    ep = Pool(P, "e_sb", [128, 512], BF16, 3)
    r1p = Pool(P, "r1", [128, 512], F32, 1)
    t1p = Pool(P, "at1", [128, 512], F32, 1)
    t2p = Pool(P, "at2", [128, 512], F32, 1)
    op_ = Pool(P, "ao", [128, 512], F32, 2)
    sqp = Pool(P, "asq", [128, 512], F32, 1)
    rsp = Pool(P, "ars", [128, 512], F32, 1)
    onp = Pool(P, "aon", [128, 512], BF16, 2)
    vview = self.V.t.ap().rearrange("(kt p) c -> p kt c", p=128)
    for h in range(8):
        q = qp.get(); k = kp.get(); v = vp.get()
        nq = NQ if full else OWN
        self.load(q, q[:, 0:nq], self.QT, self.QT.t.ap()[h, :, 0:nq])
        self.load(k, k[:], self.KT, self.KT.t.ap()[h])
        self.load(v, v[:], self.V, vview[:, :, h * 128:(h + 1) * 128])
        chunks = [(qc * 512, 512, list(range(34))) for qc in range(OWN // 512)]
        if full:
            chunks.append((OWN, 256, [32, 33]))
        for (q0, n, kts) in chunks:
            steps = [(c, kt) for c in range(2) for kt in kts]

            def emit_s(st):
                c, kt = st
                s = sp.get()
                P.op("tensor", lambda en, s=s, c=c, kt=kt: en.matmul(s[:, 0:n], lhsT=k[64 * c:64 * c + 64, kt * 128:(kt + 1) * 128],
                                                                     rhs=q[64 * c:64 * c + 64, q0:q0 + n], start=True, stop=True),
                     reads=[k, q], writes=[s])
                return s
            s_cur = emit_s(steps[0])
            for i, (c, kt) in enumerate(steps):
                s_next = emit_s(steps[i + 1]) if i + 1 < len(steps) else None
                e = ep.get()
                P.op("scalar", lambda en, e=e, s=s_cur: en.activation(out=e[:, 0:n], in_=s[:, 0:n], func=AF.Exp, scale=0.125), reads=[s_cur], writes=[e])
                first = (kt == kts[0]); last = (kt == kts[-1])
                P.op("tensor", lambda en, e=e, c=c, kt=kt, first=first, last=last: en.matmul(O[c][:, 0:n], lhsT=v[:, kt, :], rhs=e[:, 0:n], start=first, stop=last),
                     reads=[v, e], writes=[O[c]])
                P.op("tensor", lambda en, e=e, c=c, first=first, last=last: en.matmul(Z[c][:, 0:n], lhsT=ones_b[:], rhs=e[:, 0:n], start=first, stop=last),
                     reads=[ones_b, e], writes=[Z[c]])
                s_cur = s_next
            r1 = r1p.get(); t1 = t1p.get(); t2 = t2p.get(); o = op_.get(); sq = sqp.get(); rs = rsp.get(); on = onp.get()
            P.op("vector", lambda en, r1=r1: en.reciprocal(out=r1[:, 0:n], in_=Z[0][:, 0:n]), reads=[Z[0]], writes=[r1])
            P.op("vector", lambda en, r1=r1, t1=t1: en.tensor_tensor(out=t1[:, 0:n], in0=O[0][:, 0:n], in1=r1[:, 0:n], op=ALU.mult), reads=[O[0], r1], writes=[t1])
            P.op("vector", lambda en, r1=r1: en.reciprocal(out=r1[:, 0:n], in_=Z[1][:, 0:n]), reads=[Z[1], r1], writes=[r1])
            P.op("vector", lambda en, r1=r1, t2=t2: en.tensor_tensor(out=t2[:, 0:n], in0=O[1][:, 0:n], in1=r1[:, 0:n], op=ALU.mult), reads=[O[1], r1], writes=[t2])
            P.op("vector", lambda en, o=o, t1=t1, t2=t2: en.scalar_tensor_tensor(out=o[:, 0:n], in0=t2[:, 0:n], scalar=nlam[:, 0:1], in1=t1[:, 0:n], op0=ALU.mult, op1=ALU.add),
                 reads=[t1, t2, nlam], writes=[o])
            P.op("scalar", lambda en, o=o, sq=sq: en.activation(out=sq[:, 0:n], in_=o[:, 0:n], func=AF.Square), reads=[o], writes=[sq])
            P.op("tensor", lambda en, sq=sq: en.matmul(ssps[:, 0:n], lhsT=ones_f[:], rhs=sq[:, 0:n], start=True, stop=True), reads=[ones_f, sq], writes=[ssps])
            P.op("vector", lambda en, rs=rs: en.tensor_scalar(out=rs[:, 0:n], in0=ssps[:, 0:n], scalar1=1.0 / 128, scalar2=1e-5, op0=ALU.mult, op1=ALU.add), reads=[ssps], writes=[rs])
            P.op("scalar", lambda en, rs=rs: en.activation(out=rs[:, 0:n], in_=rs[:, 0:n], func=AF.Sqrt), reads=[rs], writes=[rs])
            P.op("vector", lambda en, rs=rs: en.reciprocal(out=rs[:, 0:n], in_=rs[:, 0:n]), reads=[rs], writes=[rs])
            P.op("vector", lambda en, on=on, o=o, rs=rs: en.scalar_tensor_tensor(out=on[:, 0:n], in0=o[:, 0:n], scalar=gsub[:, 0:1], in1=rs[:, 0:n], op0=ALU.mult, op1=ALU.mult),
                 reads=[o, rs, gsub], writes=[on])
            self.store(self.ONT, self.ONT.t.ap()[h, :, q0:q0 + n], on, on[:, 0:n])
    self.phase_end()


LayerBuilder.phase_attn = phase_attn


def phase_conf(self):
    OWN, NQ, NGL, NG, NTL = self.OWN, self.NQ, self.NGL, self.NG, self.NTL
    P = self.P
    full = self.ctx_full
    self.phase_begin()
    cw = P.sb("cw", [128, 4, 31], F32)
    cv = P.sb("cv", [128, 3, 4], F32)
    self.load(cw, cw[:], self.inputs["conf_w"], self.inputs["conf_w"].t.ap().rearrange("p (c j) -> p c j", c=4))
    self.load(cv, cv[:], self.inputs["conf_v"], self.inputs["conf_v"].t.ap().rearrange("p (w c) -> p w c", w=3))
    ones_f = P.sb("ones_fc", [128, 128], F32)
    self.load(ones_f, ones_f[:], self.ones_f, self.ones_f.t.ap())
    seqs = [(0, min(OWN + 15, S), OWN, 0)]
    if full:
        seqs.append((NGL, 256, 256, OWN))
    yp = Pool(P, "cy", [128, 15 + OWN + 15 + 15], F32, 2)
    acc = P.sb("cacc", [128, 4, OWN], F32)
    sq = P.sb("csq", [128, 4, 512], F32)
    ps1 = P.ps("cps1", [128, 512], F32)
    ps2 = P.ps("cps2", [128, 512], F32)
    mean = P.sb("cmean", [128, 512], F32)
    msq = P.sb("cmsq", [128, 512], F32)
    var = P.sb("cvar", [128, 512], F32)
    dp = Pool(P, "cd", [128, 512], F32, 2)
    obp = Pool(P, "cob", [128, 512], BF16, 2)
    for (c0, n_in, n_out, o0) in seqs:
        for cc in range(4):
            y = yp.get()
            eng = "vector"
            P.op("gpsimd", lambda en, y=y: en.memset(y[:], 0.0), writes=[y])
            self.load(y, y[:, 15:15 + n_in], self.YG, self.YG.t.ap()[cc, :, c0:c0 + n_in])
            P.op(eng, lambda en, y=y, cc=cc: en.tensor_scalar(out=acc[:, cc, 0:n_out], in0=y[:, 0:n_out], scalar1=cw[:, cc, 0:1], scalar2=cv[:, 0, cc:cc + 1],
                                                             op0=ALU.mult, op1=ALU.add), reads=[y, cw, cv], writes=[acc])
            for j in range(1, 31):
                P.op(eng, lambda en, y=y, cc=cc, j=j: en.scalar_tensor_tensor(out=acc[:, cc, 0:n_out], in0=y[:, j:j + n_out], scalar=cw[:, cc, j:j + 1],
                                                                             in1=acc[:, cc, 0:n_out], op0=ALU.mult, op1=ALU.add),
                     reads=[y, cw, acc], writes=[acc])
        for tg in range((n_out + 511) // 512):
            n = min(512, n_out - tg * 512)
            sl = slice(tg * 512, tg * 512 + n)
            for cc in range(4):
                P.op("tensor", lambda en, cc=cc: en.matmul(ps1[:, 0:n], lhsT=ones_f[:], rhs=acc[:, cc, sl], start=(cc == 0), stop=(cc == 3)),
                     reads=[ones_f, acc], writes=[ps1])
            P.op("scalar", lambda en: en.activation(out=sq[:, :, 0:n], in_=acc[:, :, sl], func=AF.Square), reads=[acc], writes=[sq])
            for cc in range(4):
                P.op("tensor", lambda en, cc=cc: en.matmul(ps2[:, 0:n], lhsT=ones_f[:], rhs=sq[:, cc, 0:n], start=(cc == 0), stop=(cc == 3)),
                     reads=[ones_f, sq], writes=[ps2])
            P.op("scalar", lambda en: en.mul(out=mean[:, 0:n], in_=ps1[:, 0:n], mul=1.0 / 512), reads=[ps1], writes=[mean])
            P.op("vector", lambda en: en.tensor_tensor(out=msq[:, 0:n], in0=mean[:, 0:n], in1=mean[:, 0:n], op=ALU.mult), reads=[mean], writes=[msq])
            P.op("vector", lambda en: en.scalar_tensor_tensor(out=var[:, 0:n], in0=ps2[:, 0:n], scalar=1.0 / 512, in1=msq[:, 0:n], op0=ALU.mult, op1=ALU.subtract),
                 reads=[ps2, msq], writes=[var])
            P.op("vector", lambda en: en.tensor_scalar(out=var[:, 0:n], in0=var[:, 0:n], scalar1=1e-5, scalar2=None, op0=ALU.add), reads=[var], writes=[var])
            P.op("scalar", lambda en: en.activation(out=var[:, 0:n], in_=var[:, 0:n], func=AF.Sqrt), reads=[var], writes=[var])
            P.op("vector", lambda en: en.reciprocal(out=var[:, 0:n], in_=var[:, 0:n]), reads=[var], writes=[var])
            for cc in range(4):
                d = dp.get(); ob = obp.get()
                P.op("vector", lambda en, d=d, cc=cc: en.tensor_tensor(out=d[:, 0:n], in0=acc[:, cc, sl], in1=mean[:, 0:n], op=ALU.subtract), reads=[acc, mean], writes=[d])
                P.op("vector", lambda en, d=d: en.tensor_tensor(out=d[:, 0:n], in0=d[:, 0:n], in1=var[:, 0:n], op=ALU.mult), reads=[d, var], writes=[d])
                P.op("scalar", lambda en, d=d, ob=ob, cc=cc: en.activation(out=ob[:, 0:n], in_=d[:, 0:n], func=AF.Silu, scale=cv[:, 1, cc:cc + 1], bias=cv[:, 2, cc:cc + 1]),
                     reads=[d, cv], writes=[ob])
                self.store(self.CT, self.CT.t.ap()[cc, :, o0 + tg * 512:o0 + tg * 512 + n], ob, ob[:, 0:n])
    self.phase_end()


LayerBuilder.phase_conf = phase_conf


TWO_PI = 2.0 * math.pi


def phase_hy_short(self):
    P = self.P
    full = self.ctx_full
    self.phase_begin()
    W = []
    scw = self.inputs["hy_scw"]
    for j in range(3):
        t = P.sb("hsw%d" % j, [128, 1536], F32)
        self.load(t, t[:], scw, dap(scw, j * 1536, [[0, 128], [1, 1536]]))
        W.append(t)
    Bc = P.sb("hsb", [128, 1536], F32)
    scb = self.inputs["hy_scb"]
    self.load(Bc, Bc[:], scb, dap(scb, 0, [[0, 128], [1, 1536]]))
    pp = Pool(P, "hsp", [128, 1536], F32, 2)
    cp = Pool(P, "hsc", [128, 1536], F32, 2)
    np_ = Pool(P, "hsn", [128, 1536], F32, 2)
    tp = Pool(P, "hst", [128, 1536], F32, 2)
    t2p = Pool(P, "hst2", [128, 1536], F32, 2)
    seqs = [(0, 32)] + ([(S, 2)] if full else [])
    Uv = self.U.t.ap()
    for (r0s, nt) in seqs:
        for i in range(nt):
            r0 = r0s + i * 128
            pv = pp.get(); cu = cp.get(); nx = np_.get(); t = tp.get(); t2 = t2p.get()
            self.load(cu, cu[:], self.U, Uv[r0:r0 + 128, :])
            if i == 0:
                P.op("gpsimd", lambda en, pv=pv: en.memset(pv[:], 0.0), writes=[pv])
                self.load(pv, pv[1:128, :], self.U, Uv[r0:r0 + 127, :])
            else:
                self.load(pv, pv[:], self.U, Uv[r0 - 1:r0 + 127, :])
            if i == nt - 1:
                P.op("gpsimd", lambda en, nx=nx: en.memset(nx[:], 0.0), writes=[nx])
                self.load(nx, nx[0:127, :], self.U, Uv[r0 + 1:r0 + 128, :])
            else:
                self.load(nx, nx[:], self.U, Uv[r0 + 1:r0 + 129, :])
            P.op("vector", lambda en, t=t, pv=pv: en.tensor_tensor(out=t[:], in0=pv[:], in1=W[0][:], op=ALU.mult), reads=[pv, W[0]], writes=[t])
            P.op("gpsimd", lambda en, t2=t2, cu=cu: en.tensor_tensor(out=t2[:], in0=cu[:], in1=W[1][:], op=ALU.mult), reads=[cu, W[1]], writes=[t2])
            P.op("vector", lambda en, t=t, t2=t2: en.tensor_tensor(out=t[:], in0=t[:], in1=t2[:], op=ALU.add), reads=[t, t2], writes=[t])
            P.op("gpsimd", lambda en, t2=t2, nx=nx: en.tensor_tensor(out=t2[:], in0=nx[:], in1=W[2][:], op=ALU.mult), reads=[nx, W[2], t2], writes=[t2])
            P.op("gpsimd", lambda en, t2=t2: en.tensor_tensor(out=t2[:], in0=t2[:], in1=Bc[:], op=ALU.add), reads=[t2, Bc], writes=[t2])
            P.op("vector", lambda en, t=t, t2=t2: en.tensor_tensor(out=t[:], in0=t[:], in1=t2[:], op=ALU.add), reads=[t, t2], writes=[t])
            self.store(self.HU, self.HU.t.ap()[r0:r0 + 128, :], t, t[:])
    self.phase_end()


def hy_seqs(self):
    OWN, NQ, NGL, NG, NTL = self.OWN, self.NQ, self.NGL, self.NG, self.NTL
    seqs = [dict(L=S, NA=32, NO=OWN // 128, r0=0, o0=0, emb=self.inputs["emb_lat"], tv=self.inputs["tv_lat"], EK=self.EKL, H2D=self.H2L)]
    if self.ctx_full:
        seqs.append(dict(L=LC, NA=2, NO=2, r0=S, o0=OWN, emb=self.inputs["emb_ctx"], tv=self.inputs["tv_ctx"], EK=self.EKC, H2D=self.H2C))
    return seqs


def phase_hy_mlp(self):
    P = self.P
    self.phase_begin()
    hyv = P.sb("hyv", [64, 4], F32)
    self.load(hyv, hyv[:], self.inputs["hy_v"], self.inputs["hy_v"].t.ap())
    w1 = P.sb("hw1", [33, 64], F32)
    w2 = P.sb("hw2", [64, 64], F32)
    self.load(w1, w1[:], self.inputs["hy_w1"], self.inputs["hy_w1"].t.ap())
    self.load(w2, w2[:], self.inputs["hy_w2"], self.inputs["hy_w2"].t.ap())
    cs = P.sb("hcs", [64, 4], F32)
    for k in range(2):
        P.op("vector", lambda en, k=k: en.tensor_scalar(out=cs[:, 2 * k:2 * k + 1], in0=hyv[:, 2 * k + 1:2 * k + 2], scalar1=1.0 / TWO_PI, scalar2=None, op0=ALU.mult),
             reads=[hyv, cs], writes=[cs])
        P.op("vector", lambda en, k=k: en.tensor_tensor(out=cs[:, 2 * k + 1:2 * k + 2], in0=hyv[:, 2 * k:2 * k + 1], in1=cs[:, 2 * k:2 * k + 1], op=ALU.mult),
             reads=[hyv, cs], writes=[cs])
        P.op("vector", lambda en, k=k: en.tensor_scalar(out=cs[:, 2 * k + 1:2 * k + 2], in0=cs[:, 2 * k + 1:2 * k + 2], scalar1=0.0, scalar2=None, op0=ALU.add),
             reads=[cs], writes=[cs])
    negpi = P.sb("negpi", [64, 1], F32)
    P.op("vector", lambda en: en.memset(negpi[:], -math.pi), writes=[negpi])
    ep = Pool(P, "hemb", [33, 512], F32, 2)
    pp = Pool(P, "hmp", [64, 512], F32, 2, psum=True)
    up = Pool(P, "hmu", [64, 512], F32, 2)
    hp = Pool(P, "hmh", [64, 512], F32, 2)
    uip = Pool(P, "hmui", [64, 512], I32, 2)
    ufp = Pool(P, "hmuf", [64, 512], F32, 2)
    for sq in hy_seqs(self):
        npos = 2 * sq["L"] - 1
        for c0 in range(0, npos, 512):
            n = min(512, npos - c0)
            e = ep.get()
            self.load(e, e[:, 0:n], sq["emb"], sq["emb"].t.ap()[:, c0:c0 + n])
            h = None
            for k, w in enumerate((w1, w2)):
                ps = pp.get()
                rhs = e if k == 0 else h
                P.op("tensor", lambda en, ps=ps, w=w, rhs=rhs: en.matmul(ps[:, 0:n], lhsT=w[:], rhs=rhs[:, 0:n], start=True, stop=True), reads=[w, rhs], writes=[ps])
                u = up.get()
                P.op("vector", lambda en, u=u, ps=ps, k=k: en.tensor_scalar(out=u[:, 0:n], in0=ps[:, 0:n], scalar1=cs[:, 2 * k:2 * k + 1], scalar2=cs[:, 2 * k + 1:2 * k + 2],
                                                                            op0=ALU.mult, op1=ALU.add), reads=[ps, cs], writes=[u])
                ui = uip.get(); uf = ufp.get()
                P.op("vector", lambda en, u=u, ui=ui: en.tensor_copy(out=ui[:, 0:n], in_=u[:, 0:n]), reads=[u], writes=[ui])
                P.op("vector", lambda en, uf=uf, ui=ui: en.tensor_copy(out=uf[:, 0:n], in_=ui[:, 0:n]), reads=[ui], writes=[uf])
                P.op("vector", lambda en, u=u, uf=uf: en.tensor_tensor(out=u[:, 0:n], in0=u[:, 0:n], in1=uf[:, 0:n], op=ALU.subtract), reads=[u, uf], writes=[u])
                P.op("vector", lambda en, u=u, uf=uf: en.tensor_scalar(out=uf[:, 0:n], in0=u[:, 0:n], scalar1=0.5, scalar2=None, op0=ALU.is_gt), reads=[u, uf], writes=[uf])
                P.op("vector", lambda en, u=u, uf=uf: en.tensor_tensor(out=u[:, 0:n], in0=u[:, 0:n], in1=uf[:, 0:n], op=ALU.subtract), reads=[u, uf], writes=[u])
                P.op("vector", lambda en, u=u, uf=uf: en.tensor_scalar(out=uf[:, 0:n], in0=u[:, 0:n], scalar1=-0.5, scalar2=None, op0=ALU.is_lt), reads=[u, uf], writes=[uf])
                P.op("vector", lambda en, u=u, uf=uf: en.tensor_tensor(out=u[:, 0:n], in0=u[:, 0:n], in1=uf[:, 0:n], op=ALU.add), reads=[u, uf], writes=[u])
                h = hp.get()
                P.op("scalar", lambda en, u=u, h=h: en.activation(out=h[:, 0:n], in_=u[:, 0:n], func=AF.Sin, scale=TWO_PI), reads=[u], writes=[h])
            self.store(sq["H2D"], sq["H2D"].t.ap()[:, c0:c0 + n], h, h[:, 0:n])
    self.phase_end()


def phase_hy_filt(self):
    P = self.P
    self.phase_begin()
    w3 = P.sb("hw3", [64, 2048], F32)
    self.load(w3, w3[:], self.inputs["hy_w3"], self.inputs["hy_w3"].t.ap())
    dec = P.sb("hdec", [128, 16], F32)
    self.load(dec, dec[:], self.inputs["hy_dec"], self.inputs["hy_dec"].t.ap())
    P.op("scalar", lambda en: en.activation(out=dec[:], in_=dec[:], func=AF.Abs), reads=[dec], writes=[dec])
    P.op("vector", lambda en: en.tensor_scalar(out=dec[:], in0=dec[:], scalar1=-1.0, scalar2=None, op0=ALU.mult), reads=[dec], writes=[dec])
    h2 = P.sb("hh2", [64, 8191], F32)
    tvb = P.sb("htvb", [128, 8191], F32)
    et = P.sb("het", [128, 8191], F32)
    eb = P.sb("heb", [128, 8191], BF16)
    junk = P.sb("hjunk", [128, 8191], BF16)
    ssum = P.sb("hssum", [128, 1], F32)
    pp = Pool(P, "hfp", [128, 512], F32, 2, psum=True)
    wp = Pool(P, "hfw", [128, 512], F32, 2)
    for sq in hy_seqs(self):
        L = sq["L"]
        npos = 2 * L - 1
        self.load(h2, h2[:, 0:npos], sq["H2D"], sq["H2D"].t.ap())
        self.load(tvb, tvb[:, 0:npos], sq["tv"], dap(sq["tv"], 0, [[0, 128], [1, npos]]))
        chunks = []
        for (a, b, d) in ((0, L - 1, 1), (L - 1, npos, 0)):
            for c0 in range(a, b, 512):
                chunks.append((c0, min(512, b - c0), d))
        for o in range(2):
            for cc in range(4):
                for (c0, n, d) in chunks:
                    col = o * 1024 + d * 512 + cc * 128
                    di = (o * 2 + d) * 4 + cc
                    ps = pp.get()
                    P.op("tensor", lambda en, ps=ps, col=col, c0=c0, n=n: en.matmul(ps[:, 0:n], lhsT=w3[:, col:col + 128], rhs=h2[:, c0:c0 + n], start=True, stop=True),
                         reads=[w3, h2], writes=[ps])
                    w = wp.get()
                    P.op("scalar", lambda en, w=w, c0=c0, n=n, di=di: en.activation(out=w[:, 0:n], in_=tvb[:, c0:c0 + n], func=AF.Exp, scale=dec[:, di:di + 1]),
                         reads=[tvb, dec], writes=[w])
                    P.op("vector", lambda en, ps=ps, w=w, c0=c0, n=n: en.tensor_tensor(out=et[:, c0:c0 + n], in0=ps[:, 0:n], in1=w[:, 0:n], op=ALU.mult),
                         reads=[ps, w], writes=[et])
                P.op("vector", lambda en: en.memset(ssum[:], 0.0), writes=[ssum])
                P.op("scalar", lambda en: en.activation(out=junk[:, 0:npos], in_=et[:, 0:npos], func=AF.Abs, accum_out=ssum[:]), reads=[et, ssum], writes=[junk, ssum])
                P.op("vector", lambda en: en.reciprocal(out=ssum[:], in_=ssum[:]), reads=[ssum], writes=[ssum])
                P.op("vector", lambda en: en.tensor_scalar(out=eb[:, 0:npos], in0=et[:, 0:npos], scalar1=ssum[:, 0:1], scalar2=None, op0=ALU.mult), reads=[et, ssum], writes=[eb])
                row0 = (o * 4 + cc) * 128
                self.store(sq["EK"], sq["EK"].t.ap()[row0:row0 + 128, 0:npos], eb, eb[:, 0:npos])
    self.phase_end()


def phase_hy_conv(self):
    OWN, NQ, NGL, NG, NTL = self.OWN, self.NQ, self.NGL, self.NG, self.NTL
    P = self.P
    self.phase_begin()
    jrev = P.sb("jrev", [128, 128], F32)
    self.load(jrev, jrev[:], self.inputs["jrev"], self.inputs["jrev"].t.ap())
    idf = P.sb("hidf", [128, 128], F32)
    self.load(idf, idf[:], self.ident_f, self.ident_f.t.ap())
    hb = self.inputs["hy_bias"]
    B1 = P.sb("hB1", [128, 1, 512], F32)
    B2 = P.sb("hB2", [128, 512, 1], F32)
    self.load(B1, B1[:, 0, :], hb, dap(hb, 0, [[0, 128], [1, 512]]))
    self.load(B2, B2[:, :, 0], hb, dap(hb, 512, [[0, 128], [1, 512]]))
    NAM = 32
    VV = P.sb("hVV", [128, NAM, 128], F32)
    X1 = P.sb("hX1", [128, NAM, 128], F32)
    NOM = OWN // 128
    X2 = P.sb("hX2", [128, NOM, 128], F32)
    Zp = P.sb("hZp", [128, 128, 3 * NAM - 2], BF16)
    Y1 = P.sb("hY1", [128, 128, NAM], F32)
    Y2 = P.sb("hY2", [128, 128, NOM], F32)
    Z2 = X1
    T = P.sb("hT", [128, NAM, 128], F32)
    ZTs = P.sb("hZTs", [128, NOM * 128], BF16)
    bandp = Pool(P, "hband", [128, 63 * 128], BF16, 2)
    jp = Pool(P, "hjp", [128, 512], F32, 2, psum=True)
    yp = Pool(P, "hyp", [128, 16, 32], F32, 2, psum=True)
    tpp = Pool(P, "htp", [128, 4, 128], F32, 2, psum=True)
    HUv = self.HU.t.ap()
    for sq in hy_seqs(self):
        NA, NO, r0, o0, EK = sq["NA"], sq["NO"], sq["r0"], sq["o0"], sq["EK"]
        PW = 3 * NA - 2
        BW = (2 * NA - 1) * 128
        npos = 2 * sq["L"] - 1
        EW = EK.t.ap().shape[1]
        hu3 = HUv[r0:r0 + NA * 128, :].rearrange("(a p) c -> p a c", p=128)
        for cc in range(4):
            self.load(X1, X1[:, 0:NA, :], self.HU, hu3[:, :, cc * 128:(cc + 1) * 128])
            self.load(X2, X2[:, 0:NO, :], self.HU, hu3[:, 0:NO, 512 + cc * 128:512 + (cc + 1) * 128])
            self.load(VV, VV[:, 0:NA, :], self.HU, hu3[:, :, 1024 + cc * 128:1024 + (cc + 1) * 128])
            for order in range(2):
                nout = NA if order == 0 else NO
                P.op("gpsimd", lambda en: en.memset(Zp[:], 0.0), writes=[Zp])
                if order == 0:
                    ab = 4 if NA >= 4 else NA
                    for a0 in range(0, NA, ab):
                        ps = jp.get()
                        P.op("tensor", lambda en, ps=ps, a0=a0, ab=ab: en.matmul(ps[:, 0:ab * 128], lhsT=jrev[:], rhs=VV[:, a0:a0 + ab, :], start=True, stop=True),
                             reads=[jrev, VV], writes=[ps])
                        P.op("scalar", lambda en, ps=ps, a0=a0, ab=ab, NA=NA: en.copy(out=Zp[:, :, NA - 1 + a0:NA - 1 + a0 + ab].rearrange("p c a -> p a c"),
                                                                                   in_=ps[:, 0:ab * 128].rearrange("p (a c) -> p a c", a=ab)),
                             reads=[ps], writes=[Zp])
                else:
                    cb = min(128, 512 // NA)
                    for c0 in range(0, 128, cb):
                        ps = jp.get()
                        P.op("tensor", lambda en, ps=ps, c0=c0, cb=cb, NA=NA: en.matmul(ps[:, 0:cb * NA], lhsT=jrev[:], rhs=Y1[:, c0:c0 + cb, 0:NA], start=True, stop=True),
                             reads=[jrev, Y1], writes=[ps])
                        P.op("scalar", lambda en, ps=ps, c0=c0, cb=cb, NA=NA: en.copy(out=Zp[:, c0:c0 + cb, NA - 1:2 * NA - 1],
                                                                                   in_=ps[:, 0:cb * NA].rearrange("p (c a) -> p c a", c=cb)),
                             reads=[ps], writes=[Zp])
                dmax = NA - 1 if order == 0 else NO - 1
                deltas = list(range(-(NA - 1), dmax + 1))
                Yout = Y1 if order == 0 else Y2
                yps = None
                for c in range(128):
                    band = bandp.get()
                    row = (order * 4 + cc) * 128 + c
                    self.load(band, band[:, 0:BW], EK, dap(EK, row * EW, [[1, 128], [1, BW]]))
                    if c % 16 == 0:
                        yps = yp.get()
                    for d in deltas:
                        P.op("tensor", lambda en, yps=yps, band=band, c=c, d=d, NA=NA, nout=nout: en.matmul(
                            yps[:, c % 16, 0:nout], lhsT=band[:, (d + NA - 1) * 128:(d + NA) * 128], rhs=Zp[:, c, NA - 1 - d:NA - 1 - d + nout],
                            start=(d == deltas[0]), stop=(d == deltas[-1])), reads=[band, Zp], writes=[yps])
                    if c % 16 == 15:
                        c0 = c - 15
                        if order == 0:
                            P.op("vector", lambda en, yps=yps, c0=c0, nout=nout: en.tensor_copy(out=Y1[:, c0:c0 + 16, 0:nout], in_=yps[:, :, 0:nout]), reads=[yps], writes=[Y1])
                        else:
                            P.op("vector", lambda en, yps=yps, c0=c0, nout=nout: en.tensor_copy(out=Y2[:, c0:c0 + 16, 0:nout], in_=yps[:, :, 0:nout]), reads=[yps], writes=[Y2])
                if order == 0:
                    P.op("vector", lambda en, NA=NA: en.tensor_tensor(out=T[:, 0:NA, :], in0=VV[:, 0:NA, :], in1=B1[:, :, cc * 128:(cc + 1) * 128].to_broadcast([128, NA, 128]), op=ALU.mult),
                         reads=[VV, B1], writes=[T])
                    P.op("vector", lambda en, NA=NA: en.tensor_tensor(out=Y1[:, :, 0:NA], in0=Y1[:, :, 0:NA], in1=T[:, 0:NA, :].rearrange("p a c -> p c a"), op=ALU.add),
                         reads=[Y1, T], writes=[Y1])
                    P.op("vector", lambda en, NA=NA: en.tensor_tensor(out=Y1[:, :, 0:NA], in0=Y1[:, :, 0:NA], in1=X1[:, 0:NA, :].rearrange("p a c -> p c a"), op=ALU.mult),
                         reads=[Y1, X1], writes=[Y1])
                else:
                    P.op("vector", lambda en, NO=NO: en.tensor_tensor(out=T[:, 0:NO, :].rearrange("p a c -> p c a"), in0=Y1[:, :, 0:NO],
                                                                      in1=B2[:, cc * 128:(cc + 1) * 128, :].to_broadcast([128, 128, NO]), op=ALU.mult),
                         reads=[Y1, B2], writes=[T])
                    P.op("vector", lambda en, NO=NO: en.tensor_tensor(out=T[:, 0:NO, :].rearrange("p a c -> p c a"), in0=T[:, 0:NO, :].rearrange("p a c -> p c a"),
                                                                      in1=Y2[:, :, 0:NO], op=ALU.add), reads=[T, Y2], writes=[T])
                    P.op("vector", lambda en, NO=NO: en.tensor_tensor(out=Z2[:, 0:NO, :], in0=T[:, 0:NO, :], in1=X2[:, 0:NO, :], op=ALU.mult), reads=[T, X2], writes=[Z2])
            for a0 in range(0, NO, 4):
                ab = min(4, NO - a0)
                tp = tpp.get()
                for a in range(ab):
                    P.op("tensor", lambda en, tp=tp, a=a, a0=a0: en.transpose(tp[:, a, :], Z2[:, a0 + a, :], idf[:]), reads=[Z2, idf], writes=[tp])
                P.op("scalar", lambda en, tp=tp, a0=a0, ab=ab: en.copy(out=ZTs[:, a0 * 128:(a0 + ab) * 128], in_=tp[:, 0:ab, :]), reads=[tp], writes=[ZTs])
            self.store(self.ZT, self.ZT.t.ap()[cc, :, o0:o0 + NO * 128], ZTs, ZTs[:, 0:NO * 128])
    self.phase_end()


LayerBuilder.phase_hy_short = phase_hy_short
LayerBuilder.phase_hy_mlp = phase_hy_mlp
LayerBuilder.phase_hy_filt = phase_hy_filt
LayerBuilder.phase_hy_conv = phase_hy_conv


def phase_merge(self):
    OWN, NQ, NGL, NG, NTL = self.OWN, self.NQ, self.NGL, self.NG, self.NTL
    P = self.P
    full = self.ctx_full
    self.phase_begin()
    wst = Pool(P, "mws", [128, 16, 512], F32, 2)
    wbp = Pool(P, "mwb", [128, 16, 512], BF16, 2)
    inp_ = Pool(P, "min", [128, 16, 512], BF16, 2)
    gp = Pool(P, "mg", [128, 3, 512], BF16, 2)
    pa = Pool(P, "mpa", [128, 512], F32, 2, psum=True)
    pc = Pool(P, "mpc", [128, 512], F32, 2, psum=True)
    ph = Pool(P, "mph", [128, 512], F32, 2, psum=True)
    mp = Pool(P, "mm", [128, 512], F32, 2)
    tp = Pool(P, "mt", [128, 512], F32, 2)
    obp = Pool(P, "mob", [128, 512], BF16, 2)
    wa = self.inputs["w_attn_o"].t.ap().rearrange("(k p) n -> p k n", p=128)
    wc = self.inputs["w_conf_o"].t.ap().rearrange("(k p) n -> p k n", p=128)
    wh = self.inputs["w_hy_o"].t.ap().rearrange("(k p) n -> p k n", p=128)
    groups = [(g * 512, 512) for g in range(OWN // 512)] + ([(OWN, 256)] if full else [])
    gtv = self.GT.t.ap().rearrange("(b d) p t -> b d p t", b=3)
    for dg in range(4):
        ws = wst.get()
        self.load(ws, ws[:, 0:8, :], self.inputs["w_attn_o"], wa[:, :, dg * 512:(dg + 1) * 512])
        self.load(ws, ws[:, 8:12, :], self.inputs["w_conf_o"], wc[:, :, dg * 512:(dg + 1) * 512])
        self.load(ws, ws[:, 12:16, :], self.inputs["w_hy_o"], wh[:, :, dg * 512:(dg + 1) * 512])
        wb = wbp.get()
        P.op("gpsimd", lambda en, wb=wb, ws=ws: en.tensor_copy(out=wb[:, 0:8, :], in_=ws[:, 0:8, :]), reads=[ws], writes=[wb])
        P.op("gpsimd", lambda en, wb=wb, ws=ws: en.tensor_copy(out=wb[:, 8:16, :], in_=ws[:, 8:16, :]), reads=[ws, wb], writes=[wb])
        for (t0, n) in groups:
            x = inp_.get()
            self.load(x, x[:, 0:8, 0:n], self.ONT, self.ONT.t.ap().rearrange("h p t -> p h t")[:, :, t0:t0 + n])
            self.load(x, x[:, 8:12, 0:n], self.CT, self.CT.t.ap().rearrange("h p t -> p h t")[:, :, t0:t0 + n])
            self.load(x, x[:, 12:16, 0:n], self.ZT, self.ZT.t.ap().rearrange("h p t -> p h t")[:, :, t0:t0 + n])
            for j in range(4):
                dch = dg * 4 + j
                g = gp.get()
                self.load(g, g[:, :, 0:n], self.GT, gtv[:, dch].rearrange("b p t -> p b t")[:, :, t0:t0 + n])
                pss = []
                for (pool, k0, k1) in ((pa, 0, 8), (pc, 8, 12), (ph, 12, 16)):
                    ps = pool.get()
                    for k in range(k0, k1):
                        P.op("tensor", lambda en, ps=ps, wb=wb, x=x, k=k, j=j, k0=k0, k1=k1: en.matmul(ps[:, 0:n], lhsT=wb[:, k, j * 128:(j + 1) * 128], rhs=x[:, k, 0:n],
                                                                                                   start=(k == k0), stop=(k == k1 - 1)), reads=[wb, x], writes=[ps])
                    pss.append(ps)
                m = mp.get(); t = tp.get(); ob = obp.get()
                P.op("vector", lambda en, m=m, g=g, ps=pss[0]: en.tensor_tensor(out=m[:, 0:n], in0=ps[:, 0:n], in1=g[:, 0, 0:n], op=ALU.mult), reads=[pss[0], g], writes=[m])
                P.op("vector", lambda en, t=t, g=g, ps=pss[1]: en.tensor_tensor(out=t[:, 0:n], in0=ps[:, 0:n], in1=g[:, 1, 0:n], op=ALU.mult), reads=[pss[1], g], writes=[t])
                P.op("gpsimd", lambda en, m=m, t=t: en.tensor_tensor(out=m[:, 0:n], in0=m[:, 0:n], in1=t[:, 0:n], op=ALU.add), reads=[m, t], writes=[m])
                P.op("vector", lambda en, t=t, g=g, ps=pss[2]: en.tensor_tensor(out=t[:, 0:n], in0=ps[:, 0:n], in1=g[:, 2, 0:n], op=ALU.mult), reads=[pss[2], g, t], writes=[t])
                P.op("gpsimd", lambda en, m=m, t=t, ob=ob: en.tensor_tensor(out=ob[:, 0:n], in0=m[:, 0:n], in1=t[:, 0:n], op=ALU.add), reads=[m, t], writes=[ob])
                self.store(self.MG, self.MG.t.ap()[dch, :, t0:t0 + n], ob, ob[:, 0:n])
    self.phase_end()


def phase_wout(self):
    OWN, NQ, NGL, NG, NTL = self.OWN, self.NQ, self.NGL, self.NG, self.NTL
    P = self.P
    full = self.ctx_full
    self.phase_begin()
    wst = Pool(P, "ows", [128, 16, 512], F32, 2)
    wbp = Pool(P, "owb", [128, 16, 512], BF16, 2)
    mgp = Pool(P, "omg", [128, 16, 128], BF16, 3)
    xp = Pool(P, "ox", [128, 512], F32, 3)
    pp = Pool(P, "ops", [128, 512], F32, 3, psum=True)
    tp = Pool(P, "ot", [128, 512], F32, 2)
    G1 = [P.sb("oG1_%d" % r, [128, D], F32) for r in range(2)]
    for r in range(2):
        self.bcast_mod(G1[r], r, 2)
    wo = self.inputs["w_out"].t.ap().rearrange("(k p) n -> p k n", p=128)
    mgv = self.MG.t.ap().rearrange("k p t -> p k t")
    ntile = NTL + (2 if full else 0)
    for cg in range(4):
        ws = wst.get()
        self.load(ws, ws[:], self.inputs["w_out"], wo[:, :, cg * 512:(cg + 1) * 512])
        wb = wbp.get()
        P.op("gpsimd", lambda en, wb=wb, ws=ws: en.tensor_copy(out=wb[:, 0:8, :], in_=ws[:, 0:8, :]), reads=[ws], writes=[wb])
        P.op("gpsimd", lambda en, wb=wb, ws=ws: en.tensor_copy(out=wb[:, 8:16, :], in_=ws[:, 8:16, :]), reads=[ws, wb], writes=[wb])
        for i in range(ntile):
            row = 0 if i < NTL else 1
            mg = mgp.get()
            self.load(mg, mg[:], self.MG, mgv[:, :, i * 128:(i + 1) * 128])
            x = xp.get()
            if row == 0:
                self.load(x, x[:], self.xa, self.xa.t.ap()[i * 128:(i + 1) * 128, cg * 512:(cg + 1) * 512])
            else:
                self.load(x, x[:], self.xc, self.xc.t.ap()[(i - NTL) * 128:(i - NTL + 1) * 128, cg * 512:(cg + 1) * 512])
            ps = pp.get()
            for k in range(16):
                P.op("tensor", lambda en, ps=ps, mg=mg, wb=wb, k=k: en.matmul(ps[:], lhsT=mg[:, k, :], rhs=wb[:, k, :], start=(k == 0), stop=(k == 15)), reads=[mg, wb], writes=[ps])
            t = tp.get()
            P.op("vector", lambda en, t=t, ps=ps, row=row: en.tensor_tensor(out=t[:], in0=ps[:], in1=G1[row][:, cg * 512:(cg + 1) * 512], op=ALU.mult), reads=[ps, G1[row]], writes=[t])
            P.op("gpsimd", lambda en, t=t, x=x: en.tensor_tensor(out=t[:], in0=t[:], in1=x[:], op=ALU.add), reads=[t, x], writes=[t])
            self.store(self.X1, self.X1.t.ap()[i * 128:(i + 1) * 128, cg * 512:(cg + 1) * 512], t, t[:])
    self.phase_end()


LayerBuilder.phase_merge = phase_merge
LayerBuilder.phase_wout = phase_wout


def moe_dims(self):
    OWN, NQ, NGL, NG, NTL = self.OWN, self.NQ, self.NGL, self.NG, self.NTL
    T = NQ if self.ctx_full else OWN
    ntile = T // 128
    NB = (2 * T) // 128 + 64
    return T, ntile, NB


def declare_moe(self):
    OWN, NQ, NGL, NG, NTL = self.OWN, self.NQ, self.NGL, self.NG, self.NTL
    inp = self.inp
    T, ntile, NB = moe_dims(self)
    inp("w_r", [D, 72])
    inp("w_gate", [16384, 4096]); inp("w_up", [16384, 4096]); inp("w_down", [16384, 4096])
    inp("tri", [128, 128]); inp("u64", [64, 128])
    inp("jv", [1, NB]); inp("tokid", [128, ntile], I32); inp("iota_gu", [128, 16]); inp("iota_d", [128, 4])
    inp("norm_f_g", [1, D])
    sc = self.scratch
    self.H2 = sc("H2", [T + 128, D], F32)
    self.BT = sc("BT", [NB * 128, 1], I32)
    self.Y = sc("Y", [NB * 128, D], F32)
    self.XOL = sc("XOL", [OWN, D], F32, out=self.ext_out)
    self.XOC = sc("XOC", [LC, D], F32, out=(self.ext_out and self.ctx_full))
    P = self.P
    self.d1 = P.sb("pd1", [128, ntile], I32)
    self.d2 = P.sb("pd2", [128, ntile], I32)
    self.w1 = P.sb("pw1", [128, ntile], F32)
    self.w2 = P.sb("pw2", [128, ntile], F32)
    self.be = P.sb("pbe", [128, NB], F32)


def phase_router(self):
    OWN, NQ, NGL, NG, NTL = self.OWN, self.NQ, self.NGL, self.NG, self.NTL
    P = self.P
    T, ntile, NB = moe_dims(self)
    self.phase_begin()
    AB = self.make_AB(self.norm2_g, 3, 4, "n2")
    idf = P.sb("ridf", [128, 128], F32)
    self.load(idf, idf[:], self.ident_f, self.ident_f.t.ap())
    ones = P.sb("rones", [128, 128], F32)
    self.load(ones, ones[:], self.ones_f, self.ones_f.t.ap())
    tri = P.sb("rtri", [128, 128], F32)
    self.load(tri, tri[:], self.inputs["tri"], self.inputs["tri"].t.ap())
    u64 = P.sb("ru64", [64, 128], F32)
    self.load(u64, u64[:], self.inputs["u64"], self.inputs["u64"].t.ap())
    wr = P.sb("rwr", [128, 16, 72], F32)
    self.load(wr, wr[:], self.inputs["w_r"], self.inputs["w_r"].t.ap().rearrange("(k p) n -> p k n", p=128))
    LG = P.sb("rLG", [128, ntile, 72], F32)
    xp = Pool(P, "rx", [128, D], F32, 2)
    sqp = Pool(P, "rsq", [128, D], F32, 1)
    tmpp = Pool(P, "rtmp", [128, D], F32, 1)
    hp = Pool(P, "rh", [128, D], F32, 2)
    ssp = Pool(P, "rss", [128, 1], F32, 2)
    rsp = Pool(P, "rrs", [128, 1], F32, 2)
    htp = Pool(P, "rht", [128, 16, 128], F32, 2)
    ptp = Pool(P, "rpt", [128, 4, 128], F32, 2, psum=True)
    plp = Pool(P, "rpl", [128, 72], F32, 2, psum=True)
    zero = P.sb("rzero", [128, D], F32)
    P.op("gpsimd", lambda en: en.memset(zero[:], 0.0), writes=[zero])
    self.store(self.H2, self.H2.t.ap()[T:T + 128, :], zero, zero[:])
    for i in range(ntile):
        row = 0 if i < NTL else 1
        x = xp.get()
        self.load(x, x[:], self.X1, self.X1.t.ap()[i * 128:(i + 1) * 128, :])
        h = hp.get()
        A, Bt = AB[row]
        self.norm_tile(x, A, Bt, h, sqp.get(), ssp.get(), rsp.get(), tmpp.get())
        self.store(self.H2, self.H2.t.ap()[i * 128:(i + 1) * 128, :], h, h[:])
        ht = htp.get()
        for k4 in range(4):
            pt = ptp.get()
            for a in range(4):
                kc = k4 * 4 + a
                P.op("tensor", lambda en, pt=pt, a=a, kc=kc, h=h: en.transpose(pt[:, a, :], h[:, kc * 128:(kc + 1) * 128], idf[:]), reads=[h, idf], writes=[pt])
            if k4 % 2 == 0:
                P.op("scalar", lambda en, pt=pt, ht=ht, k4=k4: en.copy(out=ht[:, k4 * 4:k4 * 4 + 4, :], in_=pt[:]), reads=[pt], writes=[ht])
            else:
                P.op("vector", lambda en, pt=pt, ht=ht, k4=k4: en.tensor_copy(out=ht[:, k4 * 4:k4 * 4 + 4, :], in_=pt[:]), reads=[pt], writes=[ht])
        pl = plp.get()
        for kc in range(16):
            P.op("tensor", lambda en, pl=pl, ht=ht, kc=kc: en.matmul(pl[:], lhsT=ht[:, kc, :], rhs=wr[:, kc, :], start=(kc == 0), stop=(kc == 15)), reads=[ht, wr], writes=[pl])
        P.op("vector", lambda en, pl=pl, i=i: en.tensor_copy(out=LG[:, i, :], in_=pl[:]), reads=[pl], writes=[LG])
    nt = ntile

    def sbt(name, shape, dt=F32):
        return P.sb(name, shape, dt)
    gmax = sbt("gmax", [128, nt, 1]); gmask = sbt("gmask", [128, nt, 8]); dd = sbt("rdd", [128, nt, 8]); sm = sbt("rsm", [128, nt, 1]); pg = sbt("rpg", [128, nt, 1])
    tmp4 = sbt("rtmp4", [128, nt, 8, 8]); les = sbt("rles", [128, nt, 8]); v1 = sbt("rv1", [128, nt, 1]); v2 = sbt("rv2", [128, nt, 1])
    m1 = sbt("rm1", [128, nt, 8]); m2 = sbt("rm2", [128, nt, 8]); le2 = sbt("rle2", [128, nt, 8]); ex = sbt("rex", [128, nt, 1])
    M1 = sbt("rM1", [128, nt, 64]); M2 = sbt("rM2", [128, nt, 64]); M = sbt("rM", [128, nt, 64])
    V = "vector"
    lg = LG[:, :, 0:8]
    le4 = LG[:, :, 8:72].rearrange("p t (g e) -> p t g e", g=8)
    P.op(V, lambda en: en.tensor_reduce(out=gmax[:], in_=lg, axis=AX.X, op=ALU.max), reads=[LG], writes=[gmax])
    P.op(V, lambda en: en.tensor_tensor(out=gmask[:], in0=lg, in1=gmax[:].to_broadcast([128, nt, 8]), op=ALU.is_equal), reads=[LG, gmax], writes=[gmask])
    P.op(V, lambda en: en.tensor_tensor(out=dd[:], in0=lg, in1=gmax[:].to_broadcast([128, nt, 8]), op=ALU.subtract), reads=[LG, gmax], writes=[dd])
    P.op("scalar", lambda en: en.activation(out=dd[:], in_=dd[:], func=AF.Exp), reads=[dd], writes=[dd])
    P.op(V, lambda en: en.tensor_reduce(out=sm[:], in_=dd[:], axis=AX.X, op=ALU.add), reads=[dd], writes=[sm])
    P.op(V, lambda en: en.reciprocal(out=pg[:], in_=sm[:]), reads=[sm], writes=[pg])
    P.op(V, lambda en: en.tensor_tensor(out=tmp4[:], in0=le4, in1=gmask[:].rearrange("p t (g o) -> p t g o", o=1).to_broadcast([128, nt, 8, 8]), op=ALU.mult),
         reads=[LG, gmask], writes=[tmp4])
    P.op(V, lambda en: en.tensor_reduce(out=les[:], in_=tmp4[:].rearrange("p t g e -> p t e g"), axis=AX.X, op=ALU.add), reads=[tmp4], writes=[les])
    P.op(V, lambda en: en.tensor_reduce(out=v1[:], in_=les[:], axis=AX.X, op=ALU.max), reads=[les], writes=[v1])
    P.op(V, lambda en: en.tensor_tensor(out=m1[:], in0=les[:], in1=v1[:].to_broadcast([128, nt, 8]), op=ALU.is_equal), reads=[les, v1], writes=[m1])
    P.op(V, lambda en: en.scalar_tensor_tensor(out=le2[:], in0=m1[:], scalar=-1e30, in1=les[:], op0=ALU.mult, op1=ALU.add), reads=[m1, les], writes=[le2])
    P.op(V, lambda en: en.tensor_reduce(out=v2[:], in_=le2[:], axis=AX.X, op=ALU.max), reads=[le2], writes=[v2])
    P.op(V, lambda en: en.tensor_tensor(out=m2[:], in0=le2[:], in1=v2[:].to_broadcast([128, nt, 8]), op=ALU.is_equal), reads=[le2, v2], writes=[m2])
    P.op(V, lambda en: en.tensor_tensor(out=ex[:], in0=v2[:], in1=v1[:], op=ALU.subtract), reads=[v1, v2], writes=[ex])
    P.op("scalar", lambda en: en.activation(out=ex[:], in_=ex[:], func=AF.Exp), reads=[ex], writes=[ex])
    P.op(V, lambda en: en.tensor_scalar(out=ex[:], in0=ex[:], scalar1=1.0, scalar2=None, op0=ALU.add), reads=[ex], writes=[ex])
    P.op(V, lambda en: en.reciprocal(out=ex[:], in_=ex[:]), reads=[ex], writes=[ex])
    P.op(V, lambda en: en.tensor_tensor(out=self.w1[:], in0=pg[:, :, 0], in1=ex[:, :, 0], op=ALU.mult), reads=[pg, ex], writes=[self.w1])
    P.op(V, lambda en: en.tensor_tensor(out=self.w2[:], in0=pg[:, :, 0], in1=self.w1[:], op=ALU.subtract), reads=[pg, self.w1], writes=[self.w2])
    for (Mk, mk) in ((M1, m1), (M2, m2)):
        P.op(V, lambda en, Mk=Mk, mk=mk: en.tensor_tensor(out=Mk[:].rearrange("p t (g e) -> p t g e", g=8),
                                                          in0=gmask[:].rearrange("p t (g o) -> p t g o", o=1).to_broadcast([128, nt, 8, 8]),
                                                          in1=mk[:].rearrange("p t (o e) -> p t o e", o=1).to_broadcast([128, nt, 8, 8]), op=ALU.mult),
             reads=[gmask, mk], writes=[Mk])
    P.op(V, lambda en: en.tensor_tensor(out=M[:], in0=M1[:], in1=M2[:], op=ALU.add), reads=[M1, M2], writes=[M])
    pcb = P.ps("rpcb", [128, 64], F32)
    pct = P.ps("rpct", [64, 128], F32)
    for i in range(nt):
        P.op("tensor", lambda en, i=i: en.matmul(pcb[:], lhsT=ones[:], rhs=M[:, i, :], start=(i == 0), stop=(i == nt - 1)), reads=[ones, M], writes=[pcb])
    for i in range(nt):
        P.op("tensor", lambda en, i=i: en.matmul(pct[:], lhsT=M[:, i, :], rhs=ones[:], start=(i == 0), stop=(i == nt - 1)), reads=[ones, M], writes=[pct])
    cT = sbt("rcT", [64, 128]); rT = sbt("rrT", [64, 128])
    P.op(V, lambda en: en.tensor_copy(out=cT[:], in_=pct[:]), reads=[pct], writes=[cT])
    qT = sbt("rqT", [64, 128]); qi = sbt("rqi", [64, 128], I32)
    P.op(V, lambda en: en.tensor_scalar(out=qT[:], in0=cT[:], scalar1=1.0 / 128, scalar2=None, op0=ALU.mult), reads=[cT], writes=[qT])
    P.op(V, lambda en: en.tensor_copy(out=qi[:], in_=qT[:]), reads=[qT], writes=[qi])
    P.op(V, lambda en: en.tensor_copy(out=rT[:], in_=qi[:]), reads=[qi], writes=[rT])
    P.op(V, lambda en: en.tensor_tensor(out=qT[:], in0=rT[:], in1=qT[:], op=ALU.is_lt), reads=[rT, qT], writes=[qT])
    P.op(V, lambda en: en.tensor_tensor(out=rT[:], in0=rT[:], in1=qT[:], op=ALU.add), reads=[rT, qT], writes=[rT])
    P.op(V, lambda en: en.tensor_scalar(out=cT[:], in0=rT[:], scalar1=128.0, scalar2=None, op0=ALU.mult), reads=[rT], writes=[cT])
    pst = P.ps("rpst", [128, 128], F32)
    P.op("tensor", lambda en: en.matmul(pst[:], lhsT=cT[:], rhs=u64[:], start=True, stop=True), reads=[cT, u64], writes=[pst])
    pse = sbt("rpse", [128, 128])
    P.op(V, lambda en: en.tensor_copy(out=pse[:], in_=pst[:]), reads=[pst], writes=[pse])
    pcum = Pool(P, "rpcum", [128, 64], F32, 1, psum=True)
    pos = Pool(P, "rpos", [128, 64], F32, 2)
    jk = Pool(P, "rjk", [128, 64], F32, 2)
    d1f = sbt("rd1f", [128, nt]); d2f = sbt("rd2f", [128, nt])
    for i in range(nt):
        pc = pcum.get()
        P.op("tensor", lambda en, pc=pc, i=i: en.matmul(pc[:], lhsT=tri[:], rhs=M[:, i, :], start=True, stop=(i == 0)), reads=[tri, M], writes=[pc])
        for i2 in range(i):
            P.op("tensor", lambda en, pc=pc, i2=i2, i=i: en.matmul(pc[:], lhsT=ones[:], rhs=M[:, i2, :], start=False, stop=(i2 == i - 1)), reads=[ones, M], writes=[pc])
        po = pos.get()
        P.op(V, lambda en, po=po, pc=pc: en.tensor_tensor(out=po[:], in0=pc[:], in1=pse[:, 0:64], op=ALU.add), reads=[pc, pse], writes=[po])
        for (Mk, df) in ((M1, d1f), (M2, d2f)):
            j = jk.get()
            P.op(V, lambda en, j=j, Mk=Mk, po=po, i=i: en.tensor_tensor(out=j[:], in0=Mk[:, i, :], in1=po[:], op=ALU.mult), reads=[Mk, po], writes=[j])
            P.op(V, lambda en, j=j, df=df, i=i: en.tensor_reduce(out=df[:, i:i + 1], in_=j[:], axis=AX.X, op=ALU.add), reads=[j, df], writes=[df])
    P.op(V, lambda en: en.tensor_copy(out=self.d1[:], in_=d1f[:]), reads=[d1f], writes=[self.d1])
    P.op(V, lambda en: en.tensor_copy(out=self.d2[:], in_=d2f[:]), reads=[d2f], writes=[self.d2])
    jv = sbt("rjv", [128, NB, 1])
    self.load(jv, jv[:, :, 0], self.inputs["jv"], dap(self.inputs["jv"], 0, [[0, 128], [1, NB]]))
    JC = 33
    cmp_ = sbt("rcmp", [128, JC, 64])
    bef = sbt("rbef", [128, NB, 1])
    for j0 in range(0, NB, JC):
        jn = min(JC, NB - j0)
        P.op(V, lambda en, j0=j0, jn=jn: en.tensor_tensor(out=cmp_[:, 0:jn, :], in0=pse[:, 64:128].rearrange("p (o e) -> p o e", o=1).to_broadcast([128, jn, 64]),
                                                          in1=jv[:, j0:j0 + jn, :].to_broadcast([128, jn, 64]), op=ALU.is_le), reads=[pse, jv], writes=[cmp_])
        P.op(V, lambda en, j0=j0, jn=jn: en.tensor_reduce(out=bef[:, j0:j0 + jn, :], in_=cmp_[:, 0:jn, :], axis=AX.X, op=ALU.add), reads=[cmp_, bef], writes=[bef])
    P.op(V, lambda en: en.tensor_scalar(out=self.be[:], in0=bef[:, :, 0], scalar1=63.0, scalar2=None, op0=ALU.min), reads=[bef], writes=[self.be])
    bti = P.sb("rbti", [128, NB], I32)
    P.op("gpsimd", lambda en: en.memset(bti[:], T), writes=[bti])
    self.store(self.BT, self.BT.t.ap().rearrange("(p j) o -> p (j o)", p=128), bti, bti[:])
    tok = P.sb("rtok", [128, nt], I32)
    self.load(tok, tok[:], self.inputs["tokid"], self.inputs["tokid"].t.ap())
    BT = self.BT
    for i in range(nt):
        for dk in (self.d1, self.d2):
            P.dma("gpsimd", lambda en, dk=dk, i=i: en.indirect_dma_start(out=BT.t.ap(), out_offset=bass.IndirectOffsetOnAxis(ap=dk[:, i:i + 1], axis=0),
                                                                        in_=tok[:, i:i + 1], in_offset=None),
                  reads=[tok, dk, BT], writes=[BT], sb=tok, into=False)
    self.phase_end()


def phase_experts(self):
    OWN, NQ, NGL, NG, NTL = self.OWN, self.NQ, self.NGL, self.NG, self.NTL
    P = self.P
    T, ntile, NB = moe_dims(self)
    self.phase_begin()
    idf = P.sb("eidf", [128, 128], F32)
    self.load(idf, idf[:], self.ident_f, self.ident_f.t.ap())
    igu = P.sb("eigu", [128, 16], F32)
    self.load(igu, igu[:], self.inputs["iota_gu"], self.inputs["iota_gu"].t.ap())
    be128 = P.sb("ebe128", [128, NB], F32)
    P.op("vector", lambda en: en.tensor_scalar(out=be128[:], in0=self.be[:], scalar1=128.0, scalar2=igu[:, 0:1], op0=ALU.mult, op1=ALU.add), reads=[self.be, igu], writes=[be128])
    idx2 = P.sb("eidx2", [128, NB, 2], F32)
    P.op("vector", lambda en: en.tensor_copy(out=idx2[:, :, 0], in_=be128[:]), reads=[be128], writes=[idx2])
    P.op("vector", lambda en: en.tensor_scalar(out=idx2[:, :, 1], in0=be128[:], scalar1=8192.0, scalar2=None, op0=ALU.add), reads=[be128, idx2], writes=[idx2])
    idxi = P.sb("eidxi", [128, NB, 2], I32)
    P.op("vector", lambda en: en.tensor_copy(out=idxi[:], in_=idx2[:]), reads=[idx2], writes=[idxi])
    tokp = Pool(P, "etok", [128, 1], I32, 3)
    xbp = Pool(P, "exb", [128, D], F32, 2)
    xtp = Pool(P, "exT", [128, 16, 128], F32, 2)
    Wg = [P.sb("eWg%d" % k, [128, 4096], F32) for k in range(2)]
    Wu = [P.sb("eWu%d" % k, [128, 4096], F32) for k in range(2)]
    Wd = [P.sb("eWd%d" % k, [128, 4096], F32) for k in range(2)]
    ptp = Pool(P, "ept", [128, 4, 128], F32, 2, psum=True)
    pg = P.ps("epg", [128, 512], F32)
    pu = P.ps("epu", [128, 512], F32)
    py = [P.ps("epy%d" % n, [128, 512], F32) for n in range(4)]
    sgp = Pool(P, "esg", [128, 512], F32, 2)
    acp = Pool(P, "eac", [128, 512], F32, 2)
    atp = Pool(P, "eaT", [128, 4, 128], F32, 2)
    ybp = Pool(P, "eyb", [128, D], F32, 2)
    wg_in, wu_in, wd_in = self.inputs["w_gate"], self.inputs["w_up"], self.inputs["w_down"]
    for j in range(NB):
        tk = tokp.get()
        self.load(tk, tk[:], self.BT, self.BT.t.ap()[j * 128:(j + 1) * 128, :])
        xb = xbp.get()
        H2 = self.H2
        P.dma("gpsimd", lambda en, xb=xb, tk=tk: en.indirect_dma_start(out=xb[:], out_offset=None, in_=H2.t.ap(),
                                                                       in_offset=bass.IndirectOffsetOnAxis(ap=tk[:, 0:1], axis=0)),
              reads=[H2, tk], writes=[xb], sb=xb)
        for hf in range(2):
            for (Wt, win) in ((Wg, wg_in), (Wu, wu_in), (Wd, wd_in)):
                P.dma("gpsimd", lambda en, Wt=Wt, win=win, hf=hf, j=j: en.indirect_dma_start(out=Wt[hf][:], out_offset=None, in_=win.t.ap(),
                                                                                           in_offset=bass.IndirectOffsetOnAxis(ap=idxi[:, j, hf:hf + 1], axis=0)),
                      reads=[win, idxi], writes=[Wt[hf]], sb=Wt[hf])
        xT = xtp.get()
        for k4 in range(4):
            pt = ptp.get()
            for a in range(4):
                kc = k4 * 4 + a
                P.op("tensor", lambda en, pt=pt, a=a, kc=kc, xb=xb: en.transpose(pt[:, a, :], xb[:, kc * 128:(kc + 1) * 128], idf[:]), reads=[xb, idf], writes=[pt])
            if k4 % 2 == 0:
                P.op("scalar", lambda en, pt=pt, xT=xT, k4=k4: en.copy(out=xT[:, k4 * 4:k4 * 4 + 4, :], in_=pt[:]), reads=[pt], writes=[xT])
            else:
                P.op("vector", lambda en, pt=pt, xT=xT, k4=k4: en.tensor_copy(out=xT[:, k4 * 4:k4 * 4 + 4, :], in_=pt[:]), reads=[pt], writes=[xT])
        for (ps_, Wt) in ((pg, Wg), (pu, Wu)):
            for kc in range(16):
                P.op("tensor", lambda en, xT=xT, kc=kc, ps_=ps_, Wt=Wt: en.matmul(ps_[:], lhsT=xT[:, kc, :], rhs=Wt[kc // 8][:, (kc % 8) * 512:(kc % 8 + 1) * 512],
                                                                                 start=(kc == 0), stop=(kc == 15)), reads=[xT, Wt[kc // 8]], writes=[ps_])
        sg = sgp.get(); ac = acp.get()
        P.op("scalar", lambda en, sg=sg: en.activation(out=sg[:], in_=pg[:], func=AF.Silu), reads=[pg], writes=[sg])
        P.op("vector", lambda en, sg=sg, ac=ac: en.tensor_tensor(out=ac[:], in0=pu[:], in1=sg[:], op=ALU.mult), reads=[pu, sg], writes=[ac])
        pt = ptp.get()
        for fc in range(4):
            P.op("tensor", lambda en, pt=pt, fc=fc, ac=ac: en.transpose(pt[:, fc, :], ac[:, fc * 128:(fc + 1) * 128], idf[:]), reads=[ac, idf], writes=[pt])
        aT = atp.get()
        P.op("scalar", lambda en, pt=pt, aT=aT: en.copy(out=aT[:], in_=pt[:]), reads=[pt], writes=[aT])
        yb = ybp.get()
        for n in range(4):
            for fc in range(4):
                P.op("tensor", lambda en, aT=aT, n=n, fc=fc: en.matmul(py[n][:], lhsT=aT[:, fc, :], rhs=Wd[fc // 2][:, (fc % 2) * 2048 + n * 512:(fc % 2) * 2048 + (n + 1) * 512],
                                                                      start=(fc == 0), stop=(fc == 3)), reads=[aT, Wd[fc // 2]], writes=[py[n]])
            if n % 2 == 0:
                P.op("scalar", lambda en, yb=yb, n=n: en.copy(out=yb[:, n * 512:(n + 1) * 512], in_=py[n][:]), reads=[py[n], yb], writes=[yb])
            else:
                P.op("vector", lambda en, yb=yb, n=n: en.tensor_copy(out=yb[:, n * 512:(n + 1) * 512], in_=py[n][:]), reads=[py[n], yb], writes=[yb])
        self.store(self.Y, self.Y.t.ap()[j * 128:(j + 1) * 128, :], yb, yb[:], eng="sync")
    self.phase_end()


def phase_combine(self):
    OWN, NQ, NGL, NG, NTL = self.OWN, self.NQ, self.NGL, self.NG, self.NTL
    P = self.P
    T, ntile, NB = moe_dims(self)
    final = (self.l == 1)
    self.phase_begin()
    G2 = [P.sb("cG2_%d" % r, [128, D], F32) for r in range(2)]
    for r in range(2):
        self.bcast_mod(G2[r], r, 5)
    if final:
        gf = P.sb("cgf", [128, D], F32)
        nf = self.inputs["norm_f_g"]
        self.load(gf, gf[:], nf, dap(nf, 0, [[0, 128], [1, D]]))
    y1p = Pool(P, "cy1", [128, D], F32, 2)
    y2p = Pool(P, "cy2", [128, D], F32, 2)
    xp = Pool(P, "cx", [128, D], F32, 2)
    mp = Pool(P, "cm", [128, D], F32, 2)
    sqp = Pool(P, "csq2", [128, D], F32, 1)
    ssp = Pool(P, "css", [128, 1], F32, 2)
    Y = self.Y
    for i in range(ntile):
        row = 0 if i < NTL else 1
        y1 = y1p.get(); y2 = y2p.get()
        for (yt, dk) in ((y1, self.d1), (y2, self.d2)):
            P.dma("gpsimd", lambda en, yt=yt, dk=dk, i=i: en.indirect_dma_start(out=yt[:], out_offset=None, in_=Y.t.ap(),
                                                                              in_offset=bass.IndirectOffsetOnAxis(ap=dk[:, i:i + 1], axis=0)),
                  reads=[Y, dk], writes=[yt], sb=yt)
        x = xp.get()
        self.load(x, x[:], self.X1, self.X1.t.ap()[i * 128:(i + 1) * 128, :])
        m = mp.get()
        P.op("vector", lambda en, m=m, y1=y1, i=i: en.tensor_scalar(out=m[:], in0=y1[:], scalar1=self.w1[:, i:i + 1], scalar2=None, op0=ALU.mult), reads=[y1, self.w1], writes=[m])
        P.op("vector", lambda en, m=m, y2=y2, i=i: en.scalar_tensor_tensor(out=m[:], in0=y2[:], scalar=self.w2[:, i:i + 1], in1=m[:], op0=ALU.mult, op1=ALU.add),
             reads=[y2, self.w2, m], writes=[m])
        if "MOE" in self.taps:
            self.store(self.MOE, self.MOE.t.ap()[i * 128:(i + 1) * 128, :], m, m[:])
        P.op("gpsimd", lambda en, m=m, row=row: en.tensor_tensor(out=m[:], in0=m[:], in1=G2[row][:], op=ALU.mult), reads=[m, G2[row]], writes=[m])
        P.op("gpsimd", lambda en, m=m, x=x: en.tensor_tensor(out=m[:], in0=m[:], in1=x[:], op=ALU.add), reads=[m, x], writes=[m])
        if final:
            sq = sqp.get(); ss = ssp.get()
            P.op("scalar", lambda en, sq=sq, ss=ss, m=m: en.activation(out=sq[:], in_=m[:], func=AF.Square, accum_out=ss[:]), reads=[m], writes=[sq, ss])
            P.op("vector", lambda en, ss=ss: en.tensor_scalar(out=ss[:], in0=ss[:], scalar1=1.0 / D, scalar2=EPS, op0=ALU.mult, op1=ALU.add), reads=[ss], writes=[ss])
            P.op("scalar", lambda en, ss=ss: en.activation(out=ss[:], in_=ss[:], func=AF.Sqrt), reads=[ss], writes=[ss])
            P.op("vector", lambda en, ss=ss: en.reciprocal(out=ss[:], in_=ss[:]), reads=[ss], writes=[ss])
            P.op("vector", lambda en, m=m, ss=ss: en.scalar_tensor_tensor(out=m[:], in0=m[:], scalar=ss[:, 0:1], in1=gf[:], op0=ALU.mult, op1=ALU.mult), reads=[m, ss, gf], writes=[m])
        if i < NTL:
            self.store(self.XOL, self.XOL.t.ap()[i * 128:(i + 1) * 128, :], m, m[:], eng="sync")
        else:
            self.store(self.XOC, self.XOC.t.ap()[(i - NTL) * 128:(i - NTL + 1) * 128, :], m, m[:], eng="sync")
    self.phase_end()


LayerBuilder.declare_moe = declare_moe
LayerBuilder.phase_router = phase_router
LayerBuilder.phase_experts = phase_experts
LayerBuilder.phase_combine = phase_combine


ALL_PHASES = ["mod", "norm1", "inproj", "attn", "conf", "hy_short", "hy_mlp", "hy_filt", "hy_conv", "merge", "wout", "router", "experts", "combine"]


def build_layer(layer, taps=(), phases=None, own=2048):
    Bd = LayerBuilder(layer, taps=taps, own=own)
    Bd.declare()
    for ph in (phases or ALL_PHASES):
        getattr(Bd, "phase_" + ph)()
    Bd.P.wait_all("gpsimd", list(Bd.outputs.values()))
    Bd.P.wait_all("sync", list(Bd.outputs.values()))
    Bd.P.emit()
    return Bd


def build_fused():
    L0 = LayerBuilder(0, own=S, sfx="_0", ext_out=False)
    L0.declare()
    for ph in ALL_PHASES:
        getattr(L0, "phase_" + ph)()
    L1 = LayerBuilder(1, own=2048, shared=(L0.nc, L0.P), sfx="_1", xa=L0.XOL, xc=L0.XOC, ext_out=True)
    L1.declare()
    for ph in ALL_PHASES:
        getattr(L1, "phase_" + ph)()
    outs = list(L1.outputs.values())
    L1.P.wait_all("gpsimd", outs)
    L1.P.wait_all("sync", outs)
    L1.P.emit()
    return L0, L1


GRID_W = 64
ROPE_BASE = 10000.0


def core_order(half):
    return np.arange(S) if half == 0 else np.arange(S - 1, -1, -1)


def rope_tables(order):
    rows = (order // GRID_W).astype(np.float32)
    cols = (order % GRID_W).astype(np.float32)
    inv = (np.float32(ROPE_BASE) ** (-np.arange(0, 32, 2, dtype=np.float32) / np.float32(32))).astype(np.float32)
    cos_t = np.zeros((128, S), np.float32)
    sin_t = np.zeros((128, S), np.float32)
    for p in range(128):
        d = p % 64
        pos = rows if d < 32 else cols
        dd = d % 32
        ang = (pos * inv[dd % 16]).astype(np.float32)
        cos_t[p] = np.cos(ang)
        sin_t[p] = -np.sin(ang) if dd < 16 else np.sin(ang)
    return cos_t, sin_t


def rope_perm():
    rm = np.zeros((128, 128), np.float32)
    for m in range(128):
        dd = m % 32
        base = m - dd
        rm[base + (dd + 16) % 32, m] = 1.0
    return rm


def hy_emb_ext(L):
    n = np.arange(L, dtype=np.float32)
    t = (n / np.float32(max(L - 1, 1))).astype(np.float32)
    w = (np.float32(2.0 * np.pi) * n / np.float32(L)).astype(np.float32)
    f = np.linspace(1e-4, 15, 16, dtype=np.float32)
    fw = (w[:, None] * f[None, :]).astype(np.float32)
    emb = np.concatenate([t[:, None], np.cos(fw), -np.sin(fw)], axis=-1).astype(np.float32)
    idx = np.abs(np.arange(2 * L - 1) - (L - 1))
    return np.ascontiguousarray(emb[idx].T), np.ascontiguousarray(t[idx].reshape(1, -1))


_WCACHE = {}


def prep_core(inp, l, b, half, xl=None, xc=None, own=2048, sfx=""):
    order = core_order(half)
    xl = inp["x"] if xl is None else xl
    xc = inp["ctx"] if xc is None else xc
    m = {}
    m["xa"] = np.ascontiguousarray(xl[b][order])
    m["xc"] = np.ascontiguousarray(xc[b] if half == 0 else xc[b][::-1])
    cvec = np.stack([inp["c"][b], inp["c_ctx"]])
    m["ct"] = np.ascontiguousarray(cvec.reshape(2, 16, 128).transpose(2, 1, 0).reshape(128, 32))
    m["w_mod"] = inp["w_mod"][l]
    m["b_mod"] = inp["b_mod"][l].reshape(1, -1)
    m["norm1_g"] = inp["norm1_g"][l].reshape(1, -1)
    m["norm2_g"] = inp["norm2_g"][l].reshape(1, -1)
    m["w_in"] = inp["w_in"][l]
    c, s = rope_tables(order)
    m["cos_t"], m["sin_t"] = c, s
    m["rm"] = rope_perm()
    for nm in ("lam_q1", "lam_k1", "lam_q2", "lam_k2", "attn_subln_g"):
        m[nm] = inp[nm][l].reshape(1, -1)
    cw = inp["conf_dw_w"][l]
    if half == 1:
        cw = cw[::-1]
    m["conf_w"] = np.ascontiguousarray(cw.T.reshape(4, 128, 31).transpose(1, 0, 2).reshape(128, 124))
    cv = np.stack([inp["conf_dw_b"][l], inp["conf_ln_g"][l], inp["conf_ln_b"][l]])
    m["conf_v"] = np.ascontiguousarray(cv.reshape(3, 4, 128).transpose(2, 0, 1).reshape(128, 12))
    scw = inp["hy_sc_w"][l]
    if half == 1:
        scw = scw[::-1]
    m["hy_scw"] = np.ascontiguousarray(scw.reshape(1, -1))
    m["hy_scb"] = inp["hy_sc_b"][l].reshape(1, -1)
    fr = inp["hy_freq"][l]
    m["hy_v"] = np.ascontiguousarray(np.stack([inp["hy_b1"][l], fr[0], inp["hy_b2"][l], fr[1]], axis=1))
    m["hy_w1"] = inp["hy_w1"][l]
    m["hy_w2"] = inp["hy_w2"][l]
    w3 = inp["hy_w3"][l].reshape(64, 2, 2, 512)
    dec = inp["hy_decay"][l].reshape(2, 2, 512)
    if half == 1:
        w3 = w3[:, :, ::-1]
        dec = dec[:, ::-1]
    m["hy_w3"] = np.ascontiguousarray(w3.reshape(64, 2048))
    m["hy_dec"] = np.ascontiguousarray(dec.reshape(2, 2, 4, 128).transpose(3, 0, 1, 2).reshape(128, 16))
    m["hy_bias"] = inp["hy_bias"][l].reshape(1, -1)
    m["jrev"] = np.ascontiguousarray(np.eye(128, dtype=np.float32)[::-1])
    for nm, L in (("lat", S), ("ctx", LC)):
        e, tv = hy_emb_ext(L)
        m["emb_" + nm] = e
        m["tv_" + nm] = tv
    for nm in ("w_attn_o", "w_conf_o", "w_hy_o", "w_out"):
        m[nm] = inp[nm][l]
    T = own + (LC if l == 0 else 0)
    NB = (2 * T) // 128 + 64
    wre = inp["w_router_expert"][l].transpose(1, 0, 2).reshape(D, 64)
    m["w_r"] = np.ascontiguousarray(np.concatenate([inp["w_router_group"][l], wre], axis=1))
    for nm, src in (("w_gate", "w_exp_gate"), ("w_up", "w_exp_up"), ("w_down", "w_exp_down")):
        key = (nm, l)
        if key not in _WCACHE:
            w = inp[src][l]
            if nm == "w_down":
                w = w.reshape(64, 2, 2, 128, 2048)
            else:
                w = w.reshape(64, 2, 8, 128, 512)
            _WCACHE[key] = np.ascontiguousarray(w.transpose(1, 0, 3, 2, 4)).reshape(16384, 4096)
        m[nm] = _WCACHE[key]
    m["tri"] = np.triu(np.ones((128, 128), np.float32), 1)
    u = np.triu(np.ones((64, 64), np.float32), 1)
    ui = np.triu(np.ones((64, 64), np.float32), 0)
    m["u64"] = np.ascontiguousarray(np.concatenate([u, ui], axis=1))
    m["jv"] = (np.arange(NB, dtype=np.float32) * 128).reshape(1, NB)
    m["tokid"] = np.ascontiguousarray((np.arange(T // 128)[None, :] * 128 + np.arange(128)[:, None]).astype(np.int32))
    m["iota_gu"] = np.ascontiguousarray((np.arange(16)[None, :] * 128 + np.arange(128)[:, None]).astype(np.float32))
    m["iota_d"] = np.ascontiguousarray((np.arange(4)[None, :] * 128 + np.arange(128)[:, None]).astype(np.float32))
    m["norm_f_g"] = inp["norm_f_g"].reshape(1, -1)
    m["ident_f"] = np.eye(128, dtype=np.float32)
    m["ones_f"] = np.ones((128, 128), np.float32)
    return {k + sfx: v for k, v in m.items()}


def kernel(**inputs):
    inp = {k: np.asarray(v) for k, v in inputs.items()}
    _WCACHE.clear()
    B = inp["x"].shape[0]
    L0, L1 = build_fused()
    needed = [k + "_0" for k in L0.inputs] + [k + "_1" for k in L1.inputs]
    in_maps = []
    for core in range(8):
        b, half = divmod(core, 2)
        m = prep_core(inp, 0, b, half, own=S, sfx="_0")
        m.update(prep_core(inp, 1, b, half, own=2048, sfx="_1"))
        in_maps.append({k: np.ascontiguousarray(m[k]) for k in needed})
    res = run_bass_kernel_spmd(L0.nc, in_maps, core_ids=list(range(8)))
    out = np.empty((B, S, D), np.float32)
    for core in range(8):
        b, half = divmod(core, 2)
        order = core_order(half)
        out[b][order[:2048]] = np.asarray(res.results[core]["XOL_1"])
    return out
```

```python
import numpy as np
import concourse.bass as bass
import concourse.mybir as mybir
from concourse.bass_utils import run_bass_kernel_spmd

F32 = mybir.dt.float32
BF16 = mybir.dt.bfloat16
I32 = mybir.dt.int32
ALU = mybir.AluOpType
AF = mybir.ActivationFunctionType
AX = mybir.AxisListType

ENGS = ("tensor", "vector", "scalar", "gpsimd", "sync")


class TT:
    def __init__(self, prog, t, name, acc=False):
        self.prog = prog
        self.t = t
        self.name = name
        self.acc = acc
        self.wr = {}
        self.rd = {}
        self.sem_in = None
        self.sem_out = None

    def __getitem__(self, k):
        return self.t[k]

    def ap(self):
        return self.t[:] if not hasattr(self.t, "ap") else self.t.ap()


class _Rec:
    def __init__(self):
        self.call = None

    def __getattr__(self, name):
        def f(*a, **kw):
            self.call = (name, a, kw)
            return self
        return f


def _capture(fn):
    r = _Rec()
    fn(r)
    assert r.call is not None
    return r.call


class Prog:
    def __init__(self, nc, self_wait=True):
        self.nc = nc
        self.q = {e: [] for e in ENGS}
        self.esem = {}
        self.ecnt = {e: 0 for e in ENGS}
        self.waited = {e: {} for e in ENGS}
        self.sems = {}
        self.semcnt = {}
        self.self_wait = self_wait
        self.nsem = 0
        self._ctx = []
        self._semctx = []
        self.free_sems = []
        self._tiles = []
        self.uid = 0
        self.cur_phase = "init"
        self.use_scopes = False
        for e in ENGS:
            self.esem[e] = self.new_sem("e_" + e)

    def new_sem(self, name):
        if name.startswith("d") and self.free_sems:
            return self.free_sems.pop()
        cm = self.nc.semaphore(name + "_%d" % self.nsem)
        s = cm.__enter__()
        self._semctx.append(cm)
        self.nsem += 1
        self.semcnt[id(s)] = 0
        self.sems[id(s)] = s
        return s

    def sb(self, name, shape, dt, acc=False):
        self.uid += 1
        name = "%s_u%d" % (name, self.uid)
        cm = self.nc.sbuf_tensor(name, list(shape), dt)
        t = cm.__enter__()
        self._ctx.append(cm)
        tt = TT(self, t, name, acc)
        self._tiles.append(tt)
        return tt

    def ps(self, name, shape, dt=F32):
        self.uid += 1
        name = "%s_u%d" % (name, self.uid)
        cm = self.nc.psum_tensor(name, list(shape), dt)
        t = cm.__enter__()
        self._ctx.append(cm)
        tt = TT(self, t, name)
        self._tiles.append(tt)
        return tt

    def free_to(self, mark):
        while len(self._ctx) > mark:
            self._ctx.pop().__exit__(None, None, None)
            tt = self._tiles.pop()
            for s in (tt.sem_in, tt.sem_out):
                if s is not None:
                    self.free_sems.append(s)

    def barrier(self):
        cur = {}
        for e in ENGS:
            cur[id(self.esem[e])] = self.ecnt[e]
        for k, v in self.semcnt.items():
            if v > 0 and k not in cur:
                cur[k] = v
        for e in ENGS:
            waits = []
            wd = self.waited[e]
            for k, v in cur.items():
                if v == 0 or wd.get(k, 0) >= v:
                    continue
                wd[k] = v
                waits.append((self.sems[k], v))
            self.q[e].append((waits, None, None, 0, self.cur_phase))

    def dram(self, name, shape, dt, kind="Internal", acc=True, addr_space="Local"):
        t = self.nc.dram_tensor(name, list(shape), dt, kind=kind, addr_space=addr_space)
        return TT(self, t, name, acc)

    def _deps(self, eng, reads, writes):
        deps = {}

        def add(d):
            for k, v in d.items():
                if deps.get(k, 0) < v:
                    deps[k] = v

        for r in reads:
            add(r.wr)
        for w in writes:
            add(w.rd)
            if not w.acc:
                add(w.wr)
        out = []
        wd = self.waited[eng]
        own = id(self.esem[eng])
        for k, v in deps.items():
            if k == own and (not self.self_wait or eng == "tensor"):
                continue
            if wd.get(k, 0) >= v:
                continue
            wd[k] = v
            out.append((self.sems[k], v))
        return out

    def _post(self, ev, reads, writes):
        k, v = ev
        for r in reads:
            if r.rd.get(k, 0) < v:
                r.rd[k] = v
        for w in writes:
            if w.acc:
                if w.wr.get(k, 0) < v:
                    w.wr[k] = v
            else:
                w.wr = {k: v}
                w.rd = {}

    def op(self, eng, fn, reads=(), writes=()):
        waits = self._deps(eng, reads, writes)
        self.ecnt[eng] += 1
        n = self.ecnt[eng]
        s = self.esem[eng]
        self.q[eng].append((waits, _capture(fn), s, 1, self.cur_phase))
        self._post((id(s), n), reads, writes)

    def dma(self, eng, fn, reads=(), writes=(), sb=None, into=True):
        waits = self._deps(eng, reads, writes)
        if into:
            if sb.sem_in is None:
                sb.sem_in = self.new_sem("di_" + sb.name)
            s = sb.sem_in
        else:
            if sb.sem_out is None:
                sb.sem_out = self.new_sem("do_" + sb.name)
            s = sb.sem_out
        self.semcnt[id(s)] += 16
        v = self.semcnt[id(s)]
        self.q[eng].append((waits, _capture(fn), s, 16, self.cur_phase))
        self._post((id(s), v), reads, writes)

    def dma_fn(self, eng, emit_fn, reads=(), writes=(), sb=None, into=True):
        waits = self._deps(eng, reads, writes)
        if into:
            if sb.sem_in is None:
                sb.sem_in = self.new_sem("di_" + sb.name)
            s = sb.sem_in
        else:
            if sb.sem_out is None:
                sb.sem_out = self.new_sem("do_" + sb.name)
            s = sb.sem_out
        self.semcnt[id(s)] += 16
        v = self.semcnt[id(s)]
        self.q[eng].append((waits, ("__fn__", emit_fn), s, 16, self.cur_phase))
        self._post((id(s), v), reads, writes)

    def wait_all(self, eng, tiles):
        waits = self._deps(eng, tiles, ())
        self.q[eng].append((waits, None, None, 0, self.cur_phase))

    def emit(self):
        nc = self.nc
        with nc.Block() as block:
            def mk(e):
                def body(engine):
                    cur = None
                    cm = None
                    for waits, fn, s, inc, ph in self.q[e]:
                        if self.use_scopes and ph != cur:
                            if cm is not None:
                                cm.__exit__(None, None, None)
                            cm = nc.named_scope(ph)
                            cm.__enter__()
                            cur = ph
                        for (ws, wv) in waits:
                            engine.wait_ge(ws, wv)
                        if fn is not None:
                            if fn[0] == "__fn__":
                                fn[1](engine).then_inc(s, inc)
                                continue
                            name, a, kw = fn
                            try:
                                ins = getattr(engine, name)(*a, **kw)
                            except Exception:
                                def _d(x):
                                    try:
                                        return (x.tensor.name, x.shape, str(x.dtype)) if hasattr(x, "shape") else x
                                    except Exception:
                                        return repr(x)[:80]
                                print("EMIT FAIL", e, name, [_d(x) for x in a], {k: _d(v) for k, v in kw.items()}, flush=True)
                                raise
                            ins.then_inc(s, inc)
                    if cm is not None:
                        cm.__exit__(None, None, None)
                return body
            block.sync(mk("sync"))
            block.tensor(mk("tensor"))
            block.vector(mk("vector"))
            block.scalar(mk("scalar"))
            block.gpsimd(mk("gpsimd"))

    def close(self):
        for cm in reversed(self._ctx):
            cm.__exit__(None, None, None)
        for cm in reversed(self._semctx):
            cm.__exit__(None, None, None)
        self._ctx = []


import math
import numpy as np

D = 2048
S = 4096
LC = 256
NT = S + LC
INC = 11776
EPS = 1e-6


class Pool:
    def __init__(self, P, name, shape, dt, n, psum=False):
        self.tiles = [(P.ps if psum else P.sb)(f"{name}{i}", shape, dt) for i in range(n)]
        self.i = 0

    def get(self):
        t = self.tiles[self.i % len(self.tiles)]
        self.i += 1
        return t


def dap(tt, offset, pattern):
    return bass.AP(tensor=tt.t, offset=offset, ap=[list(p) for p in pattern])


class LayerBuilder:
    def __init__(self, layer, taps=(), own=2048, shared=None, sfx="", xa=None, xc=None, ext_out=True):
        self.l = layer
        self.ctx_full = (layer == 0)
        self.taps = set(taps)
        if shared is None:
            self.nc = bass.Bass("TRN2", target_bir_lowering=False)
            self.P = Prog(self.nc)
        else:
            self.nc, self.P = shared
        self.sfx = sfx
        self.xa_over, self.xc_over = xa, xc
        self.ext_out = ext_out
        self.OWN = own
        self.NQ = own + LC
        self.NGL = min(own + 512, S)
        self.NG = self.NGL + LC
        self.NTL = own // 128
        self.inputs = {}
        self.outputs = {}

    def inp(self, name, shape, dt=F32):
        t = self.P.dram(name + self.sfx, shape, dt, kind="ExternalInput")
        self.inputs[name] = t
        return t

    def scratch(self, name, shape, dt, out=False):
        kind = "ExternalOutput" if (out or name in self.taps) else "Internal"
        t = self.P.dram(name + self.sfx, shape, dt, kind=kind)
        if kind == "ExternalOutput":
            self.outputs[name] = t
        return t

    def load(self, dst, dst_ap, src, src_ap, eng="sync", extra_reads=()):
        self.P.dma(eng, lambda en: en.dma_start(out=dst_ap, in_=src_ap), reads=[src, *extra_reads], writes=[dst], sb=dst)

    def store(self, dst, dst_ap, src, src_ap, eng="gpsimd"):
        self.P.dma(eng, lambda en: en.dma_start(out=dst_ap, in_=src_ap), reads=[src], writes=[dst], sb=src, into=False)

    def phase_begin(self):
        import inspect
        self.P.cur_phase = inspect.stack()[1].function + self.sfx
        self._mark = len(self.P._ctx)

    def phase_end(self):
        P = self.P
        P.barrier()
        P.free_to(self._mark)

    def declare(self):
        inp = self.inp
        OWN, NQ, NGL, NG = self.OWN, self.NQ, self.NGL, self.NG
        self.xa = self.xa_over if self.xa_over is not None else inp("xa", [S, D])
        self.xc = self.xc_over if self.xc_over is not None else inp("xc", [LC, D])
        self.ct = inp("ct", [128, 32])
        self.w_mod = inp("w_mod", [D, 6 * D])
        self.b_mod = inp("b_mod", [1, 6 * D])
        self.norm1_g = inp("norm1_g", [1, D])
        self.norm2_g = inp("norm2_g", [1, D])
        self.w_in = inp("w_in", [D, INC])
        self.cos_t = inp("cos_t", [128, S])
        self.sin_t = inp("sin_t", [128, S])
        self.rm = inp("rm", [128, 128])
        self.ident_f = inp("ident_f", [128, 128])
        self.ones_f = inp("ones_f", [128, 128])
        sc = self.scratch
        self.MOD = sc("MOD", [2, 6 * D], F32)
        self.HT = sc("HT", [16, 128, NT], BF16)
        self.QT = sc("QT", [8, 128, NQ], BF16)
        self.KT = sc("KT", [8, 128, NT], BF16)
        self.V = sc("V", [NT, 1024], BF16)
        self.SG = sc("SG", [4, 128, NG], F32)
        self.YG = sc("YG", [4, 128, NG], F32)
        self.GT = sc("GT", [48, 128, NQ], BF16)
        self.U = sc("U", [NT, 1536], F32)
        for nm in ("lam_q1", "lam_k1", "lam_q2", "lam_k2"):
            inp(nm, [1, 64])
        inp("attn_subln_g", [1, 128])
        inp("conf_w", [128, 4 * 31])
        inp("conf_v", [128, 12])
        self.ONT = sc("ONT", [8, 128, NQ], BF16)
        inp("hy_scw", [1, 3 * 1536]); inp("hy_scb", [1, 1536])
        inp("hy_v", [64, 4]); inp("hy_w1", [33, 64]); inp("hy_w2", [64, 64]); inp("hy_w3", [64, 2048])
        inp("hy_dec", [128, 16]); inp("hy_bias", [1, 1024]); inp("jrev", [128, 128])
        inp("emb_lat", [33, 2 * S - 1]); inp("tv_lat", [1, 2 * S - 1])
        inp("emb_ctx", [33, 2 * LC - 1]); inp("tv_ctx", [1, 2 * LC - 1])
        self.HU = sc("HU", [NT, 1536], F32)
        self.H2L = sc("H2L", [64, 2 * S - 1], F32)
        self.H2C = sc("H2C", [64, 2 * LC - 1], F32)
        self.EKL = sc("EKL", [1024, 2 * S], BF16)
        self.EKC = sc("EKC", [1024, 2 * LC], BF16)
        self.ZT = sc("ZT", [4, 128, NQ], BF16)
        inp("w_attn_o", [1024, D]); inp("w_conf_o", [512, D]); inp("w_hy_o", [512, D]); inp("w_out", [D, D])
        self.MG = sc("MG", [16, 128, NQ], BF16)
        self.X1 = sc("X1", [NQ, D], F32)
        if "MOE" in self.taps:
            self.MOE = sc("MOE", [NQ, D], F32)
        self.declare_moe()
        self.CT = sc("CT", [4, 128, NQ], BF16)

    def phase_mod(self):
        P = self.P
        self.phase_begin()
        ct = P.sb("ct_s", [128, 32], F32)
        st = P.sb("st_s", [128, 32], F32)
        self.load(ct, ct[:], self.ct, self.ct.t.ap())
        P.op("scalar", lambda en: en.activation(out=st[:], in_=ct[:], func=AF.Silu), reads=[ct], writes=[st])
        wpool = Pool(P, "wm", [128, 16, 512], F32, 2)
        bpool = Pool(P, "bm", [2, 512], F32, 2)
        opool = Pool(P, "om", [2, 512], F32, 2)
        pp = Pool(P, "pm", [2, 512], F32, 2, psum=True)
        wv = self.w_mod.t.ap().rearrange("(kc p) n -> p kc n", p=128)
        for gidx in range(24):
            w = wpool.get()
            self.load(w, w[:], self.w_mod, wv[:, :, gidx * 512:(gidx + 1) * 512])
            bt = bpool.get()
            self.load(bt, bt[:], self.b_mod, dap(self.b_mod, gidx * 512, [[0, 2], [1, 512]]))
            ps = pp.get()
            for kc in range(16):
                P.op("tensor", lambda en, kc=kc, w=w, ps=ps: en.matmul(ps[:], lhsT=st[:, 2 * kc:2 * kc + 2], rhs=w[:, kc, :],
                                                                      start=(kc == 0), stop=(kc == 15)),
                     reads=[st, w], writes=[ps])
            o = opool.get()
            P.op("vector", lambda en, o=o, ps=ps, bt=bt: en.tensor_tensor(out=o[:], in0=ps[:], in1=bt[:], op=ALU.add),
                 reads=[ps, bt], writes=[o])
            self.store(self.MOD, self.MOD.t.ap()[:, gidx * 512:(gidx + 1) * 512], o, o[:])
        self.phase_end()

    def bcast_mod(self, dst, row, j):
        self.load(dst, dst[:], self.MOD, dap(self.MOD, row * 6 * D + j * D, [[0, 128], [1, D]]))

    def make_AB(self, gain, jsh, jsc, tag):
        P = self.P
        gt = P.sb("gain_" + tag, [128, D], F32)
        self.load(gt, gt[:], gain, dap(gain, 0, [[0, 128], [1, D]]))
        res = {}
        for row in ((0, 1) if True else (0,)):
            A = P.sb(f"A_{tag}{row}", [128, D], F32)
            Bt = P.sb(f"B_{tag}{row}", [128, D], F32)
            self.bcast_mod(A, row, jsc)
            self.bcast_mod(Bt, row, jsh)
            P.op("vector", lambda en, A=A: en.scalar_tensor_tensor(out=A[:], in0=A[:], scalar=1.0, in1=gt[:], op0=ALU.add, op1=ALU.mult),
                 reads=[A, gt], writes=[A])
            res[row] = (A, Bt)
        return res

    def norm_tile(self, xt, A, Bt, out, sq, ss, rstd, tmp):
        P = self.P
        P.op("scalar", lambda en: en.activation(out=sq[:], in_=xt[:], func=AF.Square, accum_out=ss[:]), reads=[xt], writes=[sq, ss])
        P.op("vector", lambda en: en.tensor_scalar(out=rstd[:], in0=ss[:], scalar1=1.0 / D, scalar2=EPS, op0=ALU.mult, op1=ALU.add),
             reads=[ss], writes=[rstd])
        P.op("scalar", lambda en: en.activation(out=rstd[:], in_=rstd[:], func=AF.Sqrt), reads=[rstd], writes=[rstd])
        P.op("vector", lambda en: en.reciprocal(out=rstd[:], in_=rstd[:]), reads=[rstd], writes=[rstd])
        P.op("vector", lambda en: en.scalar_tensor_tensor(out=tmp[:], in0=xt[:], scalar=rstd[:, 0:1], in1=A[:], op0=ALU.mult, op1=ALU.mult),
             reads=[xt, rstd, A], writes=[tmp])
        P.op("vector", lambda en: en.tensor_tensor(out=out[:], in0=tmp[:], in1=Bt[:], op=ALU.add), reads=[tmp, Bt], writes=[out])

    def phase_norm1(self):
        P = self.P
        self.phase_begin()
        AB = self.make_AB(self.norm1_g, 0, 1, "n1")
        ident = P.sb("ident_b", [128, 128], BF16)
        idf = P.sb("ident_fs", [128, 128], F32)
        self.load(idf, idf[:], self.ident_f, self.ident_f.t.ap())
        P.op("vector", lambda en: en.tensor_copy(out=ident[:], in_=idf[:]), reads=[idf], writes=[ident])
        xpool = Pool(P, "xt", [128, D], F32, 2)
        sqp = Pool(P, "sq", [128, D], F32, 1)
        tmpp = Pool(P, "tmp", [128, D], F32, 1)
        hbp = Pool(P, "hb", [128, D], BF16, 2)
        ssp = Pool(P, "ss", [128, 1], F32, 2)
        rsp = Pool(P, "rs", [128, 1], F32, 2)
        htp = Pool(P, "hts", [128, 16, 512], BF16, 2)
        ptp = Pool(P, "ptr", [128, 8, 128], BF16, 2, psum=True)
        htv = self.HT.t.ap().rearrange("kc p t -> p kc t")
        groups = [(g * 512, 512, self.xa, 0) for g in range(8)] + [(S, 256, self.xc, 1)]
        for (t0, n, src, row) in groups:
            hts = htp.get()
            A, Bt = AB[row]
            for i in range(n // 128):
                r0 = (t0 if row == 0 else 0) + i * 128
                xt = xpool.get()
                self.load(xt, xt[:], src, src.t.ap()[r0:r0 + 128, :])
                hb = hbp.get()
                self.norm_tile(xt, A, Bt, hb, sqp.get(), ssp.get(), rsp.get(), tmpp.get())
                for half in range(2):
                    pt = ptp.get()
                    for k8 in range(8):
                        kc = half * 8 + k8
                        P.op("tensor", lambda en, pt=pt, k8=k8, kc=kc, hb=hb: en.transpose(pt[:, k8, :], hb[:, kc * 128:(kc + 1) * 128], ident[:]),
                             reads=[hb, ident], writes=[pt])
                    eng = "scalar" if half == 0 else "vector"
                    if eng == "scalar":
                        P.op("scalar", lambda en, pt=pt, hts=hts, half=half, i=i: en.copy(out=hts[:, half * 8:half * 8 + 8, i * 128:(i + 1) * 128], in_=pt[:]),
                             reads=[pt], writes=[hts])
                    else:
                        P.op("vector", lambda en, pt=pt, hts=hts, half=half, i=i: en.tensor_copy(out=hts[:, half * 8:half * 8 + 8, i * 128:(i + 1) * 128], in_=pt[:]),
                             reads=[pt], writes=[hts])
            self.store(self.HT, htv[:, :, t0:t0 + n], hts, hts[:, :, 0:n])
        self.phase_end()

    def phase_inproj(self):
        OWN, NQ, NGL, NG, NTL = self.OWN, self.NQ, self.NGL, self.NG, self.NTL
        P = self.P
        full = self.ctx_full
        self.phase_begin()
        cos = P.sb("cos_s", [128, S], F32)
        sin = P.sb("sin_s", [128, S], F32)
        rm = P.sb("rm_s", [128, 128], F32)
        self.load(cos, cos[:], self.cos_t, self.cos_t.t.ap())
        self.load(sin, sin[:], self.sin_t, self.sin_t.t.ap())
        self.load(rm, rm[:], self.rm, self.rm.t.ap())
        wst = Pool(P, "wst", [128, 16, 512], F32, 2)
        wbp = Pool(P, "wb", [128, 16, 512], BF16, 2)
        htp = Pool(P, "hti", [128, 16, 512], BF16, 2)
        psp = Pool(P, "pin", [128, 512], F32, 4, psum=True)
        prp = Pool(P, "prr", [128, 512], F32, 2, psum=True)
        qfp = Pool(P, "qf", [128, 512], F32, 2)
        t1p = Pool(P, "t1", [128, 512], F32, 2)
        t2p = Pool(P, "t2", [128, 512], F32, 2)
        obp = Pool(P, "ob", [128, 512], BF16, 3)
        ofp = Pool(P, "of", [128, 512], F32, 3)
        sgp = Pool(P, "sgl", [128, 512], F32, 2)
        wv = self.w_in.t.ap().rearrange("(kc p) n -> p kc n", p=128)
        htv = self.HT.t.ap().rearrange("kc p t -> p kc t")
        lat_all = [(g * 512, 512) for g in range(8)]
        lat_own = [(g * 512, 512) for g in range(OWN // 512)]
        lat_glu = [(g * 512, 512) for g in range(NGL // 512)]
        ctxg = [(S, 256)]
        plan = [("q", 0), ("q", 1), ("k", 2), ("k", 3), ("v", 4), ("v", 5), ("glug", 7), ("glua", 6),
                ("hy", 8), ("hy", 9), ("hy", 10)] + [("gate", 11 + i) for i in range(12)]
        cnt = 0
        for kind, cg in plan:
            ws = wst.get()
            self.load(ws, ws[:], self.w_in, wv[:, :, cg * 512:(cg + 1) * 512])
            wb = wbp.get()
            P.op("gpsimd", lambda en, wb=wb, ws=ws: en.tensor_copy(out=wb[:, 0:8, :], in_=ws[:, 0:8, :]), reads=[ws], writes=[wb])
            P.op("gpsimd", lambda en, wb=wb, ws=ws: en.tensor_copy(out=wb[:, 8:16, :], in_=ws[:, 8:16, :]), reads=[ws, wb], writes=[wb])
            if kind == "q":
                groups = lat_own + (ctxg if full else [])
            elif kind in ("k", "v"):
                groups = lat_all + ctxg
            elif kind in ("glug", "glua"):
                groups = lat_glu + (ctxg if full else [])
            elif kind == "hy":
                groups = lat_all + (ctxg if full else [])
            else:
                groups = lat_own + (ctxg if full else [])
            for (t0, n) in groups:
                isctx = t0 >= S
                ht = htp.get()
                self.load(ht, ht[:, :, 0:n], self.HT, htv[:, :, t0:t0 + n])
                if kind in ("v", "hy"):
                    for i in range(n // 128):
                        ps = psp.get()
                        for kc in range(16):
                            P.op("tensor", lambda en, ps=ps, ht=ht, wb=wb, kc=kc, i=i: en.matmul(ps[:], lhsT=ht[:, kc, i * 128:(i + 1) * 128], rhs=wb[:, kc, :],
                                                                                                 start=(kc == 0), stop=(kc == 15)),
                                 reads=[ht, wb], writes=[ps])
                        r0 = t0 + i * 128
                        cnt += 1
                        eng = "scalar" if cnt % 2 else "vector"
                        if kind == "v":
                            o = obp.get()
                            dst, dst_ap = self.V, self.V.t.ap()[r0:r0 + 128, (cg - 4) * 512:(cg - 3) * 512]
                        else:
                            o = ofp.get()
                            dst, dst_ap = self.U, self.U.t.ap()[r0:r0 + 128, (cg - 8) * 512:(cg - 7) * 512]
                        if eng == "scalar":
                            P.op("scalar", lambda en, o=o, ps=ps: en.copy(out=o[:], in_=ps[:]), reads=[ps], writes=[o])
                        else:
                            P.op("vector", lambda en, o=o, ps=ps: en.tensor_copy(out=o[:], in_=ps[:]), reads=[ps], writes=[o])
                        self.store(dst, dst_ap, o, o[:])
                    continue
                for j in range(4):
                    ps = psp.get()
                    for kc in range(16):
                        P.op("tensor", lambda en, ps=ps, ht=ht, wb=wb, kc=kc, j=j, n=n: en.matmul(ps[:, 0:n], lhsT=wb[:, kc, j * 128:(j + 1) * 128], rhs=ht[:, kc, 0:n],
                                                                                                 start=(kc == 0), stop=(kc == 15)),
                             reads=[ht, wb], writes=[ps])
                    if kind in ("q", "k"):
                        head = (cg % 2) * 4 + j
                        o = obp.get()
                        if isctx:
                            P.op("scalar", lambda en, o=o, ps=ps, n=n: en.copy(out=o[:, 0:n], in_=ps[:, 0:n]), reads=[ps], writes=[o])
                        else:
                            qf = qfp.get()
                            P.op("scalar", lambda en, qf=qf, ps=ps: en.copy(out=qf[:], in_=ps[:]), reads=[ps], writes=[qf])
                            pr = prp.get()
                            P.op("tensor", lambda en, pr=pr, qf=qf: en.matmul(pr[:], lhsT=rm[:], rhs=qf[:], start=True, stop=True),
                                 reads=[rm, qf], writes=[pr])
                            t1 = t1p.get()
                            t2 = t2p.get()
                            P.op("vector", lambda en, t1=t1, qf=qf, t0=t0: en.tensor_tensor(out=t1[:], in0=qf[:], in1=cos[:, t0:t0 + 512], op=ALU.mult),
                                 reads=[qf, cos], writes=[t1])
                            P.op("vector", lambda en, t2=t2, pr=pr, t0=t0: en.tensor_tensor(out=t2[:], in0=pr[:], in1=sin[:, t0:t0 + 512], op=ALU.mult),
                                 reads=[pr, sin], writes=[t2])
                            P.op("vector", lambda en, o=o, t1=t1, t2=t2: en.tensor_tensor(out=o[:], in0=t1[:], in1=t2[:], op=ALU.add),
                                 reads=[t1, t2], writes=[o])
                        if kind == "q":
                            c0 = (OWN + t0 - S) if isctx else t0
                            self.store(self.QT, self.QT.t.ap()[head, :, c0:c0 + n], o, o[:, 0:n])
                        else:
                            self.store(self.KT, self.KT.t.ap()[head, :, t0:t0 + n], o, o[:, 0:n])
                    elif kind == "glug":
                        o = ofp.get()
                        P.op("scalar", lambda en, o=o, ps=ps, n=n: en.activation(out=o[:, 0:n], in_=ps[:, 0:n], func=AF.Sigmoid), reads=[ps], writes=[o])
                        c0 = (NGL + t0 - S) if isctx else t0
                        self.store(self.SG, self.SG.t.ap()[j, :, c0:c0 + n], o, o[:, 0:n])
                    elif kind == "glua":
                        c0 = (NGL + t0 - S) if isctx else t0
                        sg = sgp.get()
                        self.load(sg, sg[:, 0:n], self.SG, self.SG.t.ap()[j, :, c0:c0 + n])
                        o = ofp.get()
                        P.op("vector", lambda en, o=o, ps=ps, sg=sg, n=n: en.tensor_tensor(out=o[:, 0:n], in0=ps[:, 0:n], in1=sg[:, 0:n], op=ALU.mult),
                             reads=[ps, sg], writes=[o])
                        self.store(self.YG, self.YG.t.ap()[j, :, c0:c0 + n], o, o[:, 0:n])
                    else:
                        o = obp.get()
                        P.op("scalar", lambda en, o=o, ps=ps, n=n: en.activation(out=o[:, 0:n], in_=ps[:, 0:n], func=AF.Sigmoid), reads=[ps], writes=[o])
                        c0 = (OWN + t0 - S) if isctx else t0
                        ch = (cg - 11) * 4 + j
                        self.store(self.GT, self.GT.t.ap()[ch, :, c0:c0 + n], o, o[:, 0:n])
        self.phase_end()


def phase_attn(self):
    OWN, NQ, NGL, NG, NTL = self.OWN, self.NQ, self.NGL, self.NG, self.NTL
    P = self.P
    full = self.ctx_full
    lam_init = 0.8 - 0.6 * math.exp(-0.3 * self.l)
    self.phase_begin()
    lt = {}
    for nm in ("lam_q1", "lam_k1", "lam_q2", "lam_k2"):
        t = P.sb(nm + "_s", [128, 64], F32)
        src = self.inputs[nm]
        self.load(t, t[:], src, dap(src, 0, [[0, 128], [1, 64]]))
        lt[nm] = t
    pr1 = P.sb("lpr1", [128, 64], F32)
    pr2 = P.sb("lpr2", [128, 64], F32)
    e1 = P.sb("le1", [128, 1], F32)
    e2 = P.sb("le2", [128, 1], F32)
    nlam = P.sb("nlam", [128, 1], F32)
    P.op("vector", lambda en: en.tensor_tensor(out=pr1[:], in0=lt["lam_q1"][:], in1=lt["lam_k1"][:], op=ALU.mult), reads=[lt["lam_q1"], lt["lam_k1"]], writes=[pr1])
    P.op("vector", lambda en: en.tensor_tensor(out=pr2[:], in0=lt["lam_q2"][:], in1=lt["lam_k2"][:], op=ALU.mult), reads=[lt["lam_q2"], lt["lam_k2"]], writes=[pr2])
    P.op("vector", lambda en: en.reduce_sum(out=e1[:], in_=pr1[:], axis=AX.X), reads=[pr1], writes=[e1])
    P.op("vector", lambda en: en.reduce_sum(out=e2[:], in_=pr2[:], axis=AX.X), reads=[pr2], writes=[e2])
    P.op("scalar", lambda en: en.activation(out=e1[:], in_=e1[:], func=AF.Exp), reads=[e1], writes=[e1])
    P.op("scalar", lambda en: en.activation(out=e2[:], in_=e2[:], func=AF.Exp), reads=[e2], writes=[e2])
    P.op("vector", lambda en: en.scalar_tensor_tensor(out=nlam[:], in0=e2[:], scalar=-lam_init, in1=e1[:], op0=ALU.add, op1=ALU.subtract),
         reads=[e1, e2], writes=[nlam])
    gsub = P.sb("gsub", [128, 1], F32)
    sg_in = self.inputs["attn_subln_g"]
    self.load(gsub, gsub[:], sg_in, dap(sg_in, 0, [[1, 128], [1, 1]]))
    P.op("vector", lambda en: en.tensor_scalar(out=gsub[:], in0=gsub[:], scalar1=(1.0 - lam_init), scalar2=None, op0=ALU.mult), reads=[gsub], writes=[gsub])
    ones_f = P.sb("ones_fs", [128, 128], F32)
    ones_b = P.sb("ones_bs", [128, 128], BF16)
    self.load(ones_f, ones_f[:], self.ones_f, self.ones_f.t.ap())
    P.op("vector", lambda en: en.tensor_copy(out=ones_b[:], in_=ones_f[:]), reads=[ones_f], writes=[ones_b])

    qp = Pool(P, "q_sb", [128, NQ], BF16, 2)
    kp = Pool(P, "k_sb", [128, NT], BF16, 2)
    vp = Pool(P, "v_sb", [128, 34, 128], BF16, 2)
    sp = Pool(P, "s_ps", [128, 512], F32, 3, psum=True)
    O = [P.ps("o_ps%d" % c, [128, 512], F32) for c in range(2)]
    Z = [P.ps("z_ps%d" % c, [128, 512], F32) for c in range(2)]
    ssps = P.ps("ss_ps", [128, 512], F32)
    ep = Pool(P, "e_sb", [128, 512], BF16, 4)
    r1p = Pool(P, "r1", [128, 512], F32, 1)
    t1p = Pool(P, "at1", [128, 512], F32, 1)
    t2p = Pool(P, "at2", [128, 512], F32, 1)
    op_ = Pool(P, "ao", [128, 512], F32, 2)
    sqp = Pool(P, "asq", [128, 512], F32, 1)
    rsp = Pool(P, "ars", [128, 512], F32, 1)
    onp = Pool(P, "aon", [128, 512], BF16, 2)
    vview = self.V.t.ap().rearrange("(kt p) c -> p kt c", p=128)
    for h in range(8):
        q = qp.get(); k = kp.get(); v = vp.get()
        nq = NQ if full else OWN
        self.load(q, q[:, 0:nq], self.QT, self.QT.t.ap()[h, :, 0:nq])
        self.load(k, k[:], self.KT, self.KT.t.ap()[h])
        self.load(v, v[:], self.V, vview[:, :, h * 128:(h + 1) * 128])
        chunks = [(qc * 512, 512, list(range(34))) for qc in range(OWN // 512)]
        if full:
            chunks.append((OWN, 256, [32, 33]))
        for (q0, n, kts) in chunks:
            steps = [(c, kt) for c in range(2) for kt in kts]

            def emit_s(st):
                c, kt = st
                s = sp.get()
                P.op("tensor", lambda en, s=s, c=c, kt=kt: en.matmul(s[:, 0:n], lhsT=k[64 * c:64 * c + 64, kt * 128:(kt + 1) * 128],
                                                                     rhs=q[64 * c:64 * c + 64, q0:q0 + n], start=True, stop=True),
                     reads=[k, q], writes=[s])
                return s
            s_q = [emit_s(steps[0])]
            if len(steps) > 1:
                s_q.append(emit_s(steps[1]))
            for i, (c, kt) in enumerate(steps):
                if i + 2 < len(steps):
                    s_q.append(emit_s(steps[i + 2]))
                s_cur = s_q.pop(0)
                e = ep.get()
                P.op("scalar", lambda en, e=e, s=s_cur: en.activation(out=e[:, 0:n], in_=s[:, 0:n], func=AF.Exp, scale=0.125), reads=[s_cur], writes=[e])
                first = (kt == kts[0]); last = (kt == kts[-1])
                P.op("tensor", lambda en, e=e, c=c, kt=kt, first=first, last=last: en.matmul(O[c][:, 0:n], lhsT=v[:, kt, :], rhs=e[:, 0:n], start=first, stop=last),
                     reads=[v, e], writes=[O[c]])
                P.op("tensor", lambda en, e=e, c=c, first=first, last=last: en.matmul(Z[c][:, 0:n], lhsT=ones_b[:], rhs=e[:, 0:n], start=first, stop=last),
                     reads=[ones_b, e], writes=[Z[c]])
            r1 = r1p.get(); t1 = t1p.get(); t2 = t2p.get(); o = op_.get(); sq = sqp.get(); rs = rsp.get(); on = onp.get()
            P.op("vector", lambda en, r1=r1: en.reciprocal(out=r1[:, 0:n], in_=Z[0][:, 0:n]), reads=[Z[0]], writes=[r1])
            P.op("vector", lambda en, r1=r1, t1=t1: en.tensor_tensor(out=t1[:, 0:n], in0=O[0][:, 0:n], in1=r1[:, 0:n], op=ALU.mult), reads=[O[0], r1], writes=[t1])
            P.op("vector", lambda en, r1=r1: en.reciprocal(out=r1[:, 0:n], in_=Z[1][:, 0:n]), reads=[Z[1], r1], writes=[r1])
            P.op("vector", lambda en, r1=r1, t2=t2: en.tensor_tensor(out=t2[:, 0:n], in0=O[1][:, 0:n], in1=r1[:, 0:n], op=ALU.mult), reads=[O[1], r1], writes=[t2])
            P.op("vector", lambda en, o=o, t1=t1, t2=t2: en.scalar_tensor_tensor(out=o[:, 0:n], in0=t2[:, 0:n], scalar=nlam[:, 0:1], in1=t1[:, 0:n], op0=ALU.mult, op1=ALU.add),
                 reads=[t1, t2, nlam], writes=[o])
            P.op("scalar", lambda en, o=o, sq=sq: en.activation(out=sq[:, 0:n], in_=o[:, 0:n], func=AF.Square), reads=[o], writes=[sq])
            P.op("tensor", lambda en, sq=sq: en.matmul(ssps[:, 0:n], lhsT=ones_f[:], rhs=sq[:, 0:n], start=True, stop=True), reads=[ones_f, sq], writes=[ssps])
            P.op("vector", lambda en, rs=rs: en.tensor_scalar(out=rs[:, 0:n], in0=ssps[:, 0:n], scalar1=1.0 / 128, scalar2=1e-5, op0=ALU.mult, op1=ALU.add), reads=[ssps], writes=[rs])
            P.op("scalar", lambda en, rs=rs: en.activation(out=rs[:, 0:n], in_=rs[:, 0:n], func=AF.Sqrt), reads=[rs], writes=[rs])
            P.op("vector", lambda en, rs=rs: en.reciprocal(out=rs[:, 0:n], in_=rs[:, 0:n]), reads=[rs], writes=[rs])
            P.op("vector", lambda en, on=on, o=o, rs=rs: en.scalar_tensor_tensor(out=on[:, 0:n], in0=o[:, 0:n], scalar=gsub[:, 0:1], in1=rs[:, 0:n], op0=ALU.mult, op1=ALU.mult),
                 reads=[o, rs, gsub], writes=[on])
            self.store(self.ONT, self.ONT.t.ap()[h, :, q0:q0 + n], on, on[:, 0:n])
    self.phase_end()


LayerBuilder.phase_attn = phase_attn


def phase_conf(self):
    OWN, NQ, NGL, NG, NTL = self.OWN, self.NQ, self.NGL, self.NG, self.NTL
    P = self.P
    full = self.ctx_full
    self.phase_begin()
    cw = P.sb("cw", [128, 4, 31], F32)
    cv = P.sb("cv", [128, 3, 4], F32)
    self.load(cw, cw[:], self.inputs["conf_w"], self.inputs["conf_w"].t.ap().rearrange("p (c j) -> p c j", c=4))
    self.load(cv, cv[:], self.inputs["conf_v"], self.inputs["conf_v"].t.ap().rearrange("p (w c) -> p w c", w=3))
    ones_f = P.sb("ones_fc", [128, 128], F32)
    self.load(ones_f, ones_f[:], self.ones_f, self.ones_f.t.ap())
    seqs = [(0, min(OWN + 15, S), OWN, 0)]
    if full:
        seqs.append((NGL, 256, 256, OWN))
    yp = Pool(P, "cy", [128, 15 + OWN + 15 + 15], F32, 2)
    acc = P.sb("cacc", [128, 4, OWN], F32)
    sq = P.sb("csq", [128, 4, 512], F32)
    ps1 = P.ps("cps1", [128, 512], F32)
    ps2 = P.ps("cps2", [128, 512], F32)
    mean = P.sb("cmean", [128, 512], F32)
    msq = P.sb("cmsq", [128, 512], F32)
    var = P.sb("cvar", [128, 512], F32)
    dp = Pool(P, "cd", [128, 512], F32, 2)
    obp = Pool(P, "cob", [128, 512], BF16, 2)
    for (c0, n_in, n_out, o0) in seqs:
        for cc in range(4):
            y = yp.get()
            eng = "vector"
            P.op("gpsimd", lambda en, y=y: en.memset(y[:], 0.0), writes=[y])
            self.load(y, y[:, 15:15 + n_in], self.YG, self.YG.t.ap()[cc, :, c0:c0 + n_in])
            P.op(eng, lambda en, y=y, cc=cc: en.tensor_scalar(out=acc[:, cc, 0:n_out], in0=y[:, 0:n_out], scalar1=cw[:, cc, 0:1], scalar2=cv[:, 0, cc:cc + 1],
                                                             op0=ALU.mult, op1=ALU.add), reads=[y, cw, cv], writes=[acc])
            for j in range(1, 31):
                P.op(eng, lambda en, y=y, cc=cc, j=j: en.scalar_tensor_tensor(out=acc[:, cc, 0:n_out], in0=y[:, j:j + n_out], scalar=cw[:, cc, j:j + 1],
                                                                             in1=acc[:, cc, 0:n_out], op0=ALU.mult, op1=ALU.add),
                     reads=[y, cw, acc], writes=[acc])
        for tg in range((n_out + 511) // 512):
            n = min(512, n_out - tg * 512)
            sl = slice(tg * 512, tg * 512 + n)
            for cc in range(4):
                P.op("tensor", lambda en, cc=cc: en.matmul(ps1[:, 0:n], lhsT=ones_f[:], rhs=acc[:, cc, sl], start=(cc == 0), stop=(cc == 3)),
                     reads=[ones_f, acc], writes=[ps1])
            P.op("scalar", lambda en: en.activation(out=sq[:, :, 0:n], in_=acc[:, :, sl], func=AF.Square), reads=[acc], writes=[sq])
            for cc in range(4):
                P.op("tensor", lambda en, cc=cc: en.matmul(ps2[:, 0:n], lhsT=ones_f[:], rhs=sq[:, cc, 0:n], start=(cc == 0), stop=(cc == 3)),
                     reads=[ones_f, sq], writes=[ps2])
            P.op("scalar", lambda en: en.mul(out=mean[:, 0:n], in_=ps1[:, 0:n], mul=1.0 / 512), reads=[ps1], writes=[mean])
            P.op("vector", lambda en: en.tensor_tensor(out=msq[:, 0:n], in0=mean[:, 0:n], in1=mean[:, 0:n], op=ALU.mult), reads=[mean], writes=[msq])
            P.op("vector", lambda en: en.scalar_tensor_tensor(out=var[:, 0:n], in0=ps2[:, 0:n], scalar=1.0 / 512, in1=msq[:, 0:n], op0=ALU.mult, op1=ALU.subtract),
                 reads=[ps2, msq], writes=[var])
            P.op("vector", lambda en: en.tensor_scalar(out=var[:, 0:n], in0=var[:, 0:n], scalar1=1e-5, scalar2=None, op0=ALU.add), reads=[var], writes=[var])
            P.op("scalar", lambda en: en.activation(out=var[:, 0:n], in_=var[:, 0:n], func=AF.Sqrt), reads=[var], writes=[var])
            P.op("vector", lambda en: en.reciprocal(out=var[:, 0:n], in_=var[:, 0:n]), reads=[var], writes=[var])
            for cc in range(4):
                d = dp.get(); ob = obp.get()
                P.op("vector", lambda en, d=d, cc=cc: en.tensor_tensor(out=d[:, 0:n], in0=acc[:, cc, sl], in1=mean[:, 0:n], op=ALU.subtract), reads=[acc, mean], writes=[d])
                P.op("vector", lambda en, d=d: en.tensor_tensor(out=d[:, 0:n], in0=d[:, 0:n], in1=var[:, 0:n], op=ALU.mult), reads=[d, var], writes=[d])
                P.op("scalar", lambda en, d=d, ob=ob, cc=cc: en.activation(out=ob[:, 0:n], in_=d[:, 0:n], func=AF.Silu, scale=cv[:, 1, cc:cc + 1], bias=cv[:, 2, cc:cc + 1]),
                     reads=[d, cv], writes=[ob])
                self.store(self.CT, self.CT.t.ap()[cc, :, o0 + tg * 512:o0 + tg * 512 + n], ob, ob[:, 0:n])
    self.phase_end()


LayerBuilder.phase_conf = phase_conf


TWO_PI = 2.0 * math.pi


def phase_hy_short(self):
    P = self.P
    full = self.ctx_full
    self.phase_begin()
    W = []
    scw = self.inputs["hy_scw"]
    for j in range(3):
        t = P.sb("hsw%d" % j, [128, 1536], F32)
        self.load(t, t[:], scw, dap(scw, j * 1536, [[0, 128], [1, 1536]]))
        W.append(t)
    Bc = P.sb("hsb", [128, 1536], F32)
    scb = self.inputs["hy_scb"]
    self.load(Bc, Bc[:], scb, dap(scb, 0, [[0, 128], [1, 1536]]))
    pp = Pool(P, "hsp", [128, 1536], F32, 2)
    cp = Pool(P, "hsc", [128, 1536], F32, 2)
    np_ = Pool(P, "hsn", [128, 1536], F32, 2)
    tp = Pool(P, "hst", [128, 1536], F32, 2)
    t2p = Pool(P, "hst2", [128, 1536], F32, 2)
    seqs = [(0, 32)] + ([(S, 2)] if full else [])
    Uv = self.U.t.ap()
    for (r0s, nt) in seqs:
        for i in range(nt):
            r0 = r0s + i * 128
            pv = pp.get(); cu = cp.get(); nx = np_.get(); t = tp.get(); t2 = t2p.get()
            self.load(cu, cu[:], self.U, Uv[r0:r0 + 128, :])
            if i == 0:
                P.op("gpsimd", lambda en, pv=pv: en.memset(pv[:], 0.0), writes=[pv])
                self.load(pv, pv[1:128, :], self.U, Uv[r0:r0 + 127, :])
            else:
                self.load(pv, pv[:], self.U, Uv[r0 - 1:r0 + 127, :])
            if i == nt - 1:
                P.op("gpsimd", lambda en, nx=nx: en.memset(nx[:], 0.0), writes=[nx])
                self.load(nx, nx[0:127, :], self.U, Uv[r0 + 1:r0 + 128, :])
            else:
                self.load(nx, nx[:], self.U, Uv[r0 + 1:r0 + 129, :])
            P.op("vector", lambda en, t=t, pv=pv: en.tensor_tensor(out=t[:], in0=pv[:], in1=W[0][:], op=ALU.mult), reads=[pv, W[0]], writes=[t])
            P.op("gpsimd", lambda en, t2=t2, cu=cu: en.tensor_tensor(out=t2[:], in0=cu[:], in1=W[1][:], op=ALU.mult), reads=[cu, W[1]], writes=[t2])
            P.op("vector", lambda en, t=t, t2=t2: en.tensor_tensor(out=t[:], in0=t[:], in1=t2[:], op=ALU.add), reads=[t, t2], writes=[t])
            P.op("gpsimd", lambda en, t2=t2, nx=nx: en.tensor_tensor(out=t2[:], in0=nx[:], in1=W[2][:], op=ALU.mult), reads=[nx, W[2], t2], writes=[t2])
            P.op("gpsimd", lambda en, t2=t2: en.tensor_tensor(out=t2[:], in0=t2[:], in1=Bc[:], op=ALU.add), reads=[t2, Bc], writes=[t2])
            P.op("vector", lambda en, t=t, t2=t2: en.tensor_tensor(out=t[:], in0=t[:], in1=t2[:], op=ALU.add), reads=[t, t2], writes=[t])
            self.store(self.HU, self.HU.t.ap()[r0:r0 + 128, :], t, t[:])
    self.phase_end()


def hy_seqs(self):
    OWN, NQ, NGL, NG, NTL = self.OWN, self.NQ, self.NGL, self.NG, self.NTL
    seqs = [dict(L=S, NA=32, NO=OWN // 128, r0=0, o0=0, emb=self.inputs["emb_lat"], tv=self.inputs["tv_lat"], EK=self.EKL, H2D=self.H2L)]
    if self.ctx_full:
        seqs.append(dict(L=LC, NA=2, NO=2, r0=S, o0=OWN, emb=self.inputs["emb_ctx"], tv=self.inputs["tv_ctx"], EK=self.EKC, H2D=self.H2C))
    return seqs


def phase_hy_mlp(self):
    P = self.P
    self.phase_begin()
    hyv = P.sb("hyv", [64, 4], F32)
    self.load(hyv, hyv[:], self.inputs["hy_v"], self.inputs["hy_v"].t.ap())
    w1 = P.sb("hw1", [33, 64], F32)
    w2 = P.sb("hw2", [64, 64], F32)
    self.load(w1, w1[:], self.inputs["hy_w1"], self.inputs["hy_w1"].t.ap())
    self.load(w2, w2[:], self.inputs["hy_w2"], self.inputs["hy_w2"].t.ap())
    cs = P.sb("hcs", [64, 4], F32)
    for k in range(2):
        P.op("vector", lambda en, k=k: en.tensor_scalar(out=cs[:, 2 * k:2 * k + 1], in0=hyv[:, 2 * k + 1:2 * k + 2], scalar1=1.0 / TWO_PI, scalar2=None, op0=ALU.mult),
             reads=[hyv, cs], writes=[cs])
        P.op("vector", lambda en, k=k: en.tensor_tensor(out=cs[:, 2 * k + 1:2 * k + 2], in0=hyv[:, 2 * k:2 * k + 1], in1=cs[:, 2 * k:2 * k + 1], op=ALU.mult),
             reads=[hyv, cs], writes=[cs])
        P.op("vector", lambda en, k=k: en.tensor_scalar(out=cs[:, 2 * k + 1:2 * k + 2], in0=cs[:, 2 * k + 1:2 * k + 2], scalar1=0.0, scalar2=None, op0=ALU.add),
             reads=[cs], writes=[cs])
    negpi = P.sb("negpi", [64, 1], F32)
    P.op("vector", lambda en: en.memset(negpi[:], -math.pi), writes=[negpi])
    ep = Pool(P, "hemb", [33, 512], F32, 2)
    pp = Pool(P, "hmp", [64, 512], F32, 2, psum=True)
    up = Pool(P, "hmu", [64, 512], F32, 2)
    hp = Pool(P, "hmh", [64, 512], F32, 2)
    uip = Pool(P, "hmui", [64, 512], I32, 2)
    ufp = Pool(P, "hmuf", [64, 512], F32, 2)
    for sq in hy_seqs(self):
        npos = 2 * sq["L"] - 1
        for c0 in range(0, npos, 512):
            n = min(512, npos - c0)
            e = ep.get()
            self.load(e, e[:, 0:n], sq["emb"], sq["emb"].t.ap()[:, c0:c0 + n])
            h = None
            for k, w in enumerate((w1, w2)):
                ps = pp.get()
                rhs = e if k == 0 else h
                P.op("tensor", lambda en, ps=ps, w=w, rhs=rhs: en.matmul(ps[:, 0:n], lhsT=w[:], rhs=rhs[:, 0:n], start=True, stop=True), reads=[w, rhs], writes=[ps])
                u = up.get()
                P.op("vector", lambda en, u=u, ps=ps, k=k: en.tensor_scalar(out=u[:, 0:n], in0=ps[:, 0:n], scalar1=cs[:, 2 * k:2 * k + 1], scalar2=cs[:, 2 * k + 1:2 * k + 2],
                                                                            op0=ALU.mult, op1=ALU.add), reads=[ps, cs], writes=[u])
                ui = uip.get(); uf = ufp.get()
                P.op("vector", lambda en, u=u, ui=ui: en.tensor_copy(out=ui[:, 0:n], in_=u[:, 0:n]), reads=[u], writes=[ui])
                P.op("vector", lambda en, uf=uf, ui=ui: en.tensor_copy(out=uf[:, 0:n], in_=ui[:, 0:n]), reads=[ui], writes=[uf])
                P.op("vector", lambda en, u=u, uf=uf: en.tensor_tensor(out=u[:, 0:n], in0=u[:, 0:n], in1=uf[:, 0:n], op=ALU.subtract), reads=[u, uf], writes=[u])
                P.op("vector", lambda en, u=u, uf=uf: en.tensor_scalar(out=uf[:, 0:n], in0=u[:, 0:n], scalar1=0.5, scalar2=None, op0=ALU.is_gt), reads=[u, uf], writes=[uf])
                P.op("vector", lambda en, u=u, uf=uf: en.tensor_tensor(out=u[:, 0:n], in0=u[:, 0:n], in1=uf[:, 0:n], op=ALU.subtract), reads=[u, uf], writes=[u])
                P.op("vector", lambda en, u=u, uf=uf: en.tensor_scalar(out=uf[:, 0:n], in0=u[:, 0:n], scalar1=-0.5, scalar2=None, op0=ALU.is_lt), reads=[u, uf], writes=[uf])
                P.op("vector", lambda en, u=u, uf=uf: en.tensor_tensor(out=u[:, 0:n], in0=u[:, 0:n], in1=uf[:, 0:n], op=ALU.add), reads=[u, uf], writes=[u])
                h = hp.get()
                P.op("scalar", lambda en, u=u, h=h: en.activation(out=h[:, 0:n], in_=u[:, 0:n], func=AF.Sin, scale=TWO_PI), reads=[u], writes=[h])
            self.store(sq["H2D"], sq["H2D"].t.ap()[:, c0:c0 + n], h, h[:, 0:n])
    self.phase_end()


def phase_hy_filt(self):
    P = self.P
    self.phase_begin()
    w3 = P.sb("hw3", [64, 2048], F32)
    self.load(w3, w3[:], self.inputs["hy_w3"], self.inputs["hy_w3"].t.ap())
    dec = P.sb("hdec", [128, 16], F32)
    self.load(dec, dec[:], self.inputs["hy_dec"], self.inputs["hy_dec"].t.ap())
    P.op("scalar", lambda en: en.activation(out=dec[:], in_=dec[:], func=AF.Abs), reads=[dec], writes=[dec])
    P.op("vector", lambda en: en.tensor_scalar(out=dec[:], in0=dec[:], scalar1=-1.0, scalar2=None, op0=ALU.mult), reads=[dec], writes=[dec])
    h2 = P.sb("hh2", [64, 8191], F32)
    tvb = P.sb("htvb", [128, 8191], F32)
    et = P.sb("het", [128, 8191], F32)
    eb = P.sb("heb", [128, 8191], BF16)
    junk = P.sb("hjunk", [128, 8191], BF16)
    ssum = P.sb("hssum", [128, 1], F32)
    pp = Pool(P, "hfp", [128, 512], F32, 2, psum=True)
    wp = Pool(P, "hfw", [128, 512], F32, 2)
    for sq in hy_seqs(self):
        L = sq["L"]
        npos = 2 * L - 1
        self.load(h2, h2[:, 0:npos], sq["H2D"], sq["H2D"].t.ap())
        self.load(tvb, tvb[:, 0:npos], sq["tv"], dap(sq["tv"], 0, [[0, 128], [1, npos]]))
        chunks = []
        for (a, b, d) in ((0, L - 1, 1), (L - 1, npos, 0)):
            for c0 in range(a, b, 512):
                chunks.append((c0, min(512, b - c0), d))
        for o in range(2):
            for cc in range(4):
                for (c0, n, d) in chunks:
                    col = o * 1024 + d * 512 + cc * 128
                    di = (o * 2 + d) * 4 + cc
                    ps = pp.get()
                    P.op("tensor", lambda en, ps=ps, col=col, c0=c0, n=n: en.matmul(ps[:, 0:n], lhsT=w3[:, col:col + 128], rhs=h2[:, c0:c0 + n], start=True, stop=True),
                         reads=[w3, h2], writes=[ps])
                    w = wp.get()
                    P.op("scalar", lambda en, w=w, c0=c0, n=n, di=di: en.activation(out=w[:, 0:n], in_=tvb[:, c0:c0 + n], func=AF.Exp, scale=dec[:, di:di + 1]),
                         reads=[tvb, dec], writes=[w])
                    P.op("vector", lambda en, ps=ps, w=w, c0=c0, n=n: en.tensor_tensor(out=et[:, c0:c0 + n], in0=ps[:, 0:n], in1=w[:, 0:n], op=ALU.mult),
                         reads=[ps, w], writes=[et])
                P.op("vector", lambda en: en.memset(ssum[:], 0.0), writes=[ssum])
                P.op("scalar", lambda en: en.activation(out=junk[:, 0:npos], in_=et[:, 0:npos], func=AF.Abs, accum_out=ssum[:]), reads=[et, ssum], writes=[junk, ssum])
                P.op("vector", lambda en: en.reciprocal(out=ssum[:], in_=ssum[:]), reads=[ssum], writes=[ssum])
                P.op("vector", lambda en: en.tensor_scalar(out=eb[:, 0:npos], in0=et[:, 0:npos], scalar1=ssum[:, 0:1], scalar2=None, op0=ALU.mult), reads=[et, ssum], writes=[eb])
                row0 = (o * 4 + cc) * 128
                self.store(sq["EK"], sq["EK"].t.ap()[row0:row0 + 128, 0:npos], eb, eb[:, 0:npos])
    self.phase_end()


def phase_hy_conv(self):
    OWN, NQ, NGL, NG, NTL = self.OWN, self.NQ, self.NGL, self.NG, self.NTL
    P = self.P
    self.phase_begin()
    jrev = P.sb("jrev", [128, 128], F32)
    self.load(jrev, jrev[:], self.inputs["jrev"], self.inputs["jrev"].t.ap())
    idf = P.sb("hidf", [128, 128], F32)
    self.load(idf, idf[:], self.ident_f, self.ident_f.t.ap())
    hb = self.inputs["hy_bias"]
    B1 = P.sb("hB1", [128, 1, 512], F32)
    B2 = P.sb("hB2", [128, 512, 1], F32)
    self.load(B1, B1[:, 0, :], hb, dap(hb, 0, [[0, 128], [1, 512]]))
    self.load(B2, B2[:, :, 0], hb, dap(hb, 512, [[0, 128], [1, 512]]))
    NAM = 32
    VV = P.sb("hVV", [128, NAM, 128], F32)
    X1 = P.sb("hX1", [128, NAM, 128], F32)
    NOM = OWN // 128
    X2 = P.sb("hX2", [128, NOM, 128], F32)
    Zp = P.sb("hZp", [128, 128, 3 * NAM - 2], BF16)
    Y1 = P.sb("hY1", [128, 128, NAM], F32)
    Y2 = P.sb("hY2", [128, 128, NOM], F32)
    Z2 = X1
    T = P.sb("hT", [128, NAM, 128], F32)
    ZTs = P.sb("hZTs", [128, NOM * 128], BF16)
    bandp = Pool(P, "hband", [128, 63 * 128], BF16, 3)
    jp = Pool(P, "hjp", [128, 512], F32, 2, psum=True)
    yp = Pool(P, "hyp", [128, 16, 32], F32, 2, psum=True)
    tpp = Pool(P, "htp", [128, 4, 128], F32, 2, psum=True)
    HUv = self.HU.t.ap()
    for sq in hy_seqs(self):
        NA, NO, r0, o0, EK = sq["NA"], sq["NO"], sq["r0"], sq["o0"], sq["EK"]
        PW = 3 * NA - 2
        BW = (2 * NA - 1) * 128
        npos = 2 * sq["L"] - 1
        EW = EK.t.ap().shape[1]
        hu3 = HUv[r0:r0 + NA * 128, :].rearrange("(a p) c -> p a c", p=128)
        for cc in range(4):
            self.load(X1, X1[:, 0:NA, :], self.HU, hu3[:, :, cc * 128:(cc + 1) * 128])
            self.load(X2, X2[:, 0:NO, :], self.HU, hu3[:, 0:NO, 512 + cc * 128:512 + (cc + 1) * 128])
            self.load(VV, VV[:, 0:NA, :], self.HU, hu3[:, :, 1024 + cc * 128:1024 + (cc + 1) * 128])
            for order in range(2):
                nout = NA if order == 0 else NO
                P.op("gpsimd", lambda en: en.memset(Zp[:], 0.0), writes=[Zp])
                if order == 0:
                    ab = 4 if NA >= 4 else NA
                    for a0 in range(0, NA, ab):
                        ps = jp.get()
                        P.op("tensor", lambda en, ps=ps, a0=a0, ab=ab: en.matmul(ps[:, 0:ab * 128], lhsT=jrev[:], rhs=VV[:, a0:a0 + ab, :], start=True, stop=True),
                             reads=[jrev, VV], writes=[ps])
                        P.op("scalar", lambda en, ps=ps, a0=a0, ab=ab, NA=NA: en.copy(out=Zp[:, :, NA - 1 + a0:NA - 1 + a0 + ab].rearrange("p c a -> p a c"),
                                                                                   in_=ps[:, 0:ab * 128].rearrange("p (a c) -> p a c", a=ab)),
                             reads=[ps], writes=[Zp])
                else:
                    cb = min(128, 512 // NA)
                    for c0 in range(0, 128, cb):
                        ps = jp.get()
                        P.op("tensor", lambda en, ps=ps, c0=c0, cb=cb, NA=NA: en.matmul(ps[:, 0:cb * NA], lhsT=jrev[:], rhs=Y1[:, c0:c0 + cb, 0:NA], start=True, stop=True),
                             reads=[jrev, Y1], writes=[ps])
                        P.op("scalar", lambda en, ps=ps, c0=c0, cb=cb, NA=NA: en.copy(out=Zp[:, c0:c0 + cb, NA - 1:2 * NA - 1],
                                                                                   in_=ps[:, 0:cb * NA].rearrange("p (c a) -> p c a", c=cb)),
                             reads=[ps], writes=[Zp])
                dmax = NA - 1 if order == 0 else NO - 1
                deltas = list(range(-(NA - 1), dmax + 1))
                Yout = Y1 if order == 0 else Y2
                yps = None
                for c in range(128):
                    band = bandp.get()
                    row = (order * 4 + cc) * 128 + c
                    BWo = (NA + dmax) * 128
                    self.load(band, band[:, 0:BWo], EK, dap(EK, row * EW, [[1, 128], [1, BWo]]))
                    if c % 16 == 0:
                        yps = yp.get()
                    for d in deltas:
                        P.op("tensor", lambda en, yps=yps, band=band, c=c, d=d, NA=NA, nout=nout: en.matmul(
                            yps[:, c % 16, 0:nout], lhsT=band[:, (d + NA - 1) * 128:(d + NA) * 128], rhs=Zp[:, c, NA - 1 - d:NA - 1 - d + nout],
                            start=(d == deltas[0]), stop=(d == deltas[-1])), reads=[band, Zp], writes=[yps])
                    if c % 16 == 15:
                        c0 = c - 15
                        if order == 0:
                            P.op("vector", lambda en, yps=yps, c0=c0, nout=nout: en.tensor_copy(out=Y1[:, c0:c0 + 16, 0:nout], in_=yps[:, :, 0:nout]), reads=[yps], writes=[Y1])
                        else:
                            P.op("vector", lambda en, yps=yps, c0=c0, nout=nout: en.tensor_copy(out=Y2[:, c0:c0 + 16, 0:nout], in_=yps[:, :, 0:nout]), reads=[yps], writes=[Y2])
                if order == 0:
                    P.op("vector", lambda en, NA=NA: en.tensor_tensor(out=T[:, 0:NA, :], in0=VV[:, 0:NA, :], in1=B1[:, :, cc * 128:(cc + 1) * 128].to_broadcast([128, NA, 128]), op=ALU.mult),
                         reads=[VV, B1], writes=[T])
                    P.op("vector", lambda en, NA=NA: en.tensor_tensor(out=Y1[:, :, 0:NA], in0=Y1[:, :, 0:NA], in1=T[:, 0:NA, :].rearrange("p a c -> p c a"), op=ALU.add),
                         reads=[Y1, T], writes=[Y1])
                    P.op("vector", lambda en, NA=NA: en.tensor_tensor(out=Y1[:, :, 0:NA], in0=Y1[:, :, 0:NA], in1=X1[:, 0:NA, :].rearrange("p a c -> p c a"), op=ALU.mult),
                         reads=[Y1, X1], writes=[Y1])
                else:
                    P.op("vector", lambda en, NO=NO: en.tensor_tensor(out=T[:, 0:NO, :].rearrange("p a c -> p c a"), in0=Y1[:, :, 0:NO],
                                                                      in1=B2[:, cc * 128:(cc + 1) * 128, :].to_broadcast([128, 128, NO]), op=ALU.mult),
                         reads=[Y1, B2], writes=[T])
                    P.op("vector", lambda en, NO=NO: en.tensor_tensor(out=T[:, 0:NO, :].rearrange("p a c -> p c a"), in0=T[:, 0:NO, :].rearrange("p a c -> p c a"),
                                                                      in1=Y2[:, :, 0:NO], op=ALU.add), reads=[T, Y2], writes=[T])
                    P.op("vector", lambda en, NO=NO: en.tensor_tensor(out=Z2[:, 0:NO, :], in0=T[:, 0:NO, :], in1=X2[:, 0:NO, :], op=ALU.mult), reads=[T, X2], writes=[Z2])
            for a0 in range(0, NO, 4):
                ab = min(4, NO - a0)
                tp = tpp.get()
                for a in range(ab):
                    P.op("tensor", lambda en, tp=tp, a=a, a0=a0: en.transpose(tp[:, a, :], Z2[:, a0 + a, :], idf[:]), reads=[Z2, idf], writes=[tp])
                P.op("scalar", lambda en, tp=tp, a0=a0, ab=ab: en.copy(out=ZTs[:, a0 * 128:(a0 + ab) * 128], in_=tp[:, 0:ab, :]), reads=[tp], writes=[ZTs])
            self.store(self.ZT, self.ZT.t.ap()[cc, :, o0:o0 + NO * 128], ZTs, ZTs[:, 0:NO * 128])
    self.phase_end()


LayerBuilder.phase_hy_short = phase_hy_short
LayerBuilder.phase_hy_mlp = phase_hy_mlp
LayerBuilder.phase_hy_filt = phase_hy_filt
LayerBuilder.phase_hy_conv = phase_hy_conv


def phase_merge(self):
    OWN, NQ, NGL, NG, NTL = self.OWN, self.NQ, self.NGL, self.NG, self.NTL
    P = self.P
    full = self.ctx_full
    self.phase_begin()
    wst = Pool(P, "mws", [128, 16, 512], F32, 2)
    wbp = Pool(P, "mwb", [128, 16, 512], BF16, 2)
    inp_ = Pool(P, "min", [128, 16, 512], BF16, 2)
    gp = Pool(P, "mg", [128, 3, 512], BF16, 2)
    pa = Pool(P, "mpa", [128, 512], F32, 2, psum=True)
    pc = Pool(P, "mpc", [128, 512], F32, 2, psum=True)
    ph = Pool(P, "mph", [128, 512], F32, 2, psum=True)
    mp = Pool(P, "mm", [128, 512], F32, 2)
    tp = Pool(P, "mt", [128, 512], F32, 2)
    obp = Pool(P, "mob", [128, 512], BF16, 2)
    wa = self.inputs["w_attn_o"].t.ap().rearrange("(k p) n -> p k n", p=128)
    wc = self.inputs["w_conf_o"].t.ap().rearrange("(k p) n -> p k n", p=128)
    wh = self.inputs["w_hy_o"].t.ap().rearrange("(k p) n -> p k n", p=128)
    groups = [(g * 512, 512) for g in range(OWN // 512)] + ([(OWN, 256)] if full else [])
    gtv = self.GT.t.ap().rearrange("(b d) p t -> b d p t", b=3)
    for dg in range(4):
        ws = wst.get()
        self.load(ws, ws[:, 0:8, :], self.inputs["w_attn_o"], wa[:, :, dg * 512:(dg + 1) * 512])
        self.load(ws, ws[:, 8:12, :], self.inputs["w_conf_o"], wc[:, :, dg * 512:(dg + 1) * 512])
        self.load(ws, ws[:, 12:16, :], self.inputs["w_hy_o"], wh[:, :, dg * 512:(dg + 1) * 512])
        wb = wbp.get()
        P.op("gpsimd", lambda en, wb=wb, ws=ws: en.tensor_copy(out=wb[:, 0:8, :], in_=ws[:, 0:8, :]), reads=[ws], writes=[wb])
        P.op("gpsimd", lambda en, wb=wb, ws=ws: en.tensor_copy(out=wb[:, 8:16, :], in_=ws[:, 8:16, :]), reads=[ws, wb], writes=[wb])
        for (t0, n) in groups:
            x = inp_.get()
            self.load(x, x[:, 0:8, 0:n], self.ONT, self.ONT.t.ap().rearrange("h p t -> p h t")[:, :, t0:t0 + n])
            self.load(x, x[:, 8:12, 0:n], self.CT, self.CT.t.ap().rearrange("h p t -> p h t")[:, :, t0:t0 + n])
            self.load(x, x[:, 12:16, 0:n], self.ZT, self.ZT.t.ap().rearrange("h p t -> p h t")[:, :, t0:t0 + n])
            for j in range(4):
                dch = dg * 4 + j
                g = gp.get()
                self.load(g, g[:, :, 0:n], self.GT, gtv[:, dch].rearrange("b p t -> p b t")[:, :, t0:t0 + n])
                pss = []
                for (pool, k0, k1) in ((pa, 0, 8), (pc, 8, 12), (ph, 12, 16)):
                    ps = pool.get()
                    for k in range(k0, k1):
                        P.op("tensor", lambda en, ps=ps, wb=wb, x=x, k=k, j=j, k0=k0, k1=k1: en.matmul(ps[:, 0:n], lhsT=wb[:, k, j * 128:(j + 1) * 128], rhs=x[:, k, 0:n],
                                                                                                   start=(k == k0), stop=(k == k1 - 1)), reads=[wb, x], writes=[ps])
                    pss.append(ps)
                m = mp.get(); t = tp.get(); ob = obp.get()
                P.op("vector", lambda en, m=m, g=g, ps=pss[0]: en.tensor_tensor(out=m[:, 0:n], in0=ps[:, 0:n], in1=g[:, 0, 0:n], op=ALU.mult), reads=[pss[0], g], writes=[m])
                P.op("vector", lambda en, t=t, g=g, ps=pss[1]: en.tensor_tensor(out=t[:, 0:n], in0=ps[:, 0:n], in1=g[:, 1, 0:n], op=ALU.mult), reads=[pss[1], g], writes=[t])
                P.op("gpsimd", lambda en, m=m, t=t: en.tensor_tensor(out=m[:, 0:n], in0=m[:, 0:n], in1=t[:, 0:n], op=ALU.add), reads=[m, t], writes=[m])
                P.op("vector", lambda en, t=t, g=g, ps=pss[2]: en.tensor_tensor(out=t[:, 0:n], in0=ps[:, 0:n], in1=g[:, 2, 0:n], op=ALU.mult), reads=[pss[2], g, t], writes=[t])
                P.op("gpsimd", lambda en, m=m, t=t, ob=ob: en.tensor_tensor(out=ob[:, 0:n], in0=m[:, 0:n], in1=t[:, 0:n], op=ALU.add), reads=[m, t], writes=[ob])
                self.store(self.MG, self.MG.t.ap()[dch, :, t0:t0 + n], ob, ob[:, 0:n])
    self.phase_end()


def phase_wout(self):
    OWN, NQ, NGL, NG, NTL = self.OWN, self.NQ, self.NGL, self.NG, self.NTL
    P = self.P
    full = self.ctx_full
    self.phase_begin()
    wst = Pool(P, "ows", [128, 16, 512], F32, 2)
    wbp = Pool(P, "owb", [128, 16, 512], BF16, 2)
    mgp = Pool(P, "omg", [128, 16, 128], BF16, 3)
    xp = Pool(P, "ox", [128, 512], F32, 3)
    pp = Pool(P, "ops", [128, 512], F32, 3, psum=True)
    tp = Pool(P, "ot", [128, 512], F32, 2)
    G1 = [P.sb("oG1_%d" % r, [128, D], F32) for r in range(2)]
    for r in range(2):
        self.bcast_mod(G1[r], r, 2)
    wo = self.inputs["w_out"].t.ap().rearrange("(k p) n -> p k n", p=128)
    mgv = self.MG.t.ap().rearrange("k p t -> p k t")
    ntile = NTL + (2 if full else 0)
    for cg in range(4):
        ws = wst.get()
        self.load(ws, ws[:], self.inputs["w_out"], wo[:, :, cg * 512:(cg + 1) * 512])
        wb = wbp.get()
        P.op("gpsimd", lambda en, wb=wb, ws=ws: en.tensor_copy(out=wb[:, 0:8, :], in_=ws[:, 0:8, :]), reads=[ws], writes=[wb])
        P.op("gpsimd", lambda en, wb=wb, ws=ws: en.tensor_copy(out=wb[:, 8:16, :], in_=ws[:, 8:16, :]), reads=[ws, wb], writes=[wb])
        for i in range(ntile):
            row = 0 if i < NTL else 1
            mg = mgp.get()
            self.load(mg, mg[:], self.MG, mgv[:, :, i * 128:(i + 1) * 128])
            x = xp.get()
            if row == 0:
                self.load(x, x[:], self.xa, self.xa.t.ap()[i * 128:(i + 1) * 128, cg * 512:(cg + 1) * 512])
            else:
                self.load(x, x[:], self.xc, self.xc.t.ap()[(i - NTL) * 128:(i - NTL + 1) * 128, cg * 512:(cg + 1) * 512])
            ps = pp.get()
            for k in range(16):
                P.op("tensor", lambda en, ps=ps, mg=mg, wb=wb, k=k: en.matmul(ps[:], lhsT=mg[:, k, :], rhs=wb[:, k, :], start=(k == 0), stop=(k == 15)), reads=[mg, wb], writes=[ps])
            t = tp.get()
            P.op("vector", lambda en, t=t, ps=ps, row=row: en.tensor_tensor(out=t[:], in0=ps[:], in1=G1[row][:, cg * 512:(cg + 1) * 512], op=ALU.mult), reads=[ps, G1[row]], writes=[t])
            P.op("gpsimd", lambda en, t=t, x=x: en.tensor_tensor(out=t[:], in0=t[:], in1=x[:], op=ALU.add), reads=[t, x], writes=[t])
            self.store(self.X1, self.X1.t.ap()[i * 128:(i + 1) * 128, cg * 512:(cg + 1) * 512], t, t[:])
    self.phase_end()


LayerBuilder.phase_merge = phase_merge
LayerBuilder.phase_wout = phase_wout


def moe_dims(self):
    OWN, NQ, NGL, NG, NTL = self.OWN, self.NQ, self.NGL, self.NG, self.NTL
    T = NQ if self.ctx_full else OWN
    ntile = T // 128
    NB = (2 * T) // 128 + 64
    return T, ntile, NB


def declare_moe(self):
    OWN, NQ, NGL, NG, NTL = self.OWN, self.NQ, self.NGL, self.NG, self.NTL
    inp = self.inp
    T, ntile, NB = moe_dims(self)
    inp("w_r", [D, 72])
    inp("w_gate", [32768, 2048]); inp("w_up", [32768, 2048]); inp("w_down", [32768, 2048])
    inp("tri", [128, 128]); inp("u64", [64, 128])
    inp("jv", [1, NB]); inp("tokid", [128, ntile], I32); inp("iota_gu", [128, 16]); inp("iota_d", [128, 4])
    inp("norm_f_g", [1, D])
    sc = self.scratch
    self.H2 = sc("H2", [T + 128, D], F32)
    self.BT = sc("BT", [NB * 128, 1], I32)
    self.Y = sc("Y", [NB * 128, D], F32)
    self.XOL = sc("XOL", [OWN, D], F32, out=self.ext_out)
    self.XOC = sc("XOC", [LC, D], F32, out=(self.ext_out and self.ctx_full))
    P = self.P
    self.d1 = P.sb("pd1", [128, ntile], I32)
    self.d2 = P.sb("pd2", [128, ntile], I32)
    self.w1 = P.sb("pw1", [128, ntile], F32)
    self.w2 = P.sb("pw2", [128, ntile], F32)
    self.be = P.sb("pbe", [128, NB], F32)


def phase_router(self):
    OWN, NQ, NGL, NG, NTL = self.OWN, self.NQ, self.NGL, self.NG, self.NTL
    P = self.P
    T, ntile, NB = moe_dims(self)
    self.phase_begin()
    AB = self.make_AB(self.norm2_g, 3, 4, "n2")
    idf = P.sb("ridf", [128, 128], F32)
    self.load(idf, idf[:], self.ident_f, self.ident_f.t.ap())
    ones = P.sb("rones", [128, 128], F32)
    self.load(ones, ones[:], self.ones_f, self.ones_f.t.ap())
    tri = P.sb("rtri", [128, 128], F32)
    self.load(tri, tri[:], self.inputs["tri"], self.inputs["tri"].t.ap())
    u64 = P.sb("ru64", [64, 128], F32)
    self.load(u64, u64[:], self.inputs["u64"], self.inputs["u64"].t.ap())
    wr = P.sb("rwr", [128, 16, 72], F32)
    self.load(wr, wr[:], self.inputs["w_r"], self.inputs["w_r"].t.ap().rearrange("(k p) n -> p k n", p=128))
    LG = P.sb("rLG", [128, ntile, 72], F32)
    xp = Pool(P, "rx", [128, D], F32, 2)
    sqp = Pool(P, "rsq", [128, D], F32, 1)
    tmpp = Pool(P, "rtmp", [128, D], F32, 1)
    hp = Pool(P, "rh", [128, D], F32, 2)
    ssp = Pool(P, "rss", [128, 1], F32, 2)
    rsp = Pool(P, "rrs", [128, 1], F32, 2)
    htp = Pool(P, "rht", [128, 16, 128], F32, 2)
    ptp = Pool(P, "rpt", [128, 4, 128], F32, 2, psum=True)
    plp = Pool(P, "rpl", [128, 72], F32, 2, psum=True)
    zero = P.sb("rzero", [128, D], F32)
    P.op("gpsimd", lambda en: en.memset(zero[:], 0.0), writes=[zero])
    self.store(self.H2, self.H2.t.ap()[T:T + 128, :], zero, zero[:])
    for i in range(ntile):
        row = 0 if i < NTL else 1
        x = xp.get()
        self.load(x, x[:], self.X1, self.X1.t.ap()[i * 128:(i + 1) * 128, :])
        h = hp.get()
        A, Bt = AB[row]
        self.norm_tile(x, A, Bt, h, sqp.get(), ssp.get(), rsp.get(), tmpp.get())
        self.store(self.H2, self.H2.t.ap()[i * 128:(i + 1) * 128, :], h, h[:])
        ht = htp.get()
        for k4 in range(4):
            pt = ptp.get()
            for a in range(4):
                kc = k4 * 4 + a
                P.op("tensor", lambda en, pt=pt, a=a, kc=kc, h=h: en.transpose(pt[:, a, :], h[:, kc * 128:(kc + 1) * 128], idf[:]), reads=[h, idf], writes=[pt])
            if k4 % 2 == 0:
                P.op("scalar", lambda en, pt=pt, ht=ht, k4=k4: en.copy(out=ht[:, k4 * 4:k4 * 4 + 4, :], in_=pt[:]), reads=[pt], writes=[ht])
            else:
                P.op("vector", lambda en, pt=pt, ht=ht, k4=k4: en.tensor_copy(out=ht[:, k4 * 4:k4 * 4 + 4, :], in_=pt[:]), reads=[pt], writes=[ht])
        pl = plp.get()
        for kc in range(16):
            P.op("tensor", lambda en, pl=pl, ht=ht, kc=kc: en.matmul(pl[:], lhsT=ht[:, kc, :], rhs=wr[:, kc, :], start=(kc == 0), stop=(kc == 15)), reads=[ht, wr], writes=[pl])
        P.op("vector", lambda en, pl=pl, i=i: en.tensor_copy(out=LG[:, i, :], in_=pl[:]), reads=[pl], writes=[LG])
    nt = ntile

    def sbt(name, shape, dt=F32):
        return P.sb(name, shape, dt)
    gmax = sbt("gmax", [128, nt, 1]); gmask = sbt("gmask", [128, nt, 8]); dd = sbt("rdd", [128, nt, 8]); sm = sbt("rsm", [128, nt, 1]); pg = sbt("rpg", [128, nt, 1])
    tmp4 = sbt("rtmp4", [128, nt, 8, 8]); les = sbt("rles", [128, nt, 8]); v1 = sbt("rv1", [128, nt, 1]); v2 = sbt("rv2", [128, nt, 1])
    m1 = sbt("rm1", [128, nt, 8]); m2 = sbt("rm2", [128, nt, 8]); le2 = sbt("rle2", [128, nt, 8]); ex = sbt("rex", [128, nt, 1])
    M1 = sbt("rM1", [128, nt, 64]); M2 = sbt("rM2", [128, nt, 64]); M = sbt("rM", [128, nt, 64])
    V = "vector"
    lg = LG[:, :, 0:8]
    le4 = LG[:, :, 8:72].rearrange("p t (g e) -> p t g e", g=8)
    P.op(V, lambda en: en.tensor_reduce(out=gmax[:], in_=lg, axis=AX.X, op=ALU.max), reads=[LG], writes=[gmax])
    P.op(V, lambda en: en.tensor_tensor(out=gmask[:], in0=lg, in1=gmax[:].to_broadcast([128, nt, 8]), op=ALU.is_equal), reads=[LG, gmax], writes=[gmask])
    P.op(V, lambda en: en.tensor_tensor(out=dd[:], in0=lg, in1=gmax[:].to_broadcast([128, nt, 8]), op=ALU.subtract), reads=[LG, gmax], writes=[dd])
    P.op("scalar", lambda en: en.activation(out=dd[:], in_=dd[:], func=AF.Exp), reads=[dd], writes=[dd])
    P.op(V, lambda en: en.tensor_reduce(out=sm[:], in_=dd[:], axis=AX.X, op=ALU.add), reads=[dd], writes=[sm])
    P.op(V, lambda en: en.reciprocal(out=pg[:], in_=sm[:]), reads=[sm], writes=[pg])
    P.op(V, lambda en: en.tensor_tensor(out=tmp4[:], in0=le4, in1=gmask[:].rearrange("p t (g o) -> p t g o", o=1).to_broadcast([128, nt, 8, 8]), op=ALU.mult),
         reads=[LG, gmask], writes=[tmp4])
    P.op(V, lambda en: en.tensor_reduce(out=les[:], in_=tmp4[:].rearrange("p t g e -> p t e g"), axis=AX.X, op=ALU.add), reads=[tmp4], writes=[les])
    P.op(V, lambda en: en.tensor_reduce(out=v1[:], in_=les[:], axis=AX.X, op=ALU.max), reads=[les], writes=[v1])
    P.op(V, lambda en: en.tensor_tensor(out=m1[:], in0=les[:], in1=v1[:].to_broadcast([128, nt, 8]), op=ALU.is_equal), reads=[les, v1], writes=[m1])
    P.op(V, lambda en: en.scalar_tensor_tensor(out=le2[:], in0=m1[:], scalar=-1e30, in1=les[:], op0=ALU.mult, op1=ALU.add), reads=[m1, les], writes=[le2])
    P.op(V, lambda en: en.tensor_reduce(out=v2[:], in_=le2[:], axis=AX.X, op=ALU.max), reads=[le2], writes=[v2])
    P.op(V, lambda en: en.tensor_tensor(out=m2[:], in0=le2[:], in1=v2[:].to_broadcast([128, nt, 8]), op=ALU.is_equal), reads=[le2, v2], writes=[m2])
    P.op(V, lambda en: en.tensor_tensor(out=ex[:], in0=v2[:], in1=v1[:], op=ALU.subtract), reads=[v1, v2], writes=[ex])
    P.op("scalar", lambda en: en.activation(out=ex[:], in_=ex[:], func=AF.Exp), reads=[ex], writes=[ex])
    P.op(V, lambda en: en.tensor_scalar(out=ex[:], in0=ex[:], scalar1=1.0, scalar2=None, op0=ALU.add), reads=[ex], writes=[ex])
    P.op(V, lambda en: en.reciprocal(out=ex[:], in_=ex[:]), reads=[ex], writes=[ex])
    P.op(V, lambda en: en.tensor_tensor(out=self.w1[:], in0=pg[:, :, 0], in1=ex[:, :, 0], op=ALU.mult), reads=[pg, ex], writes=[self.w1])
    P.op(V, lambda en: en.tensor_tensor(out=self.w2[:], in0=pg[:, :, 0], in1=self.w1[:], op=ALU.subtract), reads=[pg, self.w1], writes=[self.w2])
    for (Mk, mk) in ((M1, m1), (M2, m2)):
        P.op(V, lambda en, Mk=Mk, mk=mk: en.tensor_tensor(out=Mk[:].rearrange("p t (g e) -> p t g e", g=8),
                                                          in0=gmask[:].rearrange("p t (g o) -> p t g o", o=1).to_broadcast([128, nt, 8, 8]),
                                                          in1=mk[:].rearrange("p t (o e) -> p t o e", o=1).to_broadcast([128, nt, 8, 8]), op=ALU.mult),
             reads=[gmask, mk], writes=[Mk])
    P.op(V, lambda en: en.tensor_tensor(out=M[:], in0=M1[:], in1=M2[:], op=ALU.add), reads=[M1, M2], writes=[M])
    pcb = P.ps("rpcb", [128, 64], F32)
    pct = P.ps("rpct", [64, 128], F32)
    for i in range(nt):
        P.op("tensor", lambda en, i=i: en.matmul(pcb[:], lhsT=ones[:], rhs=M[:, i, :], start=(i == 0), stop=(i == nt - 1)), reads=[ones, M], writes=[pcb])
    for i in range(nt):
        P.op("tensor", lambda en, i=i: en.matmul(pct[:], lhsT=M[:, i, :], rhs=ones[:], start=(i == 0), stop=(i == nt - 1)), reads=[ones, M], writes=[pct])
    cT = sbt("rcT", [64, 128]); rT = sbt("rrT", [64, 128])
    P.op(V, lambda en: en.tensor_copy(out=cT[:], in_=pct[:]), reads=[pct], writes=[cT])
    qT = sbt("rqT", [64, 128]); qi = sbt("rqi", [64, 128], I32)
    P.op(V, lambda en: en.tensor_scalar(out=qT[:], in0=cT[:], scalar1=1.0 / 128, scalar2=None, op0=ALU.mult), reads=[cT], writes=[qT])
    P.op(V, lambda en: en.tensor_copy(out=qi[:], in_=qT[:]), reads=[qT], writes=[qi])
    P.op(V, lambda en: en.tensor_copy(out=rT[:], in_=qi[:]), reads=[qi], writes=[rT])
    P.op(V, lambda en: en.tensor_tensor(out=qT[:], in0=rT[:], in1=qT[:], op=ALU.is_lt), reads=[rT, qT], writes=[qT])
    P.op(V, lambda en: en.tensor_tensor(out=rT[:], in0=rT[:], in1=qT[:], op=ALU.add), reads=[rT, qT], writes=[rT])
    P.op(V, lambda en: en.tensor_scalar(out=cT[:], in0=rT[:], scalar1=128.0, scalar2=None, op0=ALU.mult), reads=[rT], writes=[cT])
    pst = P.ps("rpst", [128, 128], F32)
    P.op("tensor", lambda en: en.matmul(pst[:], lhsT=cT[:], rhs=u64[:], start=True, stop=True), reads=[cT, u64], writes=[pst])
    pse = sbt("rpse", [128, 128])
    P.op(V, lambda en: en.tensor_copy(out=pse[:], in_=pst[:]), reads=[pst], writes=[pse])
    pcum = Pool(P, "rpcum", [128, 64], F32, 1, psum=True)
    pos = Pool(P, "rpos", [128, 64], F32, 2)
    jk = Pool(P, "rjk", [128, 64], F32, 2)
    d1f = sbt("rd1f", [128, nt]); d2f = sbt("rd2f", [128, nt])
    for i in range(nt):
        pc = pcum.get()
        P.op("tensor", lambda en, pc=pc, i=i: en.matmul(pc[:], lhsT=tri[:], rhs=M[:, i, :], start=True, stop=(i == 0)), reads=[tri, M], writes=[pc])
        for i2 in range(i):
            P.op("tensor", lambda en, pc=pc, i2=i2, i=i: en.matmul(pc[:], lhsT=ones[:], rhs=M[:, i2, :], start=False, stop=(i2 == i - 1)), reads=[ones, M], writes=[pc])
        po = pos.get()
        P.op(V, lambda en, po=po, pc=pc: en.tensor_tensor(out=po[:], in0=pc[:], in1=pse[:, 0:64], op=ALU.add), reads=[pc, pse], writes=[po])
        for (Mk, df) in ((M1, d1f), (M2, d2f)):
            j = jk.get()
            P.op(V, lambda en, j=j, Mk=Mk, po=po, i=i: en.tensor_tensor(out=j[:], in0=Mk[:, i, :], in1=po[:], op=ALU.mult), reads=[Mk, po], writes=[j])
            P.op(V, lambda en, j=j, df=df, i=i: en.tensor_reduce(out=df[:, i:i + 1], in_=j[:], axis=AX.X, op=ALU.add), reads=[j, df], writes=[df])
    P.op(V, lambda en: en.tensor_copy(out=self.d1[:], in_=d1f[:]), reads=[d1f], writes=[self.d1])
    P.op(V, lambda en: en.tensor_copy(out=self.d2[:], in_=d2f[:]), reads=[d2f], writes=[self.d2])
    jv = sbt("rjv", [128, NB, 1])
    self.load(jv, jv[:, :, 0], self.inputs["jv"], dap(self.inputs["jv"], 0, [[0, 128], [1, NB]]))
    JC = 33
    cmp_ = sbt("rcmp", [128, JC, 64])
    bef = sbt("rbef", [128, NB, 1])
    for j0 in range(0, NB, JC):
        jn = min(JC, NB - j0)
        P.op(V, lambda en, j0=j0, jn=jn: en.tensor_tensor(out=cmp_[:, 0:jn, :], in0=pse[:, 64:128].rearrange("p (o e) -> p o e", o=1).to_broadcast([128, jn, 64]),
                                                          in1=jv[:, j0:j0 + jn, :].to_broadcast([128, jn, 64]), op=ALU.is_le), reads=[pse, jv], writes=[cmp_])
        P.op(V, lambda en, j0=j0, jn=jn: en.tensor_reduce(out=bef[:, j0:j0 + jn, :], in_=cmp_[:, 0:jn, :], axis=AX.X, op=ALU.add), reads=[cmp_, bef], writes=[bef])
    P.op(V, lambda en: en.tensor_scalar(out=self.be[:], in0=bef[:, :, 0], scalar1=63.0, scalar2=None, op0=ALU.min), reads=[bef], writes=[self.be])
    bti = P.sb("rbti", [128, NB], I32)
    P.op("gpsimd", lambda en: en.memset(bti[:], T), writes=[bti])
    self.store(self.BT, self.BT.t.ap().rearrange("(p j) o -> p (j o)", p=128), bti, bti[:])
    tok = P.sb("rtok", [128, nt], I32)
    self.load(tok, tok[:], self.inputs["tokid"], self.inputs["tokid"].t.ap())
    BT = self.BT
    for i in range(nt):
        for dk in (self.d1, self.d2):
            P.dma("gpsimd", lambda en, dk=dk, i=i: en.indirect_dma_start(out=BT.t.ap(), out_offset=bass.IndirectOffsetOnAxis(ap=dk[:, i:i + 1], axis=0),
                                                                        in_=tok[:, i:i + 1], in_offset=None),
                  reads=[tok, dk, BT], writes=[BT], sb=tok, into=False)
    self.phase_end()


def phase_experts(self):
    OWN, NQ, NGL, NG, NTL = self.OWN, self.NQ, self.NGL, self.NG, self.NTL
    P = self.P
    T, ntile, NB = moe_dims(self)
    self.phase_begin()
    idf = P.sb("eidf", [128, 128], F32)
    self.load(idf, idf[:], self.ident_f, self.ident_f.t.ap())
    igu = P.sb("eigu", [128, 16], F32)
    self.load(igu, igu[:], self.inputs["iota_gu"], self.inputs["iota_gu"].t.ap())
    be128 = P.sb("ebe128", [128, NB], F32)
    P.op("vector", lambda en: en.tensor_scalar(out=be128[:], in0=self.be[:], scalar1=128.0, scalar2=igu[:, 0:1], op0=ALU.mult, op1=ALU.add), reads=[self.be, igu], writes=[be128])
    idx2 = P.sb("eidx2", [128, NB, 4], F32)
    for qq in range(4):
        P.op("vector", lambda en, qq=qq: en.tensor_scalar(out=idx2[:, :, qq], in0=be128[:], scalar1=8192.0 * qq, scalar2=None, op0=ALU.add), reads=[be128, idx2], writes=[idx2])
    idxi = P.sb("eidxi", [128, NB, 4], I32)
    P.op("vector", lambda en: en.tensor_copy(out=idxi[:], in_=idx2[:]), reads=[idx2], writes=[idxi])
    tokp = Pool(P, "etok", [128, 1], I32, 3)
    xbp = Pool(P, "exb", [128, D], F32, 2)
    xtp = Pool(P, "exT", [128, 16, 128], F32, 2)
    Wg = [P.sb("eWg%d" % k, [128, 2048], F32) for k in range(4)]
    Wu = [P.sb("eWu%d" % k, [128, 2048], F32) for k in range(4)]
    Wd = [P.sb("eWd%d" % k, [128, 2048], F32) for k in range(4)]
    ptp = Pool(P, "ept", [128, 4, 128], F32, 2, psum=True)
    pg = P.ps("epg", [128, 512], F32)
    pu = P.ps("epu", [128, 512], F32)
    py = [P.ps("epy%d" % n, [128, 512], F32) for n in range(4)]
    sgp = Pool(P, "esg", [128, 512], F32, 2)
    acp = Pool(P, "eac", [128, 512], F32, 2)
    atp = Pool(P, "eaT", [128, 4, 128], F32, 2)
    ybp = Pool(P, "eyb", [128, D], F32, 2)
    wg_in, wu_in, wd_in = self.inputs["w_gate"], self.inputs["w_up"], self.inputs["w_down"]
    for j in range(NB):
        tk = tokp.get()
        self.load(tk, tk[:], self.BT, self.BT.t.ap()[j * 128:(j + 1) * 128, :])
        xb = xbp.get()
        H2 = self.H2
        P.dma("gpsimd", lambda en, xb=xb, tk=tk: en.indirect_dma_start(out=xb[:], out_offset=None, in_=H2.t.ap(),
                                                                       in_offset=bass.IndirectOffsetOnAxis(ap=tk[:, 0:1], axis=0)),
              reads=[H2, tk], writes=[xb], sb=xb)
        for hf in range(4):
            for (Wt, win) in ((Wg, wg_in), (Wu, wu_in), (Wd, wd_in)):
                P.dma("gpsimd", lambda en, Wt=Wt, win=win, hf=hf, j=j: en.indirect_dma_start(out=Wt[hf][:], out_offset=None, in_=win.t.ap(),
                                                                                           in_offset=bass.IndirectOffsetOnAxis(ap=idxi[:, j, hf:hf + 1], axis=0)),
                      reads=[win, idxi], writes=[Wt[hf]], sb=Wt[hf])
        xT = xtp.get()
        for k4 in range(4):
            pt = ptp.get()
            for a in range(4):
                kc = k4 * 4 + a
                P.op("tensor", lambda en, pt=pt, a=a, kc=kc, xb=xb: en.transpose(pt[:, a, :], xb[:, kc * 128:(kc + 1) * 128], idf[:]), reads=[xb, idf], writes=[pt])
            if k4 % 2 == 0:
                P.op("scalar", lambda en, pt=pt, xT=xT, k4=k4: en.copy(out=xT[:, k4 * 4:k4 * 4 + 4, :], in_=pt[:]), reads=[pt], writes=[xT])
            else:
                P.op("vector", lambda en, pt=pt, xT=xT, k4=k4: en.tensor_copy(out=xT[:, k4 * 4:k4 * 4 + 4, :], in_=pt[:]), reads=[pt], writes=[xT])
        for (ps_, Wt) in ((pg, Wg), (pu, Wu)):
            for kc in range(16):
                P.op("tensor", lambda en, xT=xT, kc=kc, ps_=ps_, Wt=Wt: en.matmul(ps_[:], lhsT=xT[:, kc, :], rhs=Wt[kc // 4][:, (kc % 4) * 512:(kc % 4 + 1) * 512],
                                                                                 start=(kc == 0), stop=(kc == 15)), reads=[xT, Wt[kc // 4]], writes=[ps_])
        sg = sgp.get(); ac = acp.get()
        P.op("scalar", lambda en, sg=sg: en.activation(out=sg[:], in_=pg[:], func=AF.Silu), reads=[pg], writes=[sg])
        P.op("vector", lambda en, sg=sg, ac=ac: en.tensor_tensor(out=ac[:], in0=pu[:], in1=sg[:], op=ALU.mult), reads=[pu, sg], writes=[ac])
        pt = ptp.get()
        for fc in range(4):
            P.op("tensor", lambda en, pt=pt, fc=fc, ac=ac: en.transpose(pt[:, fc, :], ac[:, fc * 128:(fc + 1) * 128], idf[:]), reads=[ac, idf], writes=[pt])
        aT = atp.get()
        P.op("scalar", lambda en, pt=pt, aT=aT: en.copy(out=aT[:], in_=pt[:]), reads=[pt], writes=[aT])
        yb = ybp.get()
        for n in range(4):
            for fc in range(4):
                P.op("tensor", lambda en, aT=aT, n=n, fc=fc: en.matmul(py[n][:], lhsT=aT[:, fc, :], rhs=Wd[fc][:, n * 512:(n + 1) * 512],
                                                                      start=(fc == 0), stop=(fc == 3)), reads=[aT, Wd[fc]], writes=[py[n]])
            if n % 2 == 0:
                P.op("scalar", lambda en, yb=yb, n=n: en.copy(out=yb[:, n * 512:(n + 1) * 512], in_=py[n][:]), reads=[py[n], yb], writes=[yb])
            else:
                P.op("vector", lambda en, yb=yb, n=n: en.tensor_copy(out=yb[:, n * 512:(n + 1) * 512], in_=py[n][:]), reads=[py[n], yb], writes=[yb])
        self.store(self.Y, self.Y.t.ap()[j * 128:(j + 1) * 128, :], yb, yb[:], eng="sync")
    self.phase_end()


def phase_combine(self):
    OWN, NQ, NGL, NG, NTL = self.OWN, self.NQ, self.NGL, self.NG, self.NTL
    P = self.P
    T, ntile, NB = moe_dims(self)
    final = (self.l == 1)
    self.phase_begin()
    G2 = [P.sb("cG2_%d" % r, [128, D], F32) for r in range(2)]
    for r in range(2):
        self.bcast_mod(G2[r], r, 5)
    if final:
        gf = P.sb("cgf", [128, D], F32)
        nf = self.inputs["norm_f_g"]
        self.load(gf, gf[:], nf, dap(nf, 0, [[0, 128], [1, D]]))
    y1p = Pool(P, "cy1", [128, D], F32, 2)
    y2p = Pool(P, "cy2", [128, D], F32, 2)
    xp = Pool(P, "cx", [128, D], F32, 2)
    mp = Pool(P, "cm", [128, D], F32, 2)
    sqp = Pool(P, "csq2", [128, D], F32, 1)
    ssp = Pool(P, "css", [128, 1], F32, 2)
    Y = self.Y
    for i in range(ntile):
        row = 0 if i < NTL else 1
        y1 = y1p.get(); y2 = y2p.get()
        for (yt, dk) in ((y1, self.d1), (y2, self.d2)):
            P.dma("gpsimd", lambda en, yt=yt, dk=dk, i=i: en.indirect_dma_start(out=yt[:], out_offset=None, in_=Y.t.ap(),
                                                                              in_offset=bass.IndirectOffsetOnAxis(ap=dk[:, i:i + 1], axis=0)),
                  reads=[Y, dk], writes=[yt], sb=yt)
        x = xp.get()
        self.load(x, x[:], self.X1, self.X1.t.ap()[i * 128:(i + 1) * 128, :])
        m = mp.get()
        P.op("vector", lambda en, m=m, y1=y1, i=i: en.tensor_scalar(out=m[:], in0=y1[:], scalar1=self.w1[:, i:i + 1], scalar2=None, op0=ALU.mult), reads=[y1, self.w1], writes=[m])
        P.op("vector", lambda en, m=m, y2=y2, i=i: en.scalar_tensor_tensor(out=m[:], in0=y2[:], scalar=self.w2[:, i:i + 1], in1=m[:], op0=ALU.mult, op1=ALU.add),
             reads=[y2, self.w2, m], writes=[m])
        if "MOE" in self.taps:
            self.store(self.MOE, self.MOE.t.ap()[i * 128:(i + 1) * 128, :], m, m[:])
        P.op("gpsimd", lambda en, m=m, row=row: en.tensor_tensor(out=m[:], in0=m[:], in1=G2[row][:], op=ALU.mult), reads=[m, G2[row]], writes=[m])
        P.op("gpsimd", lambda en, m=m, x=x: en.tensor_tensor(out=m[:], in0=m[:], in1=x[:], op=ALU.add), reads=[m, x], writes=[m])
        if final:
            sq = sqp.get(); ss = ssp.get()
            P.op("scalar", lambda en, sq=sq, ss=ss, m=m: en.activation(out=sq[:], in_=m[:], func=AF.Square, accum_out=ss[:]), reads=[m], writes=[sq, ss])
            P.op("vector", lambda en, ss=ss: en.tensor_scalar(out=ss[:], in0=ss[:], scalar1=1.0 / D, scalar2=EPS, op0=ALU.mult, op1=ALU.add), reads=[ss], writes=[ss])
            P.op("scalar", lambda en, ss=ss: en.activation(out=ss[:], in_=ss[:], func=AF.Sqrt), reads=[ss], writes=[ss])
            P.op("vector", lambda en, ss=ss: en.reciprocal(out=ss[:], in_=ss[:]), reads=[ss], writes=[ss])
            P.op("vector", lambda en, m=m, ss=ss: en.scalar_tensor_tensor(out=m[:], in0=m[:], scalar=ss[:, 0:1], in1=gf[:], op0=ALU.mult, op1=ALU.mult), reads=[m, ss, gf], writes=[m])
        if i < NTL:
            self.store(self.XOL, self.XOL.t.ap()[i * 128:(i + 1) * 128, :], m, m[:], eng="sync")
        else:
            self.store(self.XOC, self.XOC.t.ap()[(i - NTL) * 128:(i - NTL + 1) * 128, :], m, m[:], eng="sync")
    self.phase_end()


LayerBuilder.declare_moe = declare_moe
LayerBuilder.phase_router = phase_router
LayerBuilder.phase_experts = phase_experts
LayerBuilder.phase_combine = phase_combine


ALL_PHASES = ["mod", "norm1", "inproj", "attn", "conf", "hy_short", "hy_mlp", "hy_filt", "hy_conv", "merge", "wout", "router", "experts", "combine"]


def build_layer(layer, taps=(), phases=None, own=2048):
    Bd = LayerBuilder(layer, taps=taps, own=own)
    Bd.declare()
    for ph in (phases or ALL_PHASES):
        getattr(Bd, "phase_" + ph)()
    Bd.P.wait_all("gpsimd", list(Bd.outputs.values()))
    Bd.P.wait_all("sync", list(Bd.outputs.values()))
    Bd.P.emit()
    return Bd


def build_fused():
    L0 = LayerBuilder(0, own=S, sfx="_0", ext_out=False)
    L0.declare()
    for ph in ALL_PHASES:
        getattr(L0, "phase_" + ph)()
    L1 = LayerBuilder(1, own=2048, shared=(L0.nc, L0.P), sfx="_1", xa=L0.XOL, xc=L0.XOC, ext_out=True)
    L1.declare()
    for ph in ALL_PHASES:
        getattr(L1, "phase_" + ph)()
    outs = list(L1.outputs.values())
    L1.P.wait_all("gpsimd", outs)
    L1.P.wait_all("sync", outs)
    L1.P.emit()
    return L0, L1


GRID_W = 64
ROPE_BASE = 10000.0


def core_order(half):
    return np.arange(S) if half == 0 else np.arange(S - 1, -1, -1)


def rope_tables(order):
    rows = (order // GRID_W).astype(np.float32)
    cols = (order % GRID_W).astype(np.float32)
    inv = (np.float32(ROPE_BASE) ** (-np.arange(0, 32, 2, dtype=np.float32) / np.float32(32))).astype(np.float32)
    cos_t = np.zeros((128, S), np.float32)
    sin_t = np.zeros((128, S), np.float32)
    for p in range(128):
        d = p % 64
        pos = rows if d < 32 else cols
        dd = d % 32
        ang = (pos * inv[dd % 16]).astype(np.float32)
        cos_t[p] = np.cos(ang)
        sin_t[p] = -np.sin(ang) if dd < 16 else np.sin(ang)
    return cos_t, sin_t


def rope_perm():
    rm = np.zeros((128, 128), np.float32)
    for m in range(128):
        dd = m % 32
        base = m - dd
        rm[base + (dd + 16) % 32, m] = 1.0
    return rm


def hy_emb_ext(L):
    n = np.arange(L, dtype=np.float32)
    t = (n / np.float32(max(L - 1, 1))).astype(np.float32)
    w = (np.float32(2.0 * np.pi) * n / np.float32(L)).astype(np.float32)
    f = np.linspace(1e-4, 15, 16, dtype=np.float32)
    fw = (w[:, None] * f[None, :]).astype(np.float32)
    emb = np.concatenate([t[:, None], np.cos(fw), -np.sin(fw)], axis=-1).astype(np.float32)
    idx = np.abs(np.arange(2 * L - 1) - (L - 1))
    return np.ascontiguousarray(emb[idx].T), np.ascontiguousarray(t[idx].reshape(1, -1))


_WCACHE = {}


def prep_core(inp, l, b, half, xl=None, xc=None, own=2048, sfx=""):
    order = core_order(half)
    xl = inp["x"] if xl is None else xl
    xc = inp["ctx"] if xc is None else xc
    m = {}
    m["xa"] = np.ascontiguousarray(xl[b][order])
    m["xc"] = np.ascontiguousarray(xc[b] if half == 0 else xc[b][::-1])
    cvec = np.stack([inp["c"][b], inp["c_ctx"]])
    m["ct"] = np.ascontiguousarray(cvec.reshape(2, 16, 128).transpose(2, 1, 0).reshape(128, 32))
    m["w_mod"] = inp["w_mod"][l]
    m["b_mod"] = inp["b_mod"][l].reshape(1, -1)
    m["norm1_g"] = inp["norm1_g"][l].reshape(1, -1)
    m["norm2_g"] = inp["norm2_g"][l].reshape(1, -1)
    m["w_in"] = inp["w_in"][l]
    c, s = rope_tables(order)
    m["cos_t"], m["sin_t"] = c, s
    m["rm"] = rope_perm()
    for nm in ("lam_q1", "lam_k1", "lam_q2", "lam_k2", "attn_subln_g"):
        m[nm] = inp[nm][l].reshape(1, -1)
    cw = inp["conf_dw_w"][l]
    if half == 1:
        cw = cw[::-1]
    m["conf_w"] = np.ascontiguousarray(cw.T.reshape(4, 128, 31).transpose(1, 0, 2).reshape(128, 124))
    cv = np.stack([inp["conf_dw_b"][l], inp["conf_ln_g"][l], inp["conf_ln_b"][l]])
    m["conf_v"] = np.ascontiguousarray(cv.reshape(3, 4, 128).transpose(2, 0, 1).reshape(128, 12))
    scw = inp["hy_sc_w"][l]
    if half == 1:
        scw = scw[::-1]
    m["hy_scw"] = np.ascontiguousarray(scw.reshape(1, -1))
    m["hy_scb"] = inp["hy_sc_b"][l].reshape(1, -1)
    fr = inp["hy_freq"][l]
    m["hy_v"] = np.ascontiguousarray(np.stack([inp["hy_b1"][l], fr[0], inp["hy_b2"][l], fr[1]], axis=1))
    m["hy_w1"] = inp["hy_w1"][l]
    m["hy_w2"] = inp["hy_w2"][l]
    w3 = inp["hy_w3"][l].reshape(64, 2, 2, 512)
    dec = inp["hy_decay"][l].reshape(2, 2, 512)
    if half == 1:
        w3 = w3[:, :, ::-1]
        dec = dec[:, ::-1]
    m["hy_w3"] = np.ascontiguousarray(w3.reshape(64, 2048))
    m["hy_dec"] = np.ascontiguousarray(dec.reshape(2, 2, 4, 128).transpose(3, 0, 1, 2).reshape(128, 16))
    m["hy_bias"] = inp["hy_bias"][l].reshape(1, -1)
    m["jrev"] = np.ascontiguousarray(np.eye(128, dtype=np.float32)[::-1])
    for nm, L in (("lat", S), ("ctx", LC)):
        e, tv = hy_emb_ext(L)
        m["emb_" + nm] = e
        m["tv_" + nm] = tv
    for nm in ("w_attn_o", "w_conf_o", "w_hy_o", "w_out"):
        m[nm] = inp[nm][l]
    T = own + (LC if l == 0 else 0)
    NB = (2 * T) // 128 + 64
    wre = inp["w_router_expert"][l].transpose(1, 0, 2).reshape(D, 64)
    m["w_r"] = np.ascontiguousarray(np.concatenate([inp["w_router_group"][l], wre], axis=1))
    for nm, src in (("w_gate", "w_exp_gate"), ("w_up", "w_exp_up"), ("w_down", "w_exp_down")):
        key = (nm, l)
        if key not in _WCACHE:
            w = inp[src][l]
            if nm == "w_down":
                w = w.reshape(64, 4, 1, 128, 2048)
            else:
                w = w.reshape(64, 4, 4, 128, 512)
            _WCACHE[key] = np.ascontiguousarray(w.transpose(1, 0, 3, 2, 4)).reshape(32768, 2048)
        m[nm] = _WCACHE[key]
    m["tri"] = np.triu(np.ones((128, 128), np.float32), 1)
    u = np.triu(np.ones((64, 64), np.float32), 1)
    ui = np.triu(np.ones((64, 64), np.float32), 0)
    m["u64"] = np.ascontiguousarray(np.concatenate([u, ui], axis=1))
    m["jv"] = (np.arange(NB, dtype=np.float32) * 128).reshape(1, NB)
    m["tokid"] = np.ascontiguousarray((np.arange(T // 128)[None, :] * 128 + np.arange(128)[:, None]).astype(np.int32))
    m["iota_gu"] = np.ascontiguousarray((np.arange(16)[None, :] * 128 + np.arange(128)[:, None]).astype(np.float32))
    m["iota_d"] = np.ascontiguousarray((np.arange(4)[None, :] * 128 + np.arange(128)[:, None]).astype(np.float32))
    m["norm_f_g"] = inp["norm_f_g"].reshape(1, -1)
    m["ident_f"] = np.eye(128, dtype=np.float32)
    m["ones_f"] = np.ones((128, 128), np.float32)
    return {k + sfx: v for k, v in m.items()}


def kernel(**inputs):
    inp = {k: np.asarray(v) for k, v in inputs.items()}
    _WCACHE.clear()
    B = inp["x"].shape[0]
    L0, L1 = build_fused()
    needed = [k + "_0" for k in L0.inputs] + [k + "_1" for k in L1.inputs]
    in_maps = []
    for core in range(8):
        b, half = divmod(core, 2)
        m = prep_core(inp, 0, b, half, own=S, sfx="_0")
        m.update(prep_core(inp, 1, b, half, own=2048, sfx="_1"))
        in_maps.append({k: np.ascontiguousarray(m[k]) for k in needed})
    res = run_bass_kernel_spmd(L0.nc, in_maps, core_ids=list(range(8)))
    out = np.empty((B, S, D), np.float32)
    for core in range(8):
        b, half = divmod(core, 2)
        order = core_order(half)
        out[b][order[:2048]] = np.asarray(res.results[core]["XOL_1"])
    return out
```

```python
import numpy as np
import concourse.bass as bass
import concourse.mybir as mybir
from concourse.bass_utils import run_bass_kernel_spmd

F32 = mybir.dt.float32
BF16 = mybir.dt.bfloat16
I32 = mybir.dt.int32
ALU = mybir.AluOpType
AF = mybir.ActivationFunctionType
AX = mybir.AxisListType

ENGS = ("tensor", "vector", "scalar", "gpsimd", "sync")


class TT:
    def __init__(self, prog, t, name, acc=False):
        self.prog = prog
        self.t = t
        self.name = name
        self.acc = acc
        self.wr = {}
        self.rd = {}
        self.sem_in = None
        self.sem_out = None

    def __getitem__(self, k):
        return self.t[k]

    def ap(self):
        return self.t[:] if not hasattr(self.t, "ap") else self.t.ap()


class _Rec:
    def __init__(self):
        self.call = None

    def __getattr__(self, name):
        def f(*a, **kw):
            self.call = (name, a, kw)
            return self
        return f


def _capture(fn):
    r = _Rec()
    fn(r)
    assert r.call is not None
    return r.call


class Prog:
    def __init__(self, nc, self_wait=True):
        self.nc = nc
        self.q = {e: [] for e in ENGS}
        self.esem = {}
        self.ecnt = {e: 0 for e in ENGS}
        self.waited = {e: {} for e in ENGS}
        self.sems = {}
        self.semcnt = {}
        self.self_wait = self_wait
        self.nsem = 0
        self._ctx = []
        self._semctx = []
        self.free_sems = []
        self._tiles = []
        self.uid = 0
        self.cur_phase = "init"
        self.use_scopes = False
        for e in ENGS:
            self.esem[e] = self.new_sem("e_" + e)

    def new_sem(self, name):
        if name.startswith("d") and self.free_sems:
            return self.free_sems.pop()
        cm = self.nc.semaphore(name + "_%d" % self.nsem)
        s = cm.__enter__()
        self._semctx.append(cm)
        self.nsem += 1
        self.semcnt[id(s)] = 0
        self.sems[id(s)] = s
        return s

    def sb(self, name, shape, dt, acc=False):
        self.uid += 1
        name = "%s_u%d" % (name, self.uid)
        cm = self.nc.sbuf_tensor(name, list(shape), dt)
        t = cm.__enter__()
        self._ctx.append(cm)
        tt = TT(self, t, name, acc)
        self._tiles.append(tt)
        return tt

    def ps(self, name, shape, dt=F32):
        self.uid += 1
        name = "%s_u%d" % (name, self.uid)
        cm = self.nc.psum_tensor(name, list(shape), dt)
        t = cm.__enter__()
        self._ctx.append(cm)
        tt = TT(self, t, name)
        self._tiles.append(tt)
        return tt

    def free_to(self, mark):
        while len(self._ctx) > mark:
            self._ctx.pop().__exit__(None, None, None)
            tt = self._tiles.pop()
            for s in (tt.sem_in, tt.sem_out):
                if s is not None:
                    self.free_sems.append(s)

    def barrier(self):
        cur = {}
        for e in ENGS:
            cur[id(self.esem[e])] = self.ecnt[e]
        for k, v in self.semcnt.items():
            if v > 0 and k not in cur:
                cur[k] = v
        for e in ENGS:
            waits = []
            wd = self.waited[e]
            for k, v in cur.items():
                if v == 0 or wd.get(k, 0) >= v:
                    continue
                wd[k] = v
                waits.append((self.sems[k], v))
            self.q[e].append((waits, None, None, 0, self.cur_phase))

    def dram(self, name, shape, dt, kind="Internal", acc=True, addr_space="Local"):
        t = self.nc.dram_tensor(name, list(shape), dt, kind=kind, addr_space=addr_space)
        return TT(self, t, name, acc)

    def _deps(self, eng, reads, writes):
        deps = {}

        def add(d):
            for k, v in d.items():
                if deps.get(k, 0) < v:
                    deps[k] = v

        for r in reads:
            add(r.wr)
        for w in writes:
            add(w.rd)
            if not w.acc:
                add(w.wr)
        out = []
        wd = self.waited[eng]
        own = id(self.esem[eng])
        for k, v in deps.items():
            if k == own and (not self.self_wait or eng == "tensor"):
                continue
            if wd.get(k, 0) >= v:
                continue
            wd[k] = v
            out.append((self.sems[k], v))
        return out

    def _post(self, ev, reads, writes):
        k, v = ev
        for r in reads:
            if r.rd.get(k, 0) < v:
                r.rd[k] = v
        for w in writes:
            if w.acc:
                if w.wr.get(k, 0) < v:
                    w.wr[k] = v
            else:
                w.wr = {k: v}
                w.rd = {}

    def op(self, eng, fn, reads=(), writes=()):
        waits = self._deps(eng, reads, writes)
        self.ecnt[eng] += 1
        n = self.ecnt[eng]
        s = self.esem[eng]
        self.q[eng].append((waits, _capture(fn), s, 1, self.cur_phase))
        self._post((id(s), n), reads, writes)

    def dma(self, eng, fn, reads=(), writes=(), sb=None, into=True):
        waits = self._deps(eng, reads, writes)
        if into:
            if sb.sem_in is None:
                sb.sem_in = self.new_sem("di_" + sb.name)
            s = sb.sem_in
        else:
            if sb.sem_out is None:
                sb.sem_out = self.new_sem("do_" + sb.name)
            s = sb.sem_out
        self.semcnt[id(s)] += 16
        v = self.semcnt[id(s)]
        self.q[eng].append((waits, _capture(fn), s, 16, self.cur_phase))
        self._post((id(s), v), reads, writes)

    def dma_fn(self, eng, emit_fn, reads=(), writes=(), sb=None, into=True):
        waits = self._deps(eng, reads, writes)
        if into:
            if sb.sem_in is None:
                sb.sem_in = self.new_sem("di_" + sb.name)
            s = sb.sem_in
        else:
            if sb.sem_out is None:
                sb.sem_out = self.new_sem("do_" + sb.name)
            s = sb.sem_out
        self.semcnt[id(s)] += 16
        v = self.semcnt[id(s)]
        self.q[eng].append((waits, ("__fn__", emit_fn), s, 16, self.cur_phase))
        self._post((id(s), v), reads, writes)

    def wait_all(self, eng, tiles):
        waits = self._deps(eng, tiles, ())
        self.q[eng].append((waits, None, None, 0, self.cur_phase))

    def emit(self):
        nc = self.nc
        with nc.Block() as block:
            def mk(e):
                def body(engine):
                    cur = None
                    cm = None
                    for waits, fn, s, inc, ph in self.q[e]:
                        if self.use_scopes and ph != cur:
                            if cm is not None:
                                cm.__exit__(None, None, None)
                            cm = nc.named_scope(ph)
                            cm.__enter__()
                            cur = ph
                        for (ws, wv) in waits:
                            engine.wait_ge(ws, wv)
                        if fn is not None:
                            if fn[0] == "__fn__":
                                fn[1](engine).then_inc(s, inc)
                                continue
                            name, a, kw = fn
                            try:
                                ins = getattr(engine, name)(*a, **kw)
                            except Exception:
                                def _d(x):
                                    try:
                                        return (x.tensor.name, x.shape, str(x.dtype)) if hasattr(x, "shape") else x
                                    except Exception:
                                        return repr(x)[:80]
                                print("EMIT FAIL", e, name, [_d(x) for x in a], {k: _d(v) for k, v in kw.items()}, flush=True)
                                raise
                            ins.then_inc(s, inc)
                    if cm is not None:
                        cm.__exit__(None, None, None)
                return body
            block.sync(mk("sync"))
            block.tensor(mk("tensor"))
            block.vector(mk("vector"))
            block.scalar(mk("scalar"))
            block.gpsimd(mk("gpsimd"))

    def close(self):
        for cm in reversed(self._ctx):
            cm.__exit__(None, None, None)
        for cm in reversed(self._semctx):
            cm.__exit__(None, None, None)
        self._ctx = []


import math
import numpy as np

D = 2048
S = 4096
LC = 256
NT = S + LC
INC = 11776
EPS = 1e-6


class Pool:
    def __init__(self, P, name, shape, dt, n, psum=False):
        self.tiles = [(P.ps if psum else P.sb)(f"{name}{i}", shape, dt) for i in range(n)]
        self.i = 0

    def get(self):
        t = self.tiles[self.i % len(self.tiles)]
        self.i += 1
        return t


def dap(tt, offset, pattern):
    return bass.AP(tensor=tt.t, offset=offset, ap=[list(p) for p in pattern])


class LayerBuilder:
    def __init__(self, layer, taps=(), own=2048, shared=None, sfx="", xa=None, xc=None, ext_out=True):
        self.l = layer
        self.ctx_full = (layer == 0)
        self.taps = set(taps)
        if shared is None:
            self.nc = bass.Bass("TRN2", target_bir_lowering=False)
            self.P = Prog(self.nc)
        else:
            self.nc, self.P = shared
        self.sfx = sfx
        self.xa_over, self.xc_over = xa, xc
        self.ext_out = ext_out
        self.OWN = own
        self.NQ = own + LC
        self.NGL = min(own + 512, S)
        self.NG = self.NGL + LC
        self.NTL = own // 128
        self.inputs = {}
        self.outputs = {}

    def inp(self, name, shape, dt=F32):
        t = self.P.dram(name + self.sfx, shape, dt, kind="ExternalInput")
        self.inputs[name] = t
        return t

    def scratch(self, name, shape, dt, out=False):
        kind = "ExternalOutput" if (out or name in self.taps) else "Internal"
        t = self.P.dram(name + self.sfx, shape, dt, kind=kind)
        if kind == "ExternalOutput":
            self.outputs[name] = t
        return t

    def load(self, dst, dst_ap, src, src_ap, eng="sync", extra_reads=()):
        self.P.dma(eng, lambda en: en.dma_start(out=dst_ap, in_=src_ap), reads=[src, *extra_reads], writes=[dst], sb=dst)

    def store(self, dst, dst_ap, src, src_ap, eng="gpsimd"):
        self.P.dma(eng, lambda en: en.dma_start(out=dst_ap, in_=src_ap), reads=[src], writes=[dst], sb=src, into=False)

    def phase_begin(self):
        import inspect
        self.P.cur_phase = inspect.stack()[1].function + self.sfx
        self._mark = len(self.P._ctx)

    def phase_end(self):
        P = self.P
        P.barrier()
        P.free_to(self._mark)

    def declare(self):
        inp = self.inp
        OWN, NQ, NGL, NG = self.OWN, self.NQ, self.NGL, self.NG
        self.xa = self.xa_over if self.xa_over is not None else inp("xa", [S, D])
        self.xc = self.xc_over if self.xc_over is not None else inp("xc", [LC, D])
        self.ct = inp("ct", [128, 32])
        self.w_mod = inp("w_mod", [D, 6 * D])
        self.b_mod = inp("b_mod", [1, 6 * D])
        self.norm1_g = inp("norm1_g", [1, D])
        self.norm2_g = inp("norm2_g", [1, D])
        self.w_in = inp("w_in", [D, INC])
        self.cos_t = inp("cos_t", [128, S])
        self.sin_t = inp("sin_t", [128, S])
        self.rm = inp("rm", [128, 128])
        self.ident_f = inp("ident_f", [128, 128])
        self.ones_f = inp("ones_f", [128, 128])
        sc = self.scratch
        self.MOD = sc("MOD", [2, 6 * D], F32)
        self.HT = sc("HT", [16, 128, NT], BF16)
        self.QT = sc("QT", [8, 128, NQ], BF16)
        self.KT = sc("KT", [8, 128, NT], BF16)
        self.V = sc("V", [NT, 1024], BF16)
        self.SG = sc("SG", [4, 128, NG], F32)
        self.YG = sc("YG", [4, 128, NG], F32)
        self.GT = sc("GT", [48, 128, NQ], BF16)
        self.U = sc("U", [NT, 1536], F32)
        for nm in ("lam_q1", "lam_k1", "lam_q2", "lam_k2"):
            inp(nm, [1, 64])
        inp("attn_subln_g", [1, 128])
        inp("conf_w", [128, 4 * 31])
        inp("conf_v", [128, 12])
        self.ONT = sc("ONT", [8, 128, NQ], BF16)
        inp("hy_scw", [1, 3 * 1536]); inp("hy_scb", [1, 1536])
        inp("hy_v", [64, 4]); inp("hy_w1", [33, 64]); inp("hy_w2", [64, 64]); inp("hy_w3", [64, 2048])
        inp("hy_dec", [128, 16]); inp("hy_bias", [1, 1024]); inp("jrev", [128, 128])
        inp("emb_lat", [33, 2 * S - 1]); inp("tv_lat", [1, 2 * S - 1])
        inp("emb_ctx", [33, 2 * LC - 1]); inp("tv_ctx", [1, 2 * LC - 1])
        self.HU = sc("HU", [NT, 1536], F32)
        self.H2L = sc("H2L", [64, 2 * S - 1], F32)
        self.H2C = sc("H2C", [64, 2 * LC - 1], F32)
        self.EKL = sc("EKL", [1024, 2 * S], BF16)
        self.EKC = sc("EKC", [1024, 2 * LC], BF16)
        self.ZT = sc("ZT", [4, 128, NQ], BF16)
        inp("w_attn_o", [1024, D]); inp("w_conf_o", [512, D]); inp("w_hy_o", [512, D]); inp("w_out", [D, D])
        self.MG = sc("MG", [16, 128, NQ], BF16)
        self.X1 = sc("X1", [NQ, D], F32)
        if "MOE" in self.taps:
            self.MOE = sc("MOE", [NQ, D], F32)
        self.declare_moe()
        self.CT = sc("CT", [4, 128, NQ], BF16)

    def phase_mod(self):
        P = self.P
        self.phase_begin()
        ct = P.sb("ct_s", [128, 32], F32)
        st = P.sb("st_s", [128, 32], F32)
        self.load(ct, ct[:], self.ct, self.ct.t.ap())
        P.op("scalar", lambda en: en.activation(out=st[:], in_=ct[:], func=AF.Silu), reads=[ct], writes=[st])
        wpool = Pool(P, "wm", [128, 16, 512], F32, 2)
        bpool = Pool(P, "bm", [2, 512], F32, 2)
        opool = Pool(P, "om", [2, 512], F32, 2)
        pp = Pool(P, "pm", [2, 512], F32, 2, psum=True)
        wv = self.w_mod.t.ap().rearrange("(kc p) n -> p kc n", p=128)
        for gidx in range(24):
            w = wpool.get()
            self.load(w, w[:], self.w_mod, wv[:, :, gidx * 512:(gidx + 1) * 512])
            bt = bpool.get()
            self.load(bt, bt[:], self.b_mod, dap(self.b_mod, gidx * 512, [[0, 2], [1, 512]]))
            ps = pp.get()
            for kc in range(16):
                P.op("tensor", lambda en, kc=kc, w=w, ps=ps: en.matmul(ps[:], lhsT=st[:, 2 * kc:2 * kc + 2], rhs=w[:, kc, :],
                                                                      start=(kc == 0), stop=(kc == 15)),
                     reads=[st, w], writes=[ps])
            o = opool.get()
            P.op("vector", lambda en, o=o, ps=ps, bt=bt: en.tensor_tensor(out=o[:], in0=ps[:], in1=bt[:], op=ALU.add),
                 reads=[ps, bt], writes=[o])
            self.store(self.MOD, self.MOD.t.ap()[:, gidx * 512:(gidx + 1) * 512], o, o[:])
        self.phase_end()

    def bcast_mod(self, dst, row, j):
        self.load(dst, dst[:], self.MOD, dap(self.MOD, row * 6 * D + j * D, [[0, 128], [1, D]]))

    def make_AB(self, gain, jsh, jsc, tag):
        P = self.P
        gt = P.sb("gain_" + tag, [128, D], F32)
        self.load(gt, gt[:], gain, dap(gain, 0, [[0, 128], [1, D]]))
        res = {}
        for row in ((0, 1) if True else (0,)):
            A = P.sb(f"A_{tag}{row}", [128, D], F32)
            Bt = P.sb(f"B_{tag}{row}", [128, D], F32)
            self.bcast_mod(A, row, jsc)
            self.bcast_mod(Bt, row, jsh)
            P.op("vector", lambda en, A=A: en.scalar_tensor_tensor(out=A[:], in0=A[:], scalar=1.0, in1=gt[:], op0=ALU.add, op1=ALU.mult),
                 reads=[A, gt], writes=[A])
            res[row] = (A, Bt)
        return res

    def norm_tile(self, xt, A, Bt, out, sq, ss, rstd, tmp):
        P = self.P
        P.op("scalar", lambda en: en.activation(out=sq[:], in_=xt[:], func=AF.Square, accum_out=ss[:]), reads=[xt], writes=[sq, ss])
        P.op("vector", lambda en: en.tensor_scalar(out=rstd[:], in0=ss[:], scalar1=1.0 / D, scalar2=EPS, op0=ALU.mult, op1=ALU.add),
             reads=[ss], writes=[rstd])
        P.op("scalar", lambda en: en.activation(out=rstd[:], in_=rstd[:], func=AF.Sqrt), reads=[rstd], writes=[rstd])
        P.op("vector", lambda en: en.reciprocal(out=rstd[:], in_=rstd[:]), reads=[rstd], writes=[rstd])
        P.op("vector", lambda en: en.scalar_tensor_tensor(out=tmp[:], in0=xt[:], scalar=rstd[:, 0:1], in1=A[:], op0=ALU.mult, op1=ALU.mult),
             reads=[xt, rstd, A], writes=[tmp])
        P.op("vector", lambda en: en.tensor_tensor(out=out[:], in0=tmp[:], in1=Bt[:], op=ALU.add), reads=[tmp, Bt], writes=[out])

    def phase_norm1(self):
        P = self.P
        self.phase_begin()
        AB = self.make_AB(self.norm1_g, 0, 1, "n1")
        ident = P.sb("ident_b", [128, 128], BF16)
        idf = P.sb("ident_fs", [128, 128], F32)
        self.load(idf, idf[:], self.ident_f, self.ident_f.t.ap())
        P.op("vector", lambda en: en.tensor_copy(out=ident[:], in_=idf[:]), reads=[idf], writes=[ident])
        xpool = Pool(P, "xt", [128, D], F32, 2)
        sqp = Pool(P, "sq", [128, D], F32, 1)
        tmpp = Pool(P, "tmp", [128, D], F32, 1)
        hbp = Pool(P, "hb", [128, D], BF16, 2)
        ssp = Pool(P, "ss", [128, 1], F32, 2)
        rsp = Pool(P, "rs", [128, 1], F32, 2)
        htp = Pool(P, "hts", [128, 16, 512], BF16, 2)
        ptp = Pool(P, "ptr", [128, 8, 128], BF16, 2, psum=True)
        htv = self.HT.t.ap().rearrange("kc p t -> p kc t")
        groups = [(g * 512, 512, self.xa, 0) for g in range(8)] + [(S, 256, self.xc, 1)]
        for (t0, n, src, row) in groups:
            hts = htp.get()
            A, Bt = AB[row]
            for i in range(n // 128):
                r0 = (t0 if row == 0 else 0) + i * 128
                xt = xpool.get()
                self.load(xt, xt[:], src, src.t.ap()[r0:r0 + 128, :])
                hb = hbp.get()
                self.norm_tile(xt, A, Bt, hb, sqp.get(), ssp.get(), rsp.get(), tmpp.get())
                for half in range(2):
                    pt = ptp.get()
                    for k8 in range(8):
                        kc = half * 8 + k8
                        P.op("tensor", lambda en, pt=pt, k8=k8, kc=kc, hb=hb: en.transpose(pt[:, k8, :], hb[:, kc * 128:(kc + 1) * 128], ident[:]),
                             reads=[hb, ident], writes=[pt])
                    eng = "scalar" if half == 0 else "vector"
                    if eng == "scalar":
                        P.op("scalar", lambda en, pt=pt, hts=hts, half=half, i=i: en.copy(out=hts[:, half * 8:half * 8 + 8, i * 128:(i + 1) * 128], in_=pt[:]),
                             reads=[pt], writes=[hts])
                    else:
                        P.op("vector", lambda en, pt=pt, hts=hts, half=half, i=i: en.tensor_copy(out=hts[:, half * 8:half * 8 + 8, i * 128:(i + 1) * 128], in_=pt[:]),
                             reads=[pt], writes=[hts])
            self.store(self.HT, htv[:, :, t0:t0 + n], hts, hts[:, :, 0:n])
        self.phase_end()

    def phase_inproj(self):
        OWN, NQ, NGL, NG, NTL = self.OWN, self.NQ, self.NGL, self.NG, self.NTL
        P = self.P
        full = self.ctx_full
        self.phase_begin()
        cos = P.sb("cos_s", [128, S], F32)
        sin = P.sb("sin_s", [128, S], F32)
        rm = P.sb("rm_s", [128, 128], F32)
        self.load(cos, cos[:], self.cos_t, self.cos_t.t.ap())
        self.load(sin, sin[:], self.sin_t, self.sin_t.t.ap())
        self.load(rm, rm[:], self.rm, self.rm.t.ap())
        wst = Pool(P, "wst", [128, 16, 512], F32, 2)
        wbp = Pool(P, "wb", [128, 16, 512], BF16, 2)
        htp = Pool(P, "hti", [128, 16, 512], BF16, 2)
        psp = Pool(P, "pin", [128, 512], F32, 4, psum=True)
        prp = Pool(P, "prr", [128, 512], F32, 2, psum=True)
        qfp = Pool(P, "qf", [128, 512], F32, 2)
        t1p = Pool(P, "t1", [128, 512], F32, 2)
        t2p = Pool(P, "t2", [128, 512], F32, 2)
        obp = Pool(P, "ob", [128, 512], BF16, 3)
        ofp = Pool(P, "of", [128, 512], F32, 3)
        sgp = Pool(P, "sgl", [128, 512], F32, 2)
        wv = self.w_in.t.ap().rearrange("(kc p) n -> p kc n", p=128)
        htv = self.HT.t.ap().rearrange("kc p t -> p kc t")
        lat_all = [(g * 512, 512) for g in range(8)]
        lat_own = [(g * 512, 512) for g in range(OWN // 512)]
        lat_glu = [(g * 512, 512) for g in range(NGL // 512)]
        ctxg = [(S, 256)]
        plan = [("q", 0), ("q", 1), ("k", 2), ("k", 3), ("v", 4), ("v", 5), ("glug", 7), ("glua", 6),
                ("hy", 8), ("hy", 9), ("hy", 10)] + [("gate", 11 + i) for i in range(12)]
        cnt = 0
        for kind, cg in plan:
            ws = wst.get()
            self.load(ws, ws[:], self.w_in, wv[:, :, cg * 512:(cg + 1) * 512])
            wb = wbp.get()
            P.op("gpsimd", lambda en, wb=wb, ws=ws: en.tensor_copy(out=wb[:, 0:8, :], in_=ws[:, 0:8, :]), reads=[ws], writes=[wb])
            P.op("gpsimd", lambda en, wb=wb, ws=ws: en.tensor_copy(out=wb[:, 8:16, :], in_=ws[:, 8:16, :]), reads=[ws, wb], writes=[wb])
            if kind == "q":
                groups = lat_own + (ctxg if full else [])
            elif kind in ("k", "v"):
                groups = lat_all + ctxg
            elif kind in ("glug", "glua"):
                groups = lat_glu + (ctxg if full else [])
            elif kind == "hy":
                groups = lat_all + (ctxg if full else [])
            else:
                groups = lat_own + (ctxg if full else [])
            for (t0, n) in groups:
                isctx = t0 >= S
                ht = htp.get()
                self.load(ht, ht[:, :, 0:n], self.HT, htv[:, :, t0:t0 + n])
                if kind in ("v", "hy"):
                    for i in range(n // 128):
                        ps = psp.get()
                        for kc in range(16):
                            P.op("tensor", lambda en, ps=ps, ht=ht, wb=wb, kc=kc, i=i: en.matmul(ps[:], lhsT=ht[:, kc, i * 128:(i + 1) * 128], rhs=wb[:, kc, :],
                                                                                                 start=(kc == 0), stop=(kc == 15)),
                                 reads=[ht, wb], writes=[ps])
                        r0 = t0 + i * 128
                        cnt += 1
                        eng = "scalar" if cnt % 2 else "vector"
                        if kind == "v":
                            o = obp.get()
                            dst, dst_ap = self.V, self.V.t.ap()[r0:r0 + 128, (cg - 4) * 512:(cg - 3) * 512]
                        else:
                            o = ofp.get()
                            dst, dst_ap = self.U, self.U.t.ap()[r0:r0 + 128, (cg - 8) * 512:(cg - 7) * 512]
                        if eng == "scalar":
                            P.op("scalar", lambda en, o=o, ps=ps: en.copy(out=o[:], in_=ps[:]), reads=[ps], writes=[o])
                        else:
                            P.op("vector", lambda en, o=o, ps=ps: en.tensor_copy(out=o[:], in_=ps[:]), reads=[ps], writes=[o])
                        self.store(dst, dst_ap, o, o[:])
                    continue
                for j in range(4):
                    ps = psp.get()
                    for kc in range(16):
                        P.op("tensor", lambda en, ps=ps, ht=ht, wb=wb, kc=kc, j=j, n=n: en.matmul(ps[:, 0:n], lhsT=wb[:, kc, j * 128:(j + 1) * 128], rhs=ht[:, kc, 0:n],
                                                                                                 start=(kc == 0), stop=(kc == 15)),
                             reads=[ht, wb], writes=[ps])
                    if kind in ("q", "k"):
                        head = (cg % 2) * 4 + j
                        o = obp.get()
                        if isctx:
                            P.op("scalar", lambda en, o=o, ps=ps, n=n: en.copy(out=o[:, 0:n], in_=ps[:, 0:n]), reads=[ps], writes=[o])
                        else:
                            qf = qfp.get()
                            P.op("scalar", lambda en, qf=qf, ps=ps: en.copy(out=qf[:], in_=ps[:]), reads=[ps], writes=[qf])
                            pr = prp.get()
                            P.op("tensor", lambda en, pr=pr, qf=qf: en.matmul(pr[:], lhsT=rm[:], rhs=qf[:], start=True, stop=True),
                                 reads=[rm, qf], writes=[pr])
                            t1 = t1p.get()
                            t2 = t2p.get()
                            P.op("vector", lambda en, t1=t1, qf=qf, t0=t0: en.tensor_tensor(out=t1[:], in0=qf[:], in1=cos[:, t0:t0 + 512], op=ALU.mult),
                                 reads=[qf, cos], writes=[t1])
                            P.op("vector", lambda en, t2=t2, pr=pr, t0=t0: en.tensor_tensor(out=t2[:], in0=pr[:], in1=sin[:, t0:t0 + 512], op=ALU.mult),
                                 reads=[pr, sin], writes=[t2])
                            P.op("vector", lambda en, o=o, t1=t1, t2=t2: en.tensor_tensor(out=o[:], in0=t1[:], in1=t2[:], op=ALU.add),
                                 reads=[t1, t2], writes=[o])
                        if kind == "q":
                            c0 = (OWN + t0 - S) if isctx else t0
                            self.store(self.QT, self.QT.t.ap()[head, :, c0:c0 + n], o, o[:, 0:n])
                        else:
                            self.store(self.KT, self.KT.t.ap()[head, :, t0:t0 + n], o, o[:, 0:n])
                    elif kind == "glug":
                        o = ofp.get()
                        P.op("scalar", lambda en, o=o, ps=ps, n=n: en.activation(out=o[:, 0:n], in_=ps[:, 0:n], func=AF.Sigmoid), reads=[ps], writes=[o])
                        c0 = (NGL + t0 - S) if isctx else t0
                        self.store(self.SG, self.SG.t.ap()[j, :, c0:c0 + n], o, o[:, 0:n])
                    elif kind == "glua":
                        c0 = (NGL + t0 - S) if isctx else t0
                        sg = sgp.get()
                        self.load(sg, sg[:, 0:n], self.SG, self.SG.t.ap()[j, :, c0:c0 + n])
                        o = ofp.get()
                        P.op("vector", lambda en, o=o, ps=ps, sg=sg, n=n: en.tensor_tensor(out=o[:, 0:n], in0=ps[:, 0:n], in1=sg[:, 0:n], op=ALU.mult),
                             reads=[ps, sg], writes=[o])
                        self.store(self.YG, self.YG.t.ap()[j, :, c0:c0 + n], o, o[:, 0:n])
                    else:
                        o = obp.get()
                        P.op("scalar", lambda en, o=o, ps=ps, n=n: en.activation(out=o[:, 0:n], in_=ps[:, 0:n], func=AF.Sigmoid), reads=[ps], writes=[o])
                        c0 = (OWN + t0 - S) if isctx else t0
                        ch = (cg - 11) * 4 + j
                        self.store(self.GT, self.GT.t.ap()[ch, :, c0:c0 + n], o, o[:, 0:n])
        self.phase_end()


def phase_attn(self):
    OWN, NQ, NGL, NG, NTL = self.OWN, self.NQ, self.NGL, self.NG, self.NTL
    P = self.P
    full = self.ctx_full
    lam_init = 0.8 - 0.6 * math.exp(-0.3 * self.l)
    self.phase_begin()
    lt = {}
    for nm in ("lam_q1", "lam_k1", "lam_q2", "lam_k2"):
        t = P.sb(nm + "_s", [128, 64], F32)
        src = self.inputs[nm]
        self.load(t, t[:], src, dap(src, 0, [[0, 128], [1, 64]]))
        lt[nm] = t
    pr1 = P.sb("lpr1", [128, 64], F32)
    pr2 = P.sb("lpr2", [128, 64], F32)
    e1 = P.sb("le1", [128, 1], F32)
    e2 = P.sb("le2", [128, 1], F32)
    nlam = P.sb("nlam", [128, 1], F32)
    P.op("vector", lambda en: en.tensor_tensor(out=pr1[:], in0=lt["lam_q1"][:], in1=lt["lam_k1"][:], op=ALU.mult), reads=[lt["lam_q1"], lt["lam_k1"]], writes=[pr1])
    P.op("vector", lambda en: en.tensor_tensor(out=pr2[:], in0=lt["lam_q2"][:], in1=lt["lam_k2"][:], op=ALU.mult), reads=[lt["lam_q2"], lt["lam_k2"]], writes=[pr2])
    P.op("vector", lambda en: en.reduce_sum(out=e1[:], in_=pr1[:], axis=AX.X), reads=[pr1], writes=[e1])
    P.op("vector", lambda en: en.reduce_sum(out=e2[:], in_=pr2[:], axis=AX.X), reads=[pr2], writes=[e2])
    P.op("scalar", lambda en: en.activation(out=e1[:], in_=e1[:], func=AF.Exp), reads=[e1], writes=[e1])
    P.op("scalar", lambda en: en.activation(out=e2[:], in_=e2[:], func=AF.Exp), reads=[e2], writes=[e2])
    P.op("vector", lambda en: en.scalar_tensor_tensor(out=nlam[:], in0=e2[:], scalar=-lam_init, in1=e1[:], op0=ALU.add, op1=ALU.subtract),
         reads=[e1, e2], writes=[nlam])
    gsub = P.sb("gsub", [128, 1], F32)
    sg_in = self.inputs["attn_subln_g"]
    self.load(gsub, gsub[:], sg_in, dap(sg_in, 0, [[1, 128], [1, 1]]))
    P.op("vector", lambda en: en.tensor_scalar(out=gsub[:], in0=gsub[:], scalar1=(1.0 - lam_init), scalar2=None, op0=ALU.mult), reads=[gsub], writes=[gsub])
    ones_f = P.sb("ones_fs", [128, 128], F32)
    ones_b = P.sb("ones_bs", [128, 128], BF16)
    self.load(ones_f, ones_f[:], self.ones_f, self.ones_f.t.ap())
    P.op("vector", lambda en: en.tensor_copy(out=ones_b[:], in_=ones_f[:]), reads=[ones_f], writes=[ones_b])

    qp = Pool(P, "q_sb", [128, NQ], BF16, 2)
    kp = Pool(P, "k_sb", [128, NT], BF16, 2)
    vp = Pool(P, "v_sb", [128, 34, 128], BF16, 2)
    sp = Pool(P, "s_ps", [128, 512], F32, 3, psum=True)
    O = [P.ps("o_ps%d" % c, [128, 512], F32) for c in range(2)]
    Z = [P.ps("z_ps%d" % c, [128, 512], F32) for c in range(2)]
    ssps = P.ps("ss_ps", [128, 512], F32)
    ep = Pool(P, "e_sb", [128, 512], BF16, 4)
    r1p = Pool(P, "r1", [128, 512], F32, 1)
    t1p = Pool(P, "at1", [128, 512], F32, 1)
    t2p = Pool(P, "at2", [128, 512], F32, 1)
    op_ = Pool(P, "ao", [128, 512], F32, 2)
    sqp = Pool(P, "asq", [128, 512], F32, 1)
    rsp = Pool(P, "ars", [128, 512], F32, 1)
    onp = Pool(P, "aon", [128, 512], BF16, 2)
    vview = self.V.t.ap().rearrange("(kt p) c -> p kt c", p=128)
    for h in range(8):
        q = qp.get(); k = kp.get(); v = vp.get()
        nq = NQ if full else OWN
        self.load(q, q[:, 0:nq], self.QT, self.QT.t.ap()[h, :, 0:nq])
        self.load(k, k[:], self.KT, self.KT.t.ap()[h])
        self.load(v, v[:], self.V, vview[:, :, h * 128:(h + 1) * 128])
        chunks = [(qc * 512, 512, list(range(34))) for qc in range(OWN // 512)]
        if full:
            chunks.append((OWN, 256, [32, 33]))
        for (q0, n, kts) in chunks:
            steps = [(c, kt) for c in range(2) for kt in kts]

            def emit_s(st):
                c, kt = st
                s = sp.get()
                P.op("tensor", lambda en, s=s, c=c, kt=kt: en.matmul(s[:, 0:n], lhsT=k[64 * c:64 * c + 64, kt * 128:(kt + 1) * 128],
                                                                     rhs=q[64 * c:64 * c + 64, q0:q0 + n], start=True, stop=True),
                     reads=[k, q], writes=[s])
                return s
            s_q = [emit_s(steps[0])]
            if len(steps) > 1:
                s_q.append(emit_s(steps[1]))
            for i, (c, kt) in enumerate(steps):
                if i + 2 < len(steps):
                    s_q.append(emit_s(steps[i + 2]))
                s_cur = s_q.pop(0)
                e = ep.get()
                P.op("scalar", lambda en, e=e, s=s_cur: en.activation(out=e[:, 0:n], in_=s[:, 0:n], func=AF.Exp, scale=0.125), reads=[s_cur], writes=[e])
                first = (kt == kts[0]); last = (kt == kts[-1])
                P.op("tensor", lambda en, e=e, c=c, kt=kt, first=first, last=last: en.matmul(O[c][:, 0:n], lhsT=v[:, kt, :], rhs=e[:, 0:n], start=first, stop=last),
                     reads=[v, e], writes=[O[c]])
                P.op("tensor", lambda en, e=e, c=c, first=first, last=last: en.matmul(Z[c][:, 0:n], lhsT=ones_b[:], rhs=e[:, 0:n], start=first, stop=last),
                     reads=[ones_b, e], writes=[Z[c]])
            r1 = r1p.get(); t1 = t1p.get(); t2 = t2p.get(); o = op_.get(); sq = sqp.get(); rs = rsp.get(); on = onp.get()
            P.op("vector", lambda en, r1=r1: en.reciprocal(out=r1[:, 0:n], in_=Z[0][:, 0:n]), reads=[Z[0]], writes=[r1])
            P.op("vector", lambda en, r1=r1, t1=t1: en.tensor_tensor(out=t1[:, 0:n], in0=O[0][:, 0:n], in1=r1[:, 0:n], op=ALU.mult), reads=[O[0], r1], writes=[t1])
            P.op("vector", lambda en, r1=r1: en.reciprocal(out=r1[:, 0:n], in_=Z[1][:, 0:n]), reads=[Z[1], r1], writes=[r1])
            P.op("vector", lambda en, r1=r1, t2=t2: en.tensor_tensor(out=t2[:, 0:n], in0=O[1][:, 0:n], in1=r1[:, 0:n], op=ALU.mult), reads=[O[1], r1], writes=[t2])
            P.op("vector", lambda en, o=o, t1=t1, t2=t2: en.scalar_tensor_tensor(out=o[:, 0:n], in0=t2[:, 0:n], scalar=nlam[:, 0:1], in1=t1[:, 0:n], op0=ALU.mult, op1=ALU.add),
                 reads=[t1, t2, nlam], writes=[o])
            P.op("scalar", lambda en, o=o, sq=sq: en.activation(out=sq[:, 0:n], in_=o[:, 0:n], func=AF.Square), reads=[o], writes=[sq])
            P.op("tensor", lambda en, sq=sq: en.matmul(ssps[:, 0:n], lhsT=ones_f[:], rhs=sq[:, 0:n], start=True, stop=True), reads=[ones_f, sq], writes=[ssps])
            P.op("vector", lambda en, rs=rs: en.tensor_scalar(out=rs[:, 0:n], in0=ssps[:, 0:n], scalar1=1.0 / 128, scalar2=1e-5, op0=ALU.mult, op1=ALU.add), reads=[ssps], writes=[rs])
            P.op("scalar", lambda en, rs=rs: en.activation(out=rs[:, 0:n], in_=rs[:, 0:n], func=AF.Sqrt), reads=[rs], writes=[rs])
            P.op("vector", lambda en, rs=rs: en.reciprocal(out=rs[:, 0:n], in_=rs[:, 0:n]), reads=[rs], writes=[rs])
            P.op("vector", lambda en, on=on, o=o, rs=rs: en.scalar_tensor_tensor(out=on[:, 0:n], in0=o[:, 0:n], scalar=gsub[:, 0:1], in1=rs[:, 0:n], op0=ALU.mult, op1=ALU.mult),
                 reads=[o, rs, gsub], writes=[on])
            self.store(self.ONT, self.ONT.t.ap()[h, :, q0:q0 + n], on, on[:, 0:n])
    self.phase_end()


LayerBuilder.phase_attn = phase_attn


def phase_conf(self):
    OWN, NQ, NGL, NG, NTL = self.OWN, self.NQ, self.NGL, self.NG, self.NTL
    P = self.P
    full = self.ctx_full
    self.phase_begin()
    cw = P.sb("cw", [128, 4, 31], F32)
    cv = P.sb("cv", [128, 3, 4], F32)
    self.load(cw, cw[:], self.inputs["conf_w"], self.inputs["conf_w"].t.ap().rearrange("p (c j) -> p c j", c=4))
    self.load(cv, cv[:], self.inputs["conf_v"], self.inputs["conf_v"].t.ap().rearrange("p (w c) -> p w c", w=3))
    ones_f = P.sb("ones_fc", [128, 128], F32)
    self.load(ones_f, ones_f[:], self.ones_f, self.ones_f.t.ap())
    seqs = [(0, min(OWN + 15, S), OWN, 0)]
    if full:
        seqs.append((NGL, 256, 256, OWN))
    yp = Pool(P, "cy", [128, 15 + OWN + 15 + 15], F32, 2)
    acc = P.sb("cacc", [128, 4, OWN], F32)
    sq = P.sb("csq", [128, 4, 512], F32)
    ps1 = P.ps("cps1", [128, 512], F32)
    ps2 = P.ps("cps2", [128, 512], F32)
    mean = P.sb("cmean", [128, 512], F32)
    msq = P.sb("cmsq", [128, 512], F32)
    var = P.sb("cvar", [128, 512], F32)
    dp = Pool(P, "cd", [128, 512], F32, 2)
    obp = Pool(P, "cob", [128, 512], BF16, 2)
    for (c0, n_in, n_out, o0) in seqs:
        for cc in range(4):
            y = yp.get()
            eng = "vector"
            P.op("gpsimd", lambda en, y=y: en.memset(y[:], 0.0), writes=[y])
            self.load(y, y[:, 15:15 + n_in], self.YG, self.YG.t.ap()[cc, :, c0:c0 + n_in])
            P.op(eng, lambda en, y=y, cc=cc: en.tensor_scalar(out=acc[:, cc, 0:n_out], in0=y[:, 0:n_out], scalar1=cw[:, cc, 0:1], scalar2=cv[:, 0, cc:cc + 1],
                                                             op0=ALU.mult, op1=ALU.add), reads=[y, cw, cv], writes=[acc])
            for j in range(1, 31):
                P.op(eng, lambda en, y=y, cc=cc, j=j: en.scalar_tensor_tensor(out=acc[:, cc, 0:n_out], in0=y[:, j:j + n_out], scalar=cw[:, cc, j:j + 1],
                                                                             in1=acc[:, cc, 0:n_out], op0=ALU.mult, op1=ALU.add),
                     reads=[y, cw, acc], writes=[acc])
        for tg in range((n_out + 511) // 512):
            n = min(512, n_out - tg * 512)
            sl = slice(tg * 512, tg * 512 + n)
            for cc in range(4):
                P.op("tensor", lambda en, cc=cc: en.matmul(ps1[:, 0:n], lhsT=ones_f[:], rhs=acc[:, cc, sl], start=(cc == 0), stop=(cc == 3)),
                     reads=[ones_f, acc], writes=[ps1])
            P.op("scalar", lambda en: en.activation(out=sq[:, :, 0:n], in_=acc[:, :, sl], func=AF.Square), reads=[acc], writes=[sq])
            for cc in range(4):
                P.op("tensor", lambda en, cc=cc: en.matmul(ps2[:, 0:n], lhsT=ones_f[:], rhs=sq[:, cc, 0:n], start=(cc == 0), stop=(cc == 3)),
                     reads=[ones_f, sq], writes=[ps2])
            P.op("scalar", lambda en: en.mul(out=mean[:, 0:n], in_=ps1[:, 0:n], mul=1.0 / 512), reads=[ps1], writes=[mean])
            P.op("vector", lambda en: en.tensor_tensor(out=msq[:, 0:n], in0=mean[:, 0:n], in1=mean[:, 0:n], op=ALU.mult), reads=[mean], writes=[msq])
            P.op("vector", lambda en: en.scalar_tensor_tensor(out=var[:, 0:n], in0=ps2[:, 0:n], scalar=1.0 / 512, in1=msq[:, 0:n], op0=ALU.mult, op1=ALU.subtract),
                 reads=[ps2, msq], writes=[var])
            P.op("vector", lambda en: en.tensor_scalar(out=var[:, 0:n], in0=var[:, 0:n], scalar1=1e-5, scalar2=None, op0=ALU.add), reads=[var], writes=[var])
            P.op("scalar", lambda en: en.activation(out=var[:, 0:n], in_=var[:, 0:n], func=AF.Sqrt), reads=[var], writes=[var])
            P.op("vector", lambda en: en.reciprocal(out=var[:, 0:n], in_=var[:, 0:n]), reads=[var], writes=[var])
            for cc in range(4):
                d = dp.get(); ob = obp.get()
                P.op("vector", lambda en, d=d, cc=cc: en.tensor_tensor(out=d[:, 0:n], in0=acc[:, cc, sl], in1=mean[:, 0:n], op=ALU.subtract), reads=[acc, mean], writes=[d])
                P.op("vector", lambda en, d=d: en.tensor_tensor(out=d[:, 0:n], in0=d[:, 0:n], in1=var[:, 0:n], op=ALU.mult), reads=[d, var], writes=[d])
                P.op("scalar", lambda en, d=d, ob=ob, cc=cc: en.activation(out=ob[:, 0:n], in_=d[:, 0:n], func=AF.Silu, scale=cv[:, 1, cc:cc + 1], bias=cv[:, 2, cc:cc + 1]),
                     reads=[d, cv], writes=[ob])
                self.store(self.CT, self.CT.t.ap()[cc, :, o0 + tg * 512:o0 + tg * 512 + n], ob, ob[:, 0:n])
    self.phase_end()


LayerBuilder.phase_conf = phase_conf


TWO_PI = 2.0 * math.pi


def phase_hy_short(self):
    P = self.P
    full = self.ctx_full
    self.phase_begin()
    W = []
    scw = self.inputs["hy_scw"]
    for j in range(3):
        t = P.sb("hsw%d" % j, [128, 1536], F32)
        self.load(t, t[:], scw, dap(scw, j * 1536, [[0, 128], [1, 1536]]))
        W.append(t)
    Bc = P.sb("hsb", [128, 1536], F32)
    scb = self.inputs["hy_scb"]
    self.load(Bc, Bc[:], scb, dap(scb, 0, [[0, 128], [1, 1536]]))
    pp = Pool(P, "hsp", [128, 1536], F32, 2)
    cp = Pool(P, "hsc", [128, 1536], F32, 2)
    np_ = Pool(P, "hsn", [128, 1536], F32, 2)
    tp = Pool(P, "hst", [128, 1536], F32, 2)
    t2p = Pool(P, "hst2", [128, 1536], F32, 2)
    seqs = [(0, 32)] + ([(S, 2)] if full else [])
    Uv = self.U.t.ap()
    for (r0s, nt) in seqs:
        for i in range(nt):
            r0 = r0s + i * 128
            pv = pp.get(); cu = cp.get(); nx = np_.get(); t = tp.get(); t2 = t2p.get()
            self.load(cu, cu[:], self.U, Uv[r0:r0 + 128, :])
            if i == 0:
                P.op("gpsimd", lambda en, pv=pv: en.memset(pv[:], 0.0), writes=[pv])
                self.load(pv, pv[1:128, :], self.U, Uv[r0:r0 + 127, :])
            else:
                self.load(pv, pv[:], self.U, Uv[r0 - 1:r0 + 127, :])
            if i == nt - 1:
                P.op("gpsimd", lambda en, nx=nx: en.memset(nx[:], 0.0), writes=[nx])
                self.load(nx, nx[0:127, :], self.U, Uv[r0 + 1:r0 + 128, :])
            else:
                self.load(nx, nx[:], self.U, Uv[r0 + 1:r0 + 129, :])
            P.op("vector", lambda en, t=t, pv=pv: en.tensor_tensor(out=t[:], in0=pv[:], in1=W[0][:], op=ALU.mult), reads=[pv, W[0]], writes=[t])
            P.op("gpsimd", lambda en, t2=t2, cu=cu: en.tensor_tensor(out=t2[:], in0=cu[:], in1=W[1][:], op=ALU.mult), reads=[cu, W[1]], writes=[t2])
            P.op("vector", lambda en, t=t, t2=t2: en.tensor_tensor(out=t[:], in0=t[:], in1=t2[:], op=ALU.add), reads=[t, t2], writes=[t])
            P.op("gpsimd", lambda en, t2=t2, nx=nx: en.tensor_tensor(out=t2[:], in0=nx[:], in1=W[2][:], op=ALU.mult), reads=[nx, W[2], t2], writes=[t2])
            P.op("gpsimd", lambda en, t2=t2: en.tensor_tensor(out=t2[:], in0=t2[:], in1=Bc[:], op=ALU.add), reads=[t2, Bc], writes=[t2])
            P.op("vector", lambda en, t=t, t2=t2: en.tensor_tensor(out=t[:], in0=t[:], in1=t2[:], op=ALU.add), reads=[t, t2], writes=[t])
            self.store(self.HU, self.HU.t.ap()[r0:r0 + 128, :], t, t[:])
    self.phase_end()


def hy_seqs(self):
    OWN, NQ, NGL, NG, NTL = self.OWN, self.NQ, self.NGL, self.NG, self.NTL
    seqs = [dict(L=S, NA=32, NO=OWN // 128, r0=0, o0=0, emb=self.inputs["emb_lat"], tv=self.inputs["tv_lat"], EK=self.EKL, H2D=self.H2L)]
    if self.ctx_full:
        seqs.append(dict(L=LC, NA=2, NO=2, r0=S, o0=OWN, emb=self.inputs["emb_ctx"], tv=self.inputs["tv_ctx"], EK=self.EKC, H2D=self.H2C))
    return seqs


def phase_hy_mlp(self):
    P = self.P
    self.phase_begin()
    hyv = P.sb("hyv", [64, 4], F32)
    self.load(hyv, hyv[:], self.inputs["hy_v"], self.inputs["hy_v"].t.ap())
    w1 = P.sb("hw1", [33, 64], F32)
    w2 = P.sb("hw2", [64, 64], F32)
    self.load(w1, w1[:], self.inputs["hy_w1"], self.inputs["hy_w1"].t.ap())
    self.load(w2, w2[:], self.inputs["hy_w2"], self.inputs["hy_w2"].t.ap())
    cs = P.sb("hcs", [64, 4], F32)
    for k in range(2):
        P.op("vector", lambda en, k=k: en.tensor_scalar(out=cs[:, 2 * k:2 * k + 1], in0=hyv[:, 2 * k + 1:2 * k + 2], scalar1=1.0 / TWO_PI, scalar2=None, op0=ALU.mult),
             reads=[hyv, cs], writes=[cs])
        P.op("vector", lambda en, k=k: en.tensor_tensor(out=cs[:, 2 * k + 1:2 * k + 2], in0=hyv[:, 2 * k:2 * k + 1], in1=cs[:, 2 * k:2 * k + 1], op=ALU.mult),
             reads=[hyv, cs], writes=[cs])
        P.op("vector", lambda en, k=k: en.tensor_scalar(out=cs[:, 2 * k + 1:2 * k + 2], in0=cs[:, 2 * k + 1:2 * k + 2], scalar1=0.0, scalar2=None, op0=ALU.add),
             reads=[cs], writes=[cs])
    negpi = P.sb("negpi", [64, 1], F32)
    P.op("vector", lambda en: en.memset(negpi[:], -math.pi), writes=[negpi])
    ep = Pool(P, "hemb", [33, 512], F32, 2)
    pp = Pool(P, "hmp", [64, 512], F32, 2, psum=True)
    up = Pool(P, "hmu", [64, 512], F32, 2)
    hp = Pool(P, "hmh", [64, 512], F32, 2)
    uip = Pool(P, "hmui", [64, 512], I32, 2)
    ufp = Pool(P, "hmuf", [64, 512], F32, 2)
    for sq in hy_seqs(self):
        npos = 2 * sq["L"] - 1
        for c0 in range(0, npos, 512):
            n = min(512, npos - c0)
            e = ep.get()
            self.load(e, e[:, 0:n], sq["emb"], sq["emb"].t.ap()[:, c0:c0 + n])
            h = None
            for k, w in enumerate((w1, w2)):
                ps = pp.get()
                rhs = e if k == 0 else h
                P.op("tensor", lambda en, ps=ps, w=w, rhs=rhs: en.matmul(ps[:, 0:n], lhsT=w[:], rhs=rhs[:, 0:n], start=True, stop=True), reads=[w, rhs], writes=[ps])
                u = up.get()
                P.op("vector", lambda en, u=u, ps=ps, k=k: en.tensor_scalar(out=u[:, 0:n], in0=ps[:, 0:n], scalar1=cs[:, 2 * k:2 * k + 1], scalar2=cs[:, 2 * k + 1:2 * k + 2],
                                                                            op0=ALU.mult, op1=ALU.add), reads=[ps, cs], writes=[u])
                ui = uip.get(); uf = ufp.get()
                P.op("vector", lambda en, u=u, ui=ui: en.tensor_copy(out=ui[:, 0:n], in_=u[:, 0:n]), reads=[u], writes=[ui])
                P.op("vector", lambda en, uf=uf, ui=ui: en.tensor_copy(out=uf[:, 0:n], in_=ui[:, 0:n]), reads=[ui], writes=[uf])
                P.op("vector", lambda en, u=u, uf=uf: en.tensor_tensor(out=u[:, 0:n], in0=u[:, 0:n], in1=uf[:, 0:n], op=ALU.subtract), reads=[u, uf], writes=[u])
                P.op("vector", lambda en, u=u, uf=uf: en.tensor_scalar(out=uf[:, 0:n], in0=u[:, 0:n], scalar1=0.5, scalar2=None, op0=ALU.is_gt), reads=[u, uf], writes=[uf])
                P.op("vector", lambda en, u=u, uf=uf: en.tensor_tensor(out=u[:, 0:n], in0=u[:, 0:n], in1=uf[:, 0:n], op=ALU.subtract), reads=[u, uf], writes=[u])
                P.op("vector", lambda en, u=u, uf=uf: en.tensor_scalar(out=uf[:, 0:n], in0=u[:, 0:n], scalar1=-0.5, scalar2=None, op0=ALU.is_lt), reads=[u, uf], writes=[uf])
                P.op("vector", lambda en, u=u, uf=uf: en.tensor_tensor(out=u[:, 0:n], in0=u[:, 0:n], in1=uf[:, 0:n], op=ALU.add), reads=[u, uf], writes=[u])
                h = hp.get()
                P.op("scalar", lambda en, u=u, h=h: en.activation(out=h[:, 0:n], in_=u[:, 0:n], func=AF.Sin, scale=TWO_PI), reads=[u], writes=[h])
            self.store(sq["H2D"], sq["H2D"].t.ap()[:, c0:c0 + n], h, h[:, 0:n])
    self.phase_end()


def phase_hy_filt(self):
    P = self.P
    self.phase_begin()
    w3 = P.sb("hw3", [64, 2048], F32)
    self.load(w3, w3[:], self.inputs["hy_w3"], self.inputs["hy_w3"].t.ap())
    dec = P.sb("hdec", [128, 16], F32)
    self.load(dec, dec[:], self.inputs["hy_dec"], self.inputs["hy_dec"].t.ap())
    P.op("scalar", lambda en: en.activation(out=dec[:], in_=dec[:], func=AF.Abs), reads=[dec], writes=[dec])
    P.op("vector", lambda en: en.tensor_scalar(out=dec[:], in0=dec[:], scalar1=-1.0, scalar2=None, op0=ALU.mult), reads=[dec], writes=[dec])
    h2 = P.sb("hh2", [64, 8191], F32)
    tvb = P.sb("htvb", [128, 8191], F32)
    et = P.sb("het", [128, 8191], F32)
    eb = P.sb("heb", [128, 8191], BF16)
    junk = P.sb("hjunk", [128, 8191], BF16)
    ssum = P.sb("hssum", [128, 1], F32)
    pp = Pool(P, "hfp", [128, 512], F32, 2, psum=True)
    wp = Pool(P, "hfw", [128, 512], F32, 2)
    for sq in hy_seqs(self):
        L = sq["L"]
        npos = 2 * L - 1
        self.load(h2, h2[:, 0:npos], sq["H2D"], sq["H2D"].t.ap())
        self.load(tvb, tvb[:, 0:npos], sq["tv"], dap(sq["tv"], 0, [[0, 128], [1, npos]]))
        chunks = []
        for (a, b, d) in ((0, L - 1, 1), (L - 1, npos, 0)):
            for c0 in range(a, b, 512):
                chunks.append((c0, min(512, b - c0), d))
        for o in range(2):
            for cc in range(4):
                for (c0, n, d) in chunks:
                    col = o * 1024 + d * 512 + cc * 128
                    di = (o * 2 + d) * 4 + cc
                    ps = pp.get()
                    P.op("tensor", lambda en, ps=ps, col=col, c0=c0, n=n: en.matmul(ps[:, 0:n], lhsT=w3[:, col:col + 128], rhs=h2[:, c0:c0 + n], start=True, stop=True),
                         reads=[w3, h2], writes=[ps])
                    w = wp.get()
                    P.op("scalar", lambda en, w=w, c0=c0, n=n, di=di: en.activation(out=w[:, 0:n], in_=tvb[:, c0:c0 + n], func=AF.Exp, scale=dec[:, di:di + 1]),
                         reads=[tvb, dec], writes=[w])
                    P.op("vector", lambda en, ps=ps, w=w, c0=c0, n=n: en.tensor_tensor(out=et[:, c0:c0 + n], in0=ps[:, 0:n], in1=w[:, 0:n], op=ALU.mult),
                         reads=[ps, w], writes=[et])
                P.op("vector", lambda en: en.memset(ssum[:], 0.0), writes=[ssum])
                P.op("scalar", lambda en: en.activation(out=junk[:, 0:npos], in_=et[:, 0:npos], func=AF.Abs, accum_out=ssum[:]), reads=[et, ssum], writes=[junk, ssum])
                P.op("vector", lambda en: en.reciprocal(out=ssum[:], in_=ssum[:]), reads=[ssum], writes=[ssum])
                P.op("vector", lambda en: en.tensor_scalar(out=eb[:, 0:npos], in0=et[:, 0:npos], scalar1=ssum[:, 0:1], scalar2=None, op0=ALU.mult), reads=[et, ssum], writes=[eb])
                row0 = (o * 4 + cc) * 128
                self.store(sq["EK"], sq["EK"].t.ap()[row0:row0 + 128, 0:npos], eb, eb[:, 0:npos])
    self.phase_end()


def phase_hy_conv(self):
    OWN, NQ, NGL, NG, NTL = self.OWN, self.NQ, self.NGL, self.NG, self.NTL
    P = self.P
    self.phase_begin()
    jrev = P.sb("jrev", [128, 128], F32)
    self.load(jrev, jrev[:], self.inputs["jrev"], self.inputs["jrev"].t.ap())
    idf = P.sb("hidf", [128, 128], F32)
    self.load(idf, idf[:], self.ident_f, self.ident_f.t.ap())
    hb = self.inputs["hy_bias"]
    B1 = P.sb("hB1", [128, 1, 512], F32)
    B2 = P.sb("hB2", [128, 512, 1], F32)
    self.load(B1, B1[:, 0, :], hb, dap(hb, 0, [[0, 128], [1, 512]]))
    self.load(B2, B2[:, :, 0], hb, dap(hb, 512, [[0, 128], [1, 512]]))
    NAM = 32
    VV = P.sb("hVV", [128, NAM, 128], F32)
    X1 = P.sb("hX1", [128, NAM, 128], F32)
    NOM = OWN // 128
    X2 = P.sb("hX2", [128, NOM, 128], F32)
    Zp = P.sb("hZp", [128, 128, 3 * NAM - 2], BF16)
    Y1 = P.sb("hY1", [128, 128, NAM], F32)
    Y2 = P.sb("hY2", [128, 128, NOM], F32)
    Z2 = X1
    T = P.sb("hT", [128, NAM, 128], F32)
    ZTs = P.sb("hZTs", [128, NOM * 128], BF16)
    bandp = Pool(P, "hband", [128, 63 * 128], BF16, 3)
    jp = Pool(P, "hjp", [128, 512], F32, 2, psum=True)
    yp = Pool(P, "hyp", [128, 16, 32], F32, 2, psum=True)
    tpp = Pool(P, "htp", [128, 4, 128], F32, 2, psum=True)
    HUv = self.HU.t.ap()
    for sq in hy_seqs(self):
        NA, NO, r0, o0, EK = sq["NA"], sq["NO"], sq["r0"], sq["o0"], sq["EK"]
        PW = 3 * NA - 2
        BW = (2 * NA - 1) * 128
        npos = 2 * sq["L"] - 1
        EW = EK.t.ap().shape[1]
        hu3 = HUv[r0:r0 + NA * 128, :].rearrange("(a p) c -> p a c", p=128)
        for cc in range(4):
            self.load(X1, X1[:, 0:NA, :], self.HU, hu3[:, :, cc * 128:(cc + 1) * 128])
            self.load(X2, X2[:, 0:NO, :], self.HU, hu3[:, 0:NO, 512 + cc * 128:512 + (cc + 1) * 128])
            self.load(VV, VV[:, 0:NA, :], self.HU, hu3[:, :, 1024 + cc * 128:1024 + (cc + 1) * 128])
            for order in range(2):
                nout = NA if order == 0 else NO
                P.op("gpsimd", lambda en: en.memset(Zp[:], 0.0), writes=[Zp])
                if order == 0:
                    ab = 4 if NA >= 4 else NA
                    for a0 in range(0, NA, ab):
                        ps = jp.get()
                        P.op("tensor", lambda en, ps=ps, a0=a0, ab=ab: en.matmul(ps[:, 0:ab * 128], lhsT=jrev[:], rhs=VV[:, a0:a0 + ab, :], start=True, stop=True),
                             reads=[jrev, VV], writes=[ps])
                        P.op("scalar", lambda en, ps=ps, a0=a0, ab=ab, NA=NA: en.copy(out=Zp[:, :, NA - 1 + a0:NA - 1 + a0 + ab].rearrange("p c a -> p a c"),
                                                                                   in_=ps[:, 0:ab * 128].rearrange("p (a c) -> p a c", a=ab)),
                             reads=[ps], writes=[Zp])
                else:
                    cb = min(128, 512 // NA)
                    for c0 in range(0, 128, cb):
                        ps = jp.get()
                        P.op("tensor", lambda en, ps=ps, c0=c0, cb=cb, NA=NA: en.matmul(ps[:, 0:cb * NA], lhsT=jrev[:], rhs=Y1[:, c0:c0 + cb, 0:NA], start=True, stop=True),
                             reads=[jrev, Y1], writes=[ps])
                        P.op("scalar", lambda en, ps=ps, c0=c0, cb=cb, NA=NA: en.copy(out=Zp[:, c0:c0 + cb, NA - 1:2 * NA - 1],
                                                                                   in_=ps[:, 0:cb * NA].rearrange("p (c a) -> p c a", c=cb)),
                             reads=[ps], writes=[Zp])
                dmax = NA - 1 if order == 0 else NO - 1
                deltas = list(range(-(NA - 1), dmax + 1))
                Yout = Y1 if order == 0 else Y2
                yps = None
                for c in range(128):
                    band = bandp.get()
                    row = (order * 4 + cc) * 128 + c
                    BWo = (NA + dmax) * 128
                    self.load(band, band[:, 0:BWo], EK, dap(EK, row * EW, [[1, 128], [1, BWo]]))
                    if c % 16 == 0:
                        yps = yp.get()
                    for d in deltas:
                        P.op("tensor", lambda en, yps=yps, band=band, c=c, d=d, NA=NA, nout=nout: en.matmul(
                            yps[:, c % 16, 0:nout], lhsT=band[:, (d + NA - 1) * 128:(d + NA) * 128], rhs=Zp[:, c, NA - 1 - d:NA - 1 - d + nout],
                            start=(d == deltas[0]), stop=(d == deltas[-1])), reads=[band, Zp], writes=[yps])
                    if c % 16 == 15:
                        c0 = c - 15
                        if order == 0:
                            P.op("vector", lambda en, yps=yps, c0=c0, nout=nout: en.tensor_copy(out=Y1[:, c0:c0 + 16, 0:nout], in_=yps[:, :, 0:nout]), reads=[yps], writes=[Y1])
                        else:
                            P.op("vector", lambda en, yps=yps, c0=c0, nout=nout: en.tensor_copy(out=Y2[:, c0:c0 + 16, 0:nout], in_=yps[:, :, 0:nout]), reads=[yps], writes=[Y2])
                if order == 0:
                    P.op("vector", lambda en, NA=NA: en.tensor_tensor(out=T[:, 0:NA, :], in0=VV[:, 0:NA, :], in1=B1[:, :, cc * 128:(cc + 1) * 128].to_broadcast([128, NA, 128]), op=ALU.mult),
                         reads=[VV, B1], writes=[T])
                    P.op("vector", lambda en, NA=NA: en.tensor_tensor(out=Y1[:, :, 0:NA], in0=Y1[:, :, 0:NA], in1=T[:, 0:NA, :].rearrange("p a c -> p c a"), op=ALU.add),
                         reads=[Y1, T], writes=[Y1])
                    P.op("vector", lambda en, NA=NA: en.tensor_tensor(out=Y1[:, :, 0:NA], in0=Y1[:, :, 0:NA], in1=X1[:, 0:NA, :].rearrange("p a c -> p c a"), op=ALU.mult),
                         reads=[Y1, X1], writes=[Y1])
                else:
                    P.op("vector", lambda en, NO=NO: en.tensor_tensor(out=T[:, 0:NO, :].rearrange("p a c -> p c a"), in0=Y1[:, :, 0:NO],
                                                                      in1=B2[:, cc * 128:(cc + 1) * 128, :].to_broadcast([128, 128, NO]), op=ALU.mult),
                         reads=[Y1, B2], writes=[T])
                    P.op("vector", lambda en, NO=NO: en.tensor_tensor(out=T[:, 0:NO, :].rearrange("p a c -> p c a"), in0=T[:, 0:NO, :].rearrange("p a c -> p c a"),
                                                                      in1=Y2[:, :, 0:NO], op=ALU.add), reads=[T, Y2], writes=[T])
                    P.op("vector", lambda en, NO=NO: en.tensor_tensor(out=Z2[:, 0:NO, :], in0=T[:, 0:NO, :], in1=X2[:, 0:NO, :], op=ALU.mult), reads=[T, X2], writes=[Z2])
            for a0 in range(0, NO, 4):
                ab = min(4, NO - a0)
                tp = tpp.get()
                for a in range(ab):
                    P.op("tensor", lambda en, tp=tp, a=a, a0=a0: en.transpose(tp[:, a, :], Z2[:, a0 + a, :], idf[:]), reads=[Z2, idf], writes=[tp])
                P.op("scalar", lambda en, tp=tp, a0=a0, ab=ab: en.copy(out=ZTs[:, a0 * 128:(a0 + ab) * 128], in_=tp[:, 0:ab, :]), reads=[tp], writes=[ZTs])
            self.store(self.ZT, self.ZT.t.ap()[cc, :, o0:o0 + NO * 128], ZTs, ZTs[:, 0:NO * 128])
    self.phase_end()


LayerBuilder.phase_hy_short = phase_hy_short
LayerBuilder.phase_hy_mlp = phase_hy_mlp
LayerBuilder.phase_hy_filt = phase_hy_filt
LayerBuilder.phase_hy_conv = phase_hy_conv


def phase_merge(self):
    OWN, NQ, NGL, NG, NTL = self.OWN, self.NQ, self.NGL, self.NG, self.NTL
    P = self.P
    full = self.ctx_full
    self.phase_begin()
    wst = Pool(P, "mws", [128, 16, 512], F32, 2)
    wbp = Pool(P, "mwb", [128, 16, 512], BF16, 2)
    inp_ = Pool(P, "min", [128, 16, 512], BF16, 2)
    gp = Pool(P, "mg", [128, 3, 512], BF16, 2)
    pa = Pool(P, "mpa", [128, 512], F32, 2, psum=True)
    pc = Pool(P, "mpc", [128, 512], F32, 2, psum=True)
    ph = Pool(P, "mph", [128, 512], F32, 2, psum=True)
    mp = Pool(P, "mm", [128, 512], F32, 2)
    tp = Pool(P, "mt", [128, 512], F32, 2)
    obp = Pool(P, "mob", [128, 512], BF16, 2)
    wa = self.inputs["w_attn_o"].t.ap().rearrange("(k p) n -> p k n", p=128)
    wc = self.inputs["w_conf_o"].t.ap().rearrange("(k p) n -> p k n", p=128)
    wh = self.inputs["w_hy_o"].t.ap().rearrange("(k p) n -> p k n", p=128)
    groups = [(g * 512, 512) for g in range(OWN // 512)] + ([(OWN, 256)] if full else [])
    gtv = self.GT.t.ap().rearrange("(b d) p t -> b d p t", b=3)
    for dg in range(4):
        ws = wst.get()
        self.load(ws, ws[:, 0:8, :], self.inputs["w_attn_o"], wa[:, :, dg * 512:(dg + 1) * 512])
        self.load(ws, ws[:, 8:12, :], self.inputs["w_conf_o"], wc[:, :, dg * 512:(dg + 1) * 512])
        self.load(ws, ws[:, 12:16, :], self.inputs["w_hy_o"], wh[:, :, dg * 512:(dg + 1) * 512])
        wb = wbp.get()
        P.op("gpsimd", lambda en, wb=wb, ws=ws: en.tensor_copy(out=wb[:, 0:8, :], in_=ws[:, 0:8, :]), reads=[ws], writes=[wb])
        P.op("gpsimd", lambda en, wb=wb, ws=ws: en.tensor_copy(out=wb[:, 8:16, :], in_=ws[:, 8:16, :]), reads=[ws, wb], writes=[wb])
        for (t0, n) in groups:
            x = inp_.get()
            self.load(x, x[:, 0:8, 0:n], self.ONT, self.ONT.t.ap().rearrange("h p t -> p h t")[:, :, t0:t0 + n])
            self.load(x, x[:, 8:12, 0:n], self.CT, self.CT.t.ap().rearrange("h p t -> p h t")[:, :, t0:t0 + n])
            self.load(x, x[:, 12:16, 0:n], self.ZT, self.ZT.t.ap().rearrange("h p t -> p h t")[:, :, t0:t0 + n])
            for j in range(4):
                dch = dg * 4 + j
                g = gp.get()
                self.load(g, g[:, :, 0:n], self.GT, gtv[:, dch].rearrange("b p t -> p b t")[:, :, t0:t0 + n])
                pss = []
                for (pool, k0, k1) in ((pa, 0, 8), (pc, 8, 12), (ph, 12, 16)):
                    ps = pool.get()
                    for k in range(k0, k1):
                        P.op("tensor", lambda en, ps=ps, wb=wb, x=x, k=k, j=j, k0=k0, k1=k1: en.matmul(ps[:, 0:n], lhsT=wb[:, k, j * 128:(j + 1) * 128], rhs=x[:, k, 0:n],
                                                                                                   start=(k == k0), stop=(k == k1 - 1)), reads=[wb, x], writes=[ps])
                    pss.append(ps)
                m = mp.get(); t = tp.get(); ob = obp.get()
                P.op("vector", lambda en, m=m, g=g, ps=pss[0]: en.tensor_tensor(out=m[:, 0:n], in0=ps[:, 0:n], in1=g[:, 0, 0:n], op=ALU.mult), reads=[pss[0], g], writes=[m])
                P.op("vector", lambda en, t=t, g=g, ps=pss[1]: en.tensor_tensor(out=t[:, 0:n], in0=ps[:, 0:n], in1=g[:, 1, 0:n], op=ALU.mult), reads=[pss[1], g], writes=[t])
                P.op("gpsimd", lambda en, m=m, t=t: en.tensor_tensor(out=m[:, 0:n], in0=m[:, 0:n], in1=t[:, 0:n], op=ALU.add), reads=[m, t], writes=[m])
                P.op("vector", lambda en, t=t, g=g, ps=pss[2]: en.tensor_tensor(out=t[:, 0:n], in0=ps[:, 0:n], in1=g[:, 2, 0:n], op=ALU.mult), reads=[pss[2], g, t], writes=[t])
                P.op("gpsimd", lambda en, m=m, t=t, ob=ob: en.tensor_tensor(out=ob[:, 0:n], in0=m[:, 0:n], in1=t[:, 0:n], op=ALU.add), reads=[m, t], writes=[ob])
                self.store(self.MG, self.MG.t.ap()[dch, :, t0:t0 + n], ob, ob[:, 0:n])
    self.phase_end()


def phase_wout(self):
    OWN, NQ, NGL, NG, NTL = self.OWN, self.NQ, self.NGL, self.NG, self.NTL
    P = self.P
    full = self.ctx_full
    self.phase_begin()
    wst = Pool(P, "ows", [128, 16, 512], F32, 2)
    wbp = Pool(P, "owb", [128, 16, 512], BF16, 2)
    mgp = Pool(P, "omg", [128, 16, 128], BF16, 3)
    xp = Pool(P, "ox", [128, 512], F32, 3)
    pp = Pool(P, "ops", [128, 512], F32, 3, psum=True)
    tp = Pool(P, "ot", [128, 512], F32, 2)
    G1 = [P.sb("oG1_%d" % r, [128, D], F32) for r in range(2)]
    for r in range(2):
        self.bcast_mod(G1[r], r, 2)
    wo = self.inputs["w_out"].t.ap().rearrange("(k p) n -> p k n", p=128)
    mgv = self.MG.t.ap().rearrange("k p t -> p k t")
    ntile = NTL + (2 if full else 0)
    for cg in range(4):
        ws = wst.get()
        self.load(ws, ws[:], self.inputs["w_out"], wo[:, :, cg * 512:(cg + 1) * 512])
        wb = wbp.get()
        P.op("gpsimd", lambda en, wb=wb, ws=ws: en.tensor_copy(out=wb[:, 0:8, :], in_=ws[:, 0:8, :]), reads=[ws], writes=[wb])
        P.op("gpsimd", lambda en, wb=wb, ws=ws: en.tensor_copy(out=wb[:, 8:16, :], in_=ws[:, 8:16, :]), reads=[ws, wb], writes=[wb])
        for i in range(ntile):
            row = 0 if i < NTL else 1
            mg = mgp.get()
            self.load(mg, mg[:], self.MG, mgv[:, :, i * 128:(i + 1) * 128])
            x = xp.get()
            if row == 0:
                self.load(x, x[:], self.xa, self.xa.t.ap()[i * 128:(i + 1) * 128, cg * 512:(cg + 1) * 512])
            else:
                self.load(x, x[:], self.xc, self.xc.t.ap()[(i - NTL) * 128:(i - NTL + 1) * 128, cg * 512:(cg + 1) * 512])
            ps = pp.get()
            for k in range(16):
                P.op("tensor", lambda en, ps=ps, mg=mg, wb=wb, k=k: en.matmul(ps[:], lhsT=mg[:, k, :], rhs=wb[:, k, :], start=(k == 0), stop=(k == 15)), reads=[mg, wb], writes=[ps])
            t = tp.get()
            P.op("vector", lambda en, t=t, ps=ps, row=row: en.tensor_tensor(out=t[:], in0=ps[:], in1=G1[row][:, cg * 512:(cg + 1) * 512], op=ALU.mult), reads=[ps, G1[row]], writes=[t])
            P.op("gpsimd", lambda en, t=t, x=x: en.tensor_tensor(out=t[:], in0=t[:], in1=x[:], op=ALU.add), reads=[t, x], writes=[t])
            self.store(self.X1, self.X1.t.ap()[i * 128:(i + 1) * 128, cg * 512:(cg + 1) * 512], t, t[:])
    self.phase_end()


LayerBuilder.phase_merge = phase_merge
LayerBuilder.phase_wout = phase_wout


def moe_dims(self):
    OWN, NQ, NGL, NG, NTL = self.OWN, self.NQ, self.NGL, self.NG, self.NTL
    T = NQ if self.ctx_full else OWN
    ntile = T // 128
    NB = (2 * T) // 128 + 64
    return T, ntile, NB


def declare_moe(self):
    OWN, NQ, NGL, NG, NTL = self.OWN, self.NQ, self.NGL, self.NG, self.NTL
    inp = self.inp
    T, ntile, NB = moe_dims(self)
    inp("w_r", [D, 72])
    inp("w_gate", [32768, 2048]); inp("w_up", [32768, 2048]); inp("w_down", [32768, 2048])
    inp("tri", [128, 128]); inp("u64", [64, 128])
    inp("jv", [1, NB]); inp("tokid", [128, ntile], I32); inp("iota_gu", [128, 16]); inp("iota_d", [128, 4])
    inp("norm_f_g", [1, D])
    sc = self.scratch
    self.H2 = sc("H2", [T + 128, D], F32)
    self.BT = sc("BT", [NB * 128, 1], I32)
    self.Y = sc("Y", [NB * 128, D], F32)
    self.XOL = sc("XOL", [OWN, D], F32, out=self.ext_out)
    self.XOC = sc("XOC", [LC, D], F32, out=(self.ext_out and self.ctx_full))
    P = self.P
    self.d1 = P.sb("pd1", [128, ntile], I32)
    self.d2 = P.sb("pd2", [128, ntile], I32)
    self.w1 = P.sb("pw1", [128, ntile], F32)
    self.w2 = P.sb("pw2", [128, ntile], F32)
    self.be = P.sb("pbe", [128, NB], F32)


def phase_router(self):
    OWN, NQ, NGL, NG, NTL = self.OWN, self.NQ, self.NGL, self.NG, self.NTL
    P = self.P
    T, ntile, NB = moe_dims(self)
    self.phase_begin()
    AB = self.make_AB(self.norm2_g, 3, 4, "n2")
    idf = P.sb("ridf", [128, 128], F32)
    self.load(idf, idf[:], self.ident_f, self.ident_f.t.ap())
    ones = P.sb("rones", [128, 128], F32)
    self.load(ones, ones[:], self.ones_f, self.ones_f.t.ap())
    tri = P.sb("rtri", [128, 128], F32)
    self.load(tri, tri[:], self.inputs["tri"], self.inputs["tri"].t.ap())
    u64 = P.sb("ru64", [64, 128], F32)
    self.load(u64, u64[:], self.inputs["u64"], self.inputs["u64"].t.ap())
    wr = P.sb("rwr", [128, 16, 72], F32)
    self.load(wr, wr[:], self.inputs["w_r"], self.inputs["w_r"].t.ap().rearrange("(k p) n -> p k n", p=128))
    LG = P.sb("rLG", [128, ntile, 72], F32)
    xp = Pool(P, "rx", [128, D], F32, 2)
    sqp = Pool(P, "rsq", [128, D], F32, 1)
    tmpp = Pool(P, "rtmp", [128, D], F32, 1)
    hp = Pool(P, "rh", [128, D], F32, 2)
    ssp = Pool(P, "rss", [128, 1], F32, 2)
    rsp = Pool(P, "rrs", [128, 1], F32, 2)
    htp = Pool(P, "rht", [128, 16, 128], F32, 2)
    ptp = Pool(P, "rpt", [128, 4, 128], F32, 2, psum=True)
    plp = Pool(P, "rpl", [128, 72], F32, 2, psum=True)
    zero = P.sb("rzero", [128, D], F32)
    P.op("gpsimd", lambda en: en.memset(zero[:], 0.0), writes=[zero])
    self.store(self.H2, self.H2.t.ap()[T:T + 128, :], zero, zero[:])
    for i in range(ntile):
        row = 0 if i < NTL else 1
        x = xp.get()
        self.load(x, x[:], self.X1, self.X1.t.ap()[i * 128:(i + 1) * 128, :])
        h = hp.get()
        A, Bt = AB[row]
        self.norm_tile(x, A, Bt, h, sqp.get(), ssp.get(), rsp.get(), tmpp.get())
        self.store(self.H2, self.H2.t.ap()[i * 128:(i + 1) * 128, :], h, h[:])
        ht = htp.get()
        for k4 in range(4):
            pt = ptp.get()
            for a in range(4):
                kc = k4 * 4 + a
                P.op("tensor", lambda en, pt=pt, a=a, kc=kc, h=h: en.transpose(pt[:, a, :], h[:, kc * 128:(kc + 1) * 128], idf[:]), reads=[h, idf], writes=[pt])
            if k4 % 2 == 0:
                P.op("scalar", lambda en, pt=pt, ht=ht, k4=k4: en.copy(out=ht[:, k4 * 4:k4 * 4 + 4, :], in_=pt[:]), reads=[pt], writes=[ht])
            else:
                P.op("vector", lambda en, pt=pt, ht=ht, k4=k4: en.tensor_copy(out=ht[:, k4 * 4:k4 * 4 + 4, :], in_=pt[:]), reads=[pt], writes=[ht])
        pl = plp.get()
        for kc in range(16):
            P.op("tensor", lambda en, pl=pl, ht=ht, kc=kc: en.matmul(pl[:], lhsT=ht[:, kc, :], rhs=wr[:, kc, :], start=(kc == 0), stop=(kc == 15)), reads=[ht, wr], writes=[pl])
        P.op("vector", lambda en, pl=pl, i=i: en.tensor_copy(out=LG[:, i, :], in_=pl[:]), reads=[pl], writes=[LG])
    nt = ntile

    def sbt(name, shape, dt=F32):
        return P.sb(name, shape, dt)
    gmax = sbt("gmax", [128, nt, 1]); gmask = sbt("gmask", [128, nt, 8]); dd = sbt("rdd", [128, nt, 8]); sm = sbt("rsm", [128, nt, 1]); pg = sbt("rpg", [128, nt, 1])
    tmp4 = sbt("rtmp4", [128, nt, 8, 8]); les = sbt("rles", [128, nt, 8]); v1 = sbt("rv1", [128, nt, 1]); v2 = sbt("rv2", [128, nt, 1])
    m1 = sbt("rm1", [128, nt, 8]); m2 = sbt("rm2", [128, nt, 8]); le2 = sbt("rle2", [128, nt, 8]); ex = sbt("rex", [128, nt, 1])
    M1 = sbt("rM1", [128, nt, 64]); M2 = sbt("rM2", [128, nt, 64]); M = sbt("rM", [128, nt, 64])
    V = "vector"
    lg = LG[:, :, 0:8]
    le4 = LG[:, :, 8:72].rearrange("p t (g e) -> p t g e", g=8)
    P.op(V, lambda en: en.tensor_reduce(out=gmax[:], in_=lg, axis=AX.X, op=ALU.max), reads=[LG], writes=[gmax])
    P.op(V, lambda en: en.tensor_tensor(out=gmask[:], in0=lg, in1=gmax[:].to_broadcast([128, nt, 8]), op=ALU.is_equal), reads=[LG, gmax], writes=[gmask])
    P.op(V, lambda en: en.tensor_tensor(out=dd[:], in0=lg, in1=gmax[:].to_broadcast([128, nt, 8]), op=ALU.subtract), reads=[LG, gmax], writes=[dd])
    P.op("scalar", lambda en: en.activation(out=dd[:], in_=dd[:], func=AF.Exp), reads=[dd], writes=[dd])
    P.op(V, lambda en: en.tensor_reduce(out=sm[:], in_=dd[:], axis=AX.X, op=ALU.add), reads=[dd], writes=[sm])
    P.op(V, lambda en: en.reciprocal(out=pg[:], in_=sm[:]), reads=[sm], writes=[pg])
    P.op(V, lambda en: en.tensor_tensor(out=tmp4[:], in0=le4, in1=gmask[:].rearrange("p t (g o) -> p t g o", o=1).to_broadcast([128, nt, 8, 8]), op=ALU.mult),
         reads=[LG, gmask], writes=[tmp4])
    P.op(V, lambda en: en.tensor_reduce(out=les[:], in_=tmp4[:].rearrange("p t g e -> p t e g"), axis=AX.X, op=ALU.add), reads=[tmp4], writes=[les])
    P.op(V, lambda en: en.tensor_reduce(out=v1[:], in_=les[:], axis=AX.X, op=ALU.max), reads=[les], writes=[v1])
    P.op(V, lambda en: en.tensor_tensor(out=m1[:], in0=les[:], in1=v1[:].to_broadcast([128, nt, 8]), op=ALU.is_equal), reads=[les, v1], writes=[m1])
    P.op(V, lambda en: en.scalar_tensor_tensor(out=le2[:], in0=m1[:], scalar=-1e30, in1=les[:], op0=ALU.mult, op1=ALU.add), reads=[m1, les], writes=[le2])
    P.op(V, lambda en: en.tensor_reduce(out=v2[:], in_=le2[:], axis=AX.X, op=ALU.max), reads=[le2], writes=[v2])
    P.op(V, lambda en: en.tensor_tensor(out=m2[:], in0=le2[:], in1=v2[:].to_broadcast([128, nt, 8]), op=ALU.is_equal), reads=[le2, v2], writes=[m2])
    P.op(V, lambda en: en.tensor_tensor(out=ex[:], in0=v2[:], in1=v1[:], op=ALU.subtract), reads=[v1, v2], writes=[ex])
    P.op("scalar", lambda en: en.activation(out=ex[:], in_=ex[:], func=AF.Exp), reads=[ex], writes=[ex])
    P.op(V, lambda en: en.tensor_scalar(out=ex[:], in0=ex[:], scalar1=1.0, scalar2=None, op0=ALU.add), reads=[ex], writes=[ex])
    P.op(V, lambda en: en.reciprocal(out=ex[:], in_=ex[:]), reads=[ex], writes=[ex])
    P.op(V, lambda en: en.tensor_tensor(out=self.w1[:], in0=pg[:, :, 0], in1=ex[:, :, 0], op=ALU.mult), reads=[pg, ex], writes=[self.w1])
    P.op(V, lambda en: en.tensor_tensor(out=self.w2[:], in0=pg[:, :, 0], in1=self.w1[:], op=ALU.subtract), reads=[pg, self.w1], writes=[self.w2])
    for (Mk, mk) in ((M1, m1), (M2, m2)):
        P.op(V, lambda en, Mk=Mk, mk=mk: en.tensor_tensor(out=Mk[:].rearrange("p t (g e) -> p t g e", g=8),
                                                          in0=gmask[:].rearrange("p t (g o) -> p t g o", o=1).to_broadcast([128, nt, 8, 8]),
                                                          in1=mk[:].rearrange("p t (o e) -> p t o e", o=1).to_broadcast([128, nt, 8, 8]), op=ALU.mult),
             reads=[gmask, mk], writes=[Mk])
    P.op(V, lambda en: en.tensor_tensor(out=M[:], in0=M1[:], in1=M2[:], op=ALU.add), reads=[M1, M2], writes=[M])
    pcb = P.ps("rpcb", [128, 64], F32)
    pct = P.ps("rpct", [64, 128], F32)
    for i in range(nt):
        P.op("tensor", lambda en, i=i: en.matmul(pcb[:], lhsT=ones[:], rhs=M[:, i, :], start=(i == 0), stop=(i == nt - 1)), reads=[ones, M], writes=[pcb])
    for i in range(nt):
        P.op("tensor", lambda en, i=i: en.matmul(pct[:], lhsT=M[:, i, :], rhs=ones[:], start=(i == 0), stop=(i == nt - 1)), reads=[ones, M], writes=[pct])
    cT = sbt("rcT", [64, 128]); rT = sbt("rrT", [64, 128])
    P.op(V, lambda en: en.tensor_copy(out=cT[:], in_=pct[:]), reads=[pct], writes=[cT])
    qT = sbt("rqT", [64, 128]); qi = sbt("rqi", [64, 128], I32)
    P.op(V, lambda en: en.tensor_scalar(out=qT[:], in0=cT[:], scalar1=1.0 / 128, scalar2=None, op0=ALU.mult), reads=[cT], writes=[qT])
    P.op(V, lambda en: en.tensor_copy(out=qi[:], in_=qT[:]), reads=[qT], writes=[qi])
    P.op(V, lambda en: en.tensor_copy(out=rT[:], in_=qi[:]), reads=[qi], writes=[rT])
    P.op(V, lambda en: en.tensor_tensor(out=qT[:], in0=rT[:], in1=qT[:], op=ALU.is_lt), reads=[rT, qT], writes=[qT])
    P.op(V, lambda en: en.tensor_tensor(out=rT[:], in0=rT[:], in1=qT[:], op=ALU.add), reads=[rT, qT], writes=[rT])
    P.op(V, lambda en: en.tensor_scalar(out=cT[:], in0=rT[:], scalar1=128.0, scalar2=None, op0=ALU.mult), reads=[rT], writes=[cT])
    pst = P.ps("rpst", [128, 128], F32)
    P.op("tensor", lambda en: en.matmul(pst[:], lhsT=cT[:], rhs=u64[:], start=True, stop=True), reads=[cT, u64], writes=[pst])
    pse = sbt("rpse", [128, 128])
    P.op(V, lambda en: en.tensor_copy(out=pse[:], in_=pst[:]), reads=[pst], writes=[pse])
    pcum = Pool(P, "rpcum", [128, 64], F32, 1, psum=True)
    pos = Pool(P, "rpos", [128, 64], F32, 2)
    jk = Pool(P, "rjk", [128, 64], F32, 2)
    d1f = sbt("rd1f", [128, nt]); d2f = sbt("rd2f", [128, nt])
    for i in range(nt):
        pc = pcum.get()
        P.op("tensor", lambda en, pc=pc, i=i: en.matmul(pc[:], lhsT=tri[:], rhs=M[:, i, :], start=True, stop=(i == 0)), reads=[tri, M], writes=[pc])
        for i2 in range(i):
            P.op("tensor", lambda en, pc=pc, i2=i2, i=i: en.matmul(pc[:], lhsT=ones[:], rhs=M[:, i2, :], start=False, stop=(i2 == i - 1)), reads=[ones, M], writes=[pc])
        po = pos.get()
        P.op(V, lambda en, po=po, pc=pc: en.tensor_tensor(out=po[:], in0=pc[:], in1=pse[:, 0:64], op=ALU.add), reads=[pc, pse], writes=[po])
        for (Mk, df) in ((M1, d1f), (M2, d2f)):
            j = jk.get()
            P.op(V, lambda en, j=j, Mk=Mk, po=po, i=i: en.tensor_tensor(out=j[:], in0=Mk[:, i, :], in1=po[:], op=ALU.mult), reads=[Mk, po], writes=[j])
            P.op(V, lambda en, j=j, df=df, i=i: en.tensor_reduce(out=df[:, i:i + 1], in_=j[:], axis=AX.X, op=ALU.add), reads=[j, df], writes=[df])
    P.op(V, lambda en: en.tensor_copy(out=self.d1[:], in_=d1f[:]), reads=[d1f], writes=[self.d1])
    P.op(V, lambda en: en.tensor_copy(out=self.d2[:], in_=d2f[:]), reads=[d2f], writes=[self.d2])
    jv = sbt("rjv", [128, NB, 1])
    self.load(jv, jv[:, :, 0], self.inputs["jv"], dap(self.inputs["jv"], 0, [[0, 128], [1, NB]]))
    JC = 33
    cmp_ = sbt("rcmp", [128, JC, 64])
    bef = sbt("rbef", [128, NB, 1])
    for j0 in range(0, NB, JC):
        jn = min(JC, NB - j0)
        P.op(V, lambda en, j0=j0, jn=jn: en.tensor_tensor(out=cmp_[:, 0:jn, :], in0=pse[:, 64:128].rearrange("p (o e) -> p o e", o=1).to_broadcast([128, jn, 64]),
                                                          in1=jv[:, j0:j0 + jn, :].to_broadcast([128, jn, 64]), op=ALU.is_le), reads=[pse, jv], writes=[cmp_])
        P.op(V, lambda en, j0=j0, jn=jn: en.tensor_reduce(out=bef[:, j0:j0 + jn, :], in_=cmp_[:, 0:jn, :], axis=AX.X, op=ALU.add), reads=[cmp_, bef], writes=[bef])
    P.op(V, lambda en: en.tensor_scalar(out=self.be[:], in0=bef[:, :, 0], scalar1=63.0, scalar2=None, op0=ALU.min), reads=[bef], writes=[self.be])
    bti = P.sb("rbti", [128, NB], I32)
    P.op("gpsimd", lambda en: en.memset(bti[:], T), writes=[bti])
    self.store(self.BT, self.BT.t.ap().rearrange("(p j) o -> p (j o)", p=128), bti, bti[:])
    tok = P.sb("rtok", [128, nt], I32)
    self.load(tok, tok[:], self.inputs["tokid"], self.inputs["tokid"].t.ap())
    BT = self.BT
    for i in range(nt):
        for dk in (self.d1, self.d2):
            P.dma("gpsimd", lambda en, dk=dk, i=i: en.indirect_dma_start(out=BT.t.ap(), out_offset=bass.IndirectOffsetOnAxis(ap=dk[:, i:i + 1], axis=0),
                                                                        in_=tok[:, i:i + 1], in_offset=None),
                  reads=[tok, dk, BT], writes=[BT], sb=tok, into=False)
    self.phase_end()


def phase_experts(self):
    OWN, NQ, NGL, NG, NTL = self.OWN, self.NQ, self.NGL, self.NG, self.NTL
    P = self.P
    T, ntile, NB = moe_dims(self)
    self.phase_begin()
    idf = P.sb("eidf", [128, 128], F32)
    self.load(idf, idf[:], self.ident_f, self.ident_f.t.ap())
    igu = P.sb("eigu", [128, 16], F32)
    self.load(igu, igu[:], self.inputs["iota_gu"], self.inputs["iota_gu"].t.ap())
    be128 = P.sb("ebe128", [128, NB], F32)
    P.op("vector", lambda en: en.tensor_scalar(out=be128[:], in0=self.be[:], scalar1=128.0, scalar2=igu[:, 0:1], op0=ALU.mult, op1=ALU.add), reads=[self.be, igu], writes=[be128])
    idx2 = P.sb("eidx2", [128, NB, 4], F32)
    for qq in range(4):
        P.op("vector", lambda en, qq=qq: en.tensor_scalar(out=idx2[:, :, qq], in0=be128[:], scalar1=8192.0 * qq, scalar2=None, op0=ALU.add), reads=[be128, idx2], writes=[idx2])
    idxi = P.sb("eidxi", [128, NB, 4], I32)
    P.op("vector", lambda en: en.tensor_copy(out=idxi[:], in_=idx2[:]), reads=[idx2], writes=[idxi])
    tokp = Pool(P, "etok", [128, 1], I32, 3)
    xbp = Pool(P, "exb", [128, D], F32, 2)
    xtp = Pool(P, "exT", [128, 16, 128], F32, 2)
    Wg = [P.sb("eWg%d" % k, [128, 2048], F32) for k in range(4)]
    Wu = [P.sb("eWu%d" % k, [128, 2048], F32) for k in range(4)]
    Wd = [P.sb("eWd%d" % k, [128, 2048], F32) for k in range(4)]
    ptp = Pool(P, "ept", [128, 4, 128], F32, 2, psum=True)
    pg = P.ps("epg", [128, 512], F32)
    pu = P.ps("epu", [128, 512], F32)
    py = [P.ps("epy%d" % n, [128, 512], F32) for n in range(4)]
    sgp = Pool(P, "esg", [128, 512], F32, 2)
    acp = Pool(P, "eac", [128, 512], F32, 2)
    atp = Pool(P, "eaT", [128, 4, 128], F32, 2)
    ybp = Pool(P, "eyb", [128, D], F32, 2)
    wg_in, wu_in, wd_in = self.inputs["w_gate"], self.inputs["w_up"], self.inputs["w_down"]
    for j in range(NB):
        tk = tokp.get()
        self.load(tk, tk[:], self.BT, self.BT.t.ap()[j * 128:(j + 1) * 128, :])
        xb = xbp.get()
        H2 = self.H2
        P.dma("gpsimd", lambda en, xb=xb, tk=tk: en.indirect_dma_start(out=xb[:], out_offset=None, in_=H2.t.ap(),
                                                                       in_offset=bass.IndirectOffsetOnAxis(ap=tk[:, 0:1], axis=0)),
              reads=[H2, tk], writes=[xb], sb=xb)
        for (Wt, win) in ((Wg, wg_in), (Wu, wu_in), (Wd, wd_in)):
            for hf in range(4):
                P.dma("gpsimd", lambda en, Wt=Wt, win=win, hf=hf, j=j: en.indirect_dma_start(out=Wt[hf][:], out_offset=None, in_=win.t.ap(),
                                                                                           in_offset=bass.IndirectOffsetOnAxis(ap=idxi[:, j, hf:hf + 1], axis=0)),
                      reads=[win, idxi], writes=[Wt[hf]], sb=Wt[hf])
        xT = xtp.get()
        for k4 in range(4):
            pt = ptp.get()
            for a in range(4):
                kc = k4 * 4 + a
                P.op("tensor", lambda en, pt=pt, a=a, kc=kc, xb=xb: en.transpose(pt[:, a, :], xb[:, kc * 128:(kc + 1) * 128], idf[:]), reads=[xb, idf], writes=[pt])
            if k4 % 2 == 0:
                P.op("scalar", lambda en, pt=pt, xT=xT, k4=k4: en.copy(out=xT[:, k4 * 4:k4 * 4 + 4, :], in_=pt[:]), reads=[pt], writes=[xT])
            else:
                P.op("vector", lambda en, pt=pt, xT=xT, k4=k4: en.tensor_copy(out=xT[:, k4 * 4:k4 * 4 + 4, :], in_=pt[:]), reads=[pt], writes=[xT])
        for (ps_, Wt) in ((pg, Wg), (pu, Wu)):
            for kc in range(16):
                P.op("tensor", lambda en, xT=xT, kc=kc, ps_=ps_, Wt=Wt: en.matmul(ps_[:], lhsT=xT[:, kc, :], rhs=Wt[kc // 4][:, (kc % 4) * 512:(kc % 4 + 1) * 512],
                                                                                 start=(kc == 0), stop=(kc == 15)), reads=[xT, Wt[kc // 4]], writes=[ps_])
        sg = sgp.get(); ac = acp.get()
        P.op("scalar", lambda en, sg=sg: en.activation(out=sg[:], in_=pg[:], func=AF.Silu), reads=[pg], writes=[sg])
        P.op("vector", lambda en, sg=sg, ac=ac: en.tensor_tensor(out=ac[:], in0=pu[:], in1=sg[:], op=ALU.mult), reads=[pu, sg], writes=[ac])
        pt = ptp.get()
        for fc in range(4):
            P.op("tensor", lambda en, pt=pt, fc=fc, ac=ac: en.transpose(pt[:, fc, :], ac[:, fc * 128:(fc + 1) * 128], idf[:]), reads=[ac, idf], writes=[pt])
        aT = atp.get()
        P.op("scalar", lambda en, pt=pt, aT=aT: en.copy(out=aT[:], in_=pt[:]), reads=[pt], writes=[aT])
        yb = ybp.get()
        for n in range(4):
            for fc in range(4):
                P.op("tensor", lambda en, aT=aT, n=n, fc=fc: en.matmul(py[n][:], lhsT=aT[:, fc, :], rhs=Wd[fc][:, n * 512:(n + 1) * 512],
                                                                      start=(fc == 0), stop=(fc == 3)), reads=[aT, Wd[fc]], writes=[py[n]])
            if n % 2 == 0:
                P.op("scalar", lambda en, yb=yb, n=n: en.copy(out=yb[:, n * 512:(n + 1) * 512], in_=py[n][:]), reads=[py[n], yb], writes=[yb])
            else:
                P.op("vector", lambda en, yb=yb, n=n: en.tensor_copy(out=yb[:, n * 512:(n + 1) * 512], in_=py[n][:]), reads=[py[n], yb], writes=[yb])
        self.store(self.Y, self.Y.t.ap()[j * 128:(j + 1) * 128, :], yb, yb[:], eng="sync")
    self.phase_end()


def phase_combine(self):
    OWN, NQ, NGL, NG, NTL = self.OWN, self.NQ, self.NGL, self.NG, self.NTL
    P = self.P
    T, ntile, NB = moe_dims(self)
    final = (self.l == 1)
    self.phase_begin()
    G2 = [P.sb("cG2_%d" % r, [128, D], F32) for r in range(2)]
    for r in range(2):
        self.bcast_mod(G2[r], r, 5)
    if final:
        gf = P.sb("cgf", [128, D], F32)
        nf = self.inputs["norm_f_g"]
        self.load(gf, gf[:], nf, dap(nf, 0, [[0, 128], [1, D]]))
    y1p = Pool(P, "cy1", [128, D], F32, 2)
    y2p = Pool(P, "cy2", [128, D], F32, 2)
    xp = Pool(P, "cx", [128, D], F32, 2)
    mp = Pool(P, "cm", [128, D], F32, 2)
    sqp = Pool(P, "csq2", [128, D], F32, 1)
    ssp = Pool(P, "css", [128, 1], F32, 2)
    Y = self.Y
    for i in range(ntile):
        row = 0 if i < NTL else 1
        y1 = y1p.get(); y2 = y2p.get()
        for (yt, dk) in ((y1, self.d1), (y2, self.d2)):
            P.dma("gpsimd", lambda en, yt=yt, dk=dk, i=i: en.indirect_dma_start(out=yt[:], out_offset=None, in_=Y.t.ap(),
                                                                              in_offset=bass.IndirectOffsetOnAxis(ap=dk[:, i:i + 1], axis=0)),
                  reads=[Y, dk], writes=[yt], sb=yt)
        x = xp.get()
        self.load(x, x[:], self.X1, self.X1.t.ap()[i * 128:(i + 1) * 128, :])
        m = mp.get()
        P.op("vector", lambda en, m=m, y1=y1, i=i: en.tensor_scalar(out=m[:], in0=y1[:], scalar1=self.w1[:, i:i + 1], scalar2=None, op0=ALU.mult), reads=[y1, self.w1], writes=[m])
        P.op("vector", lambda en, m=m, y2=y2, i=i: en.scalar_tensor_tensor(out=m[:], in0=y2[:], scalar=self.w2[:, i:i + 1], in1=m[:], op0=ALU.mult, op1=ALU.add),
             reads=[y2, self.w2, m], writes=[m])
        if "MOE" in self.taps:
            self.store(self.MOE, self.MOE.t.ap()[i * 128:(i + 1) * 128, :], m, m[:])
        P.op("gpsimd", lambda en, m=m, row=row: en.tensor_tensor(out=m[:], in0=m[:], in1=G2[row][:], op=ALU.mult), reads=[m, G2[row]], writes=[m])
        P.op("gpsimd", lambda en, m=m, x=x: en.tensor_tensor(out=m[:], in0=m[:], in1=x[:], op=ALU.add), reads=[m, x], writes=[m])
        if final:
            sq = sqp.get(); ss = ssp.get()
            P.op("scalar", lambda en, sq=sq, ss=ss, m=m: en.activation(out=sq[:], in_=m[:], func=AF.Square, accum_out=ss[:]), reads=[m], writes=[sq, ss])
            P.op("vector", lambda en, ss=ss: en.tensor_scalar(out=ss[:], in0=ss[:], scalar1=1.0 / D, scalar2=EPS, op0=ALU.mult, op1=ALU.add), reads=[ss], writes=[ss])
            P.op("scalar", lambda en, ss=ss: en.activation(out=ss[:], in_=ss[:], func=AF.Sqrt), reads=[ss], writes=[ss])
            P.op("vector", lambda en, ss=ss: en.reciprocal(out=ss[:], in_=ss[:]), reads=[ss], writes=[ss])
            P.op("vector", lambda en, m=m, ss=ss: en.scalar_tensor_tensor(out=m[:], in0=m[:], scalar=ss[:, 0:1], in1=gf[:], op0=ALU.mult, op1=ALU.mult), reads=[m, ss, gf], writes=[m])
        if i < NTL:
            self.store(self.XOL, self.XOL.t.ap()[i * 128:(i + 1) * 128, :], m, m[:], eng="sync")
        else:
            self.store(self.XOC, self.XOC.t.ap()[(i - NTL) * 128:(i - NTL + 1) * 128, :], m, m[:], eng="sync")
    self.phase_end()


LayerBuilder.declare_moe = declare_moe
LayerBuilder.phase_router = phase_router
LayerBuilder.phase_experts = phase_experts
LayerBuilder.phase_combine = phase_combine


ALL_PHASES = ["mod", "norm1", "inproj", "attn", "conf", "hy_short", "hy_mlp", "hy_filt", "hy_conv", "merge", "wout", "router", "experts", "combine"]


def build_layer(layer, taps=(), phases=None, own=2048):
    Bd = LayerBuilder(layer, taps=taps, own=own)
    Bd.declare()
    for ph in (phases or ALL_PHASES):
        getattr(Bd, "phase_" + ph)()
    Bd.P.wait_all("gpsimd", list(Bd.outputs.values()))
    Bd.P.wait_all("sync", list(Bd.outputs.values()))
    Bd.P.emit()
    return Bd


def build_fused():
    L0 = LayerBuilder(0, own=S, sfx="_0", ext_out=False)
    L0.declare()
    for ph in ALL_PHASES:
        getattr(L0, "phase_" + ph)()
    L1 = LayerBuilder(1, own=2048, shared=(L0.nc, L0.P), sfx="_1", xa=L0.XOL, xc=L0.XOC, ext_out=True)
    L1.declare()
    for ph in ALL_PHASES:
        getattr(L1, "phase_" + ph)()
    outs = list(L1.outputs.values())
    L1.P.wait_all("gpsimd", outs)
    L1.P.wait_all("sync", outs)
    L1.P.emit()
    return L0, L1


GRID_W = 64
ROPE_BASE = 10000.0


def core_order(half):
    return np.arange(S) if half == 0 else np.arange(S - 1, -1, -1)


def rope_tables(order):
    rows = (order // GRID_W).astype(np.float32)
    cols = (order % GRID_W).astype(np.float32)
    inv = (np.float32(ROPE_BASE) ** (-np.arange(0, 32, 2, dtype=np.float32) / np.float32(32))).astype(np.float32)
    cos_t = np.zeros((128, S), np.float32)
    sin_t = np.zeros((128, S), np.float32)
    for p in range(128):
        d = p % 64
        pos = rows if d < 32 else cols
        dd = d % 32
        ang = (pos * inv[dd % 16]).astype(np.float32)
        cos_t[p] = np.cos(ang)
        sin_t[p] = -np.sin(ang) if dd < 16 else np.sin(ang)
    return cos_t, sin_t


def rope_perm():
    rm = np.zeros((128, 128), np.float32)
    for m in range(128):
        dd = m % 32
        base = m - dd
        rm[base + (dd + 16) % 32, m] = 1.0
    return rm


def hy_emb_ext(L):
    n = np.arange(L, dtype=np.float32)
    t = (n / np.float32(max(L - 1, 1))).astype(np.float32)
    w = (np.float32(2.0 * np.pi) * n / np.float32(L)).astype(np.float32)
    f = np.linspace(1e-4, 15, 16, dtype=np.float32)
    fw = (w[:, None] * f[None, :]).astype(np.float32)
    emb = np.concatenate([t[:, None], np.cos(fw), -np.sin(fw)], axis=-1).astype(np.float32)
    idx = np.abs(np.arange(2 * L - 1) - (L - 1))
    return np.ascontiguousarray(emb[idx].T), np.ascontiguousarray(t[idx].reshape(1, -1))


_WCACHE = {}


def prep_core(inp, l, b, half, xl=None, xc=None, own=2048, sfx=""):
    order = core_order(half)
    xl = inp["x"] if xl is None else xl
    xc = inp["ctx"] if xc is None else xc
    m = {}
    m["xa"] = np.ascontiguousarray(xl[b][order])
    m["xc"] = np.ascontiguousarray(xc[b] if half == 0 else xc[b][::-1])
    cvec = np.stack([inp["c"][b], inp["c_ctx"]])
    m["ct"] = np.ascontiguousarray(cvec.reshape(2, 16, 128).transpose(2, 1, 0).reshape(128, 32))
    m["w_mod"] = inp["w_mod"][l]
    m["b_mod"] = inp["b_mod"][l].reshape(1, -1)
    m["norm1_g"] = inp["norm1_g"][l].reshape(1, -1)
    m["norm2_g"] = inp["norm2_g"][l].reshape(1, -1)
    m["w_in"] = inp["w_in"][l]
    c, s = rope_tables(order)
    m["cos_t"], m["sin_t"] = c, s
    m["rm"] = rope_perm()
    for nm in ("lam_q1", "lam_k1", "lam_q2", "lam_k2", "attn_subln_g"):
        m[nm] = inp[nm][l].reshape(1, -1)
    cw = inp["conf_dw_w"][l]
    if half == 1:
        cw = cw[::-1]
    m["conf_w"] = np.ascontiguousarray(cw.T.reshape(4, 128, 31).transpose(1, 0, 2).reshape(128, 124))
    cv = np.stack([inp["conf_dw_b"][l], inp["conf_ln_g"][l], inp["conf_ln_b"][l]])
    m["conf_v"] = np.ascontiguousarray(cv.reshape(3, 4, 128).transpose(2, 0, 1).reshape(128, 12))
    scw = inp["hy_sc_w"][l]
    if half == 1:
        scw = scw[::-1]
    m["hy_scw"] = np.ascontiguousarray(scw.reshape(1, -1))
    m["hy_scb"] = inp["hy_sc_b"][l].reshape(1, -1)
    fr = inp["hy_freq"][l]
    m["hy_v"] = np.ascontiguousarray(np.stack([inp["hy_b1"][l], fr[0], inp["hy_b2"][l], fr[1]], axis=1))
    m["hy_w1"] = inp["hy_w1"][l]
    m["hy_w2"] = inp["hy_w2"][l]
    w3 = inp["hy_w3"][l].reshape(64, 2, 2, 512)
    dec = inp["hy_decay"][l].reshape(2, 2, 512)
    if half == 1:
        w3 = w3[:, :, ::-1]
        dec = dec[:, ::-1]
    m["hy_w3"] = np.ascontiguousarray(w3.reshape(64, 2048))
    m["hy_dec"] = np.ascontiguousarray(dec.reshape(2, 2, 4, 128).transpose(3, 0, 1, 2).reshape(128, 16))
    m["hy_bias"] = inp["hy_bias"][l].reshape(1, -1)
    m["jrev"] = np.ascontiguousarray(np.eye(128, dtype=np.float32)[::-1])
    for nm, L in (("lat", S), ("ctx", LC)):
        e, tv = hy_emb_ext(L)
        m["emb_" + nm] = e
        m["tv_" + nm] = tv
    for nm in ("w_attn_o", "w_conf_o", "w_hy_o", "w_out"):
        m[nm] = inp[nm][l]
    T = own + (LC if l == 0 else 0)
    NB = (2 * T) // 128 + 64
    wre = inp["w_router_expert"][l].transpose(1, 0, 2).reshape(D, 64)
    m["w_r"] = np.ascontiguousarray(np.concatenate([inp["w_router_group"][l], wre], axis=1))
    for nm, src in (("w_gate", "w_exp_gate"), ("w_up", "w_exp_up"), ("w_down", "w_exp_down")):
        key = (nm, l)
        if key not in _WCACHE:
            w = inp[src][l]
            if nm == "w_down":
                w = w.reshape(64, 4, 1, 128, 2048)
            else:
                w = w.reshape(64, 4, 4, 128, 512)
            _WCACHE[key] = np.ascontiguousarray(w.transpose(1, 0, 3, 2, 4)).reshape(32768, 2048)
        m[nm] = _WCACHE[key]
    m["tri"] = np.triu(np.ones((128, 128), np.float32), 1)
    u = np.triu(np.ones((64, 64), np.float32), 1)
    ui = np.triu(np.ones((64, 64), np.float32), 0)
    m["u64"] = np.ascontiguousarray(np.concatenate([u, ui], axis=1))
    m["jv"] = (np.arange(NB, dtype=np.float32) * 128).reshape(1, NB)
    m["tokid"] = np.ascontiguousarray((np.arange(T // 128)[None, :] * 128 + np.arange(128)[:, None]).astype(np.int32))
    m["iota_gu"] = np.ascontiguousarray((np.arange(16)[None, :] * 128 + np.arange(128)[:, None]).astype(np.float32))
    m["iota_d"] = np.ascontiguousarray((np.arange(4)[None, :] * 128 + np.arange(128)[:, None]).astype(np.float32))
    m["norm_f_g"] = inp["norm_f_g"].reshape(1, -1)
    m["ident_f"] = np.eye(128, dtype=np.float32)
    m["ones_f"] = np.ones((128, 128), np.float32)
    return {k + sfx: v for k, v in m.items()}


def kernel(**inputs):
    inp = {k: np.asarray(v) for k, v in inputs.items()}
    _WCACHE.clear()
    B = inp["x"].shape[0]
    L0, L1 = build_fused()
    needed = [k + "_0" for k in L0.inputs] + [k + "_1" for k in L1.inputs]
    in_maps = []
    for core in range(8):
        b, half = divmod(core, 2)
        m = prep_core(inp, 0, b, half, own=S, sfx="_0")
        m.update(prep_core(inp, 1, b, half, own=2048, sfx="_1"))
        in_maps.append({k: np.ascontiguousarray(m[k]) for k in needed})
    res = run_bass_kernel_spmd(L0.nc, in_maps, core_ids=list(range(8)))
    out = np.empty((B, S, D), np.float32)
    for core in range(8):
        b, half = divmod(core, 2)
        order = core_order(half)
        out[b][order[:2048]] = np.asarray(res.results[core]["XOL_1"])
    return out
```
